# Optimizing a Trainium2 kernel written in Bass

```python
import math
import jax
import jax.numpy as jnp
from jax import lax
import numpy as np


D_MODEL = 1024
BATCH = 8
SEQ = 2048
DEPTH = 1

F32 = jnp.float32
EPS = 1e-6
PLE_DIM = 256

A_HEADS = 8
A_HEAD_DIM = 64
A_WIDTH = A_HEADS * A_HEAD_DIM
MOBA_BLOCK = 256
MOBA_TOPK = 3
MOBA_QCHUNK = 16

SSD_D_INNER = D_MODEL
SSD_HEAD_DIM = 64
SSD_HEADS = SSD_D_INNER // SSD_HEAD_DIM
SSD_GROUPS = 2
SSD_D_STATE = 128
SSD_CONV = 4
SSD_CHUNK = 256
SSD_CONV_DIM = SSD_D_INNER + 2 * SSD_GROUPS * SSD_D_STATE

MOE_GROUPS = 4
MOE_EXPERTS_PER_GROUP = 4
MOE_EXPERTS = MOE_GROUPS * MOE_EXPERTS_PER_GROUP
MOE_TOPK = 2
MOE_D_FF = 512

OFF_Q = 0
OFF_K = OFF_Q + A_WIDTH
OFF_V = OFF_K + A_WIDTH
OFF_Z = OFF_V + A_WIDTH
OFF_XBC = OFF_Z + SSD_D_INNER
OFF_DT = OFF_XBC + SSD_CONV_DIM
OFF_GA = OFF_DT + SSD_HEADS
OFF_GB = OFF_GA + D_MODEL
IN_DIM = OFF_GB + D_MODEL

kernel_name = 'hybrid_moba_ssd_hmoe_block'


def rms_norm(x, g):
    xf = x.astype(F32)
    y = xf * lax.rsqrt(jnp.mean(xf * xf, axis=-1, keepdims=True) + EPS)
    return (y * g.astype(F32)).astype(x.dtype)


def alibi_slopes(n):
    return 2.0 ** (-8.0 * jnp.arange(1, n + 1, dtype=F32) / n)


def moba_attention(q, k, v):
    bsz, s, nh, hd = q.shape
    nb = -(-s // MOBA_BLOCK)
    s_pad = nb * MOBA_BLOCK
    pad = ((0, 0), (0, s_pad - s), (0, 0), (0, 0))
    q, k, v = [jnp.pad(t, pad).transpose(0, 2, 1, 3) for t in (q, k, v)]
    scale = hd ** -0.5
    kb = k.reshape(bsz, nh, nb, MOBA_BLOCK, hd)
    vb = v.reshape(bsz, nh, nb, MOBA_BLOCK, hd)
    k_mean = jnp.mean(kb.astype(F32), axis=3)
    gate = jnp.einsum('bhtd,bhnd->bhtn', q.astype(F32), k_mean)
    q_blk = jnp.arange(s_pad) // MOBA_BLOCK
    past = jnp.arange(nb)[None, :] < q_blk[:, None]
    gate = jnp.where(past[None, None], gate, -jnp.inf)
    topk = min(MOBA_TOPK, nb)
    _, top_idx = lax.top_k(gate, topk)
    sel_valid = jnp.arange(topk)[None, :] < q_blk[:, None]
    slopes = alibi_slopes(nh)[None, :, None, None]
    bi = jnp.arange(bsz)[:, None, None, None]
    hi = jnp.arange(nh)[None, :, None, None]
    offs = jnp.arange(MOBA_BLOCK)
    n_chunks = s_pad // MOBA_QCHUNK

    def chunk(c):
        t0 = c * MOBA_QCHUNK
        qc = lax.dynamic_slice_in_dim(q, t0, MOBA_QCHUNK, axis=2)
        idx = lax.dynamic_slice_in_dim(top_idx, t0, MOBA_QCHUNK, axis=2)
        valid = lax.dynamic_slice_in_dim(sel_valid, t0, MOBA_QCHUNK, axis=0)
        t = t0 + jnp.arange(MOBA_QCHUNK)
        kg = kb[bi, hi, idx]
        vg = vb[bi, hi, idx]
        s_sel = jnp.einsum('bhqd,bhqkld->bhqkl', qc, kg, preferred_element_type=F32) * scale
        kpos = idx[..., None] * MOBA_BLOCK + offs
        s_sel = s_sel - slopes[..., None] * (t[:, None, None] - kpos).astype(F32)
        s_sel = jnp.where(valid[:, :, None], s_sel, -jnp.inf)
        blk0 = (t0 // MOBA_BLOCK) * MOBA_BLOCK
        ko = lax.dynamic_slice_in_dim(k, blk0, MOBA_BLOCK, axis=2)
        vo = lax.dynamic_slice_in_dim(v, blk0, MOBA_BLOCK, axis=2)
        s_own = jnp.einsum('bhqd,bhld->bhql', qc, ko, preferred_element_type=F32) * scale
        dist = t[:, None] - (blk0 + offs)[None, :]
        s_own = jnp.where(dist >= 0, s_own - slopes * dist.astype(F32), -jnp.inf)
        sc = jnp.concatenate([s_sel.reshape(bsz, nh, MOBA_QCHUNK, topk * MOBA_BLOCK), s_own], axis=-1)
        pr = jax.nn.softmax(sc, axis=-1).astype(v.dtype)
        p_sel = pr[..., :topk * MOBA_BLOCK].reshape(bsz, nh, MOBA_QCHUNK, topk, MOBA_BLOCK)
        p_own = pr[..., topk * MOBA_BLOCK:]
        return (jnp.einsum('bhqkl,bhqkld->bhqd', p_sel, vg)
                + jnp.einsum('bhql,bhld->bhqd', p_own, vo))

    out = lax.map(chunk, jnp.arange(n_chunks))
    out = out.transpose(1, 2, 0, 3, 4).reshape(bsz, nh, s_pad, hd)[:, :, :s]
    return out.transpose(0, 2, 1, 3)


def causal_depthwise_conv(u, w, b):
    c = u.shape[-1]
    out = lax.conv_general_dilated(u, w[:, None, :].astype(u.dtype), window_strides=(1,),
                                   padding=[(SSD_CONV - 1, 0)],
                                   dimension_numbers=('NWC', 'WIO', 'NWC'),
                                   feature_group_count=c)
    return out + b


def segsum(a):
    tlen = a.shape[-1]
    cs = jnp.cumsum(a, axis=-1)
    diff = cs[..., :, None] - cs[..., None, :]
    mask = jnp.tril(jnp.ones((tlen, tlen), dtype=bool))
    return jnp.where(mask, diff, -jnp.inf)


def ssd_chunked_scan(x, dt, a, b_in, c_in):
    bsz, s, nh, hp = x.shape
    ng, ns = b_in.shape[2], b_in.shape[3]
    hpg = nh // ng
    nc = -(-s // SSD_CHUNK)
    sp = nc * SSD_CHUNK

    def padt(t):
        return jnp.pad(t.astype(F32), [(0, 0), (0, sp - s)] + [(0, 0)] * (t.ndim - 2))

    x, dt, b_in, c_in = padt(x), padt(dt), padt(b_in), padt(c_in)
    xdt = (x * dt[..., None]).reshape(bsz, nc, SSD_CHUNK, ng, hpg, hp)
    da = (dt * a.astype(F32)).reshape(bsz, nc, SSD_CHUNK, ng, hpg).transpose(0, 3, 4, 1, 2)
    bc = b_in.reshape(bsz, nc, SSD_CHUNK, ng, ns)
    cc = c_in.reshape(bsz, nc, SSD_CHUNK, ng, ns)
    da_cs = jnp.cumsum(da, axis=-1)
    decay_in = jnp.exp(segsum(da))
    cb = jnp.einsum('bclgn,bcsgn->bgcls', cc, bc)
    y_diag = jnp.einsum('bgcls,bgjcls,bcsgjp->bclgjp', cb, decay_in, xdt)
    decay_to_end = jnp.exp(da_cs[..., -1:] - da_cs)
    chunk_states = jnp.einsum('bclgn,bgjcl,bclgjp->cbgjpn', bc, decay_to_end, xdt)
    chunk_decay = jnp.exp(da_cs[..., -1]).transpose(3, 0, 1, 2)

    def carry_state(h, inp):
        st, dec = inp
        return h * dec[..., None, None] + st, h

    h0 = jnp.zeros(chunk_states.shape[1:], F32)
    _, h_in = lax.scan(carry_state, h0, (chunk_states, chunk_decay))
    y_off = jnp.einsum('bclgn,cbgjpn,bgjcl->bclgjp', cc, h_in, jnp.exp(da_cs))
    return (y_diag + y_off).reshape(bsz, sp, nh, hp)[:, :s]


def ssd_mixer(z, xbc, dt_raw, conv_w, conv_b, dt_bias, a_log, d_skip, norm_g):
    bsz, s, _ = z.shape
    xbc = jax.nn.silu(causal_depthwise_conv(xbc, conv_w, conv_b))
    xs = xbc[..., :SSD_D_INNER].reshape(bsz, s, SSD_HEADS, SSD_HEAD_DIM)
    gs = SSD_GROUPS * SSD_D_STATE
    b_in = xbc[..., SSD_D_INNER:SSD_D_INNER + gs].reshape(bsz, s, SSD_GROUPS, SSD_D_STATE)
    c_in = xbc[..., SSD_D_INNER + gs:].reshape(bsz, s, SSD_GROUPS, SSD_D_STATE)
    dt = jax.nn.softplus(dt_raw.astype(F32) + dt_bias.astype(F32))
    a = -jnp.exp(a_log.astype(F32))
    y = ssd_chunked_scan(xs, dt, a, b_in, c_in)
    y = y + d_skip.astype(F32)[:, None] * xs.astype(F32)
    u = (y.reshape(bsz, s, SSD_D_INNER) * jax.nn.silu(z.astype(F32)))
    u = u.reshape(bsz, s, SSD_GROUPS, SSD_D_INNER // SSD_GROUPS)
    u = u * lax.rsqrt(jnp.mean(u * u, axis=-1, keepdims=True) + EPS)
    return (u.reshape(bsz, s, SSD_D_INNER) * norm_g.astype(F32)).astype(z.dtype)


def hier_moe(h, w_rg, b_rg, w_re, b_re, w_gate, w_up, w_down):
    bsz, s, _ = h.shape
    hf = h.astype(F32)
    g_logits = hf @ w_rg.astype(F32) + b_rg.astype(F32)
    g_prob = jax.nn.softmax(g_logits, axis=-1)
    g_pw, g_sel = lax.top_k(g_prob, 1)
    e_logits = (hf @ w_re.astype(F32) + b_re.astype(F32)).reshape(bsz, s, MOE_GROUPS, MOE_EXPERTS_PER_GROUP)
    e_in_group = jnp.take_along_axis(e_logits, g_sel[..., None], axis=2)[:, :, 0]
    e_val, e_idx = lax.top_k(e_in_group, MOE_TOPK)
    e_w = jax.nn.softmax(e_val, axis=-1) * g_pw
    expert_id = g_sel * MOE_EXPERTS_PER_GROUP + e_idx
    combine = jnp.sum(jax.nn.one_hot(expert_id, MOE_EXPERTS, dtype=F32) * e_w[..., None], axis=-2)
    out = jnp.zeros_like(h)
    for e in range(MOE_EXPERTS):
        he = jax.nn.silu(h @ w_gate[e]) * (h @ w_up[e])
        out = out + combine[..., e:e + 1].astype(h.dtype) * (he @ w_down[e])
    return out


def setup_inputs(seed: int = 0) -> dict:
    key = jax.random.key(seed)
    ks = jax.random.split(key, 24)
    nrm = jax.random.normal

    def gain(k, n):
        return 1.0 + 0.02 * nrm(k, (DEPTH, n), F32)

    dt0 = jnp.exp(jax.random.uniform(ks[6], (DEPTH, SSD_HEADS), F32)
                  * (math.log(0.1) - math.log(1e-3)) + math.log(1e-3))
    return {
        'x': nrm(ks[0], (BATCH, SEQ, D_MODEL), F32),
        'p': nrm(ks[1], (DEPTH, BATCH, SEQ, PLE_DIM), F32),
        'mix_norm_g': gain(ks[2], D_MODEL),
        'w_in': nrm(ks[3], (DEPTH, D_MODEL, IN_DIM), F32) * D_MODEL ** -0.5,
        'conv_w': nrm(ks[4], (DEPTH, SSD_CONV, SSD_CONV_DIM), F32) * SSD_CONV ** -0.5,
        'conv_b': 0.02 * nrm(ks[5], (DEPTH, SSD_CONV_DIM), F32),
        'dt_bias': dt0 + jnp.log(-jnp.expm1(-dt0)),
        'a_log': jnp.log(jax.random.uniform(ks[7], (DEPTH, SSD_HEADS), F32, minval=1.0, maxval=16.0)),
        'd_skip': gain(ks[8], SSD_HEADS),
        'ssd_norm_g': gain(ks[9], SSD_D_INNER),
        'w_out_a': nrm(ks[10], (DEPTH, A_WIDTH, D_MODEL), F32) * A_WIDTH ** -0.5,
        'w_out_b': nrm(ks[11], (DEPTH, SSD_D_INNER, D_MODEL), F32) * SSD_D_INNER ** -0.5,
        'w_out': nrm(ks[12], (DEPTH, D_MODEL, D_MODEL), F32) * D_MODEL ** -0.5,
        'ffn_norm_g': gain(ks[13], D_MODEL),
        'w_rg': nrm(ks[14], (DEPTH, D_MODEL, MOE_GROUPS), F32) * D_MODEL ** -0.5,
        'b_rg': 0.01 * nrm(ks[15], (DEPTH, MOE_GROUPS), F32),
        'w_re': nrm(ks[16], (DEPTH, D_MODEL, MOE_EXPERTS), F32) * D_MODEL ** -0.5,
        'b_re': 0.01 * nrm(ks[17], (DEPTH, MOE_EXPERTS), F32),
        'w_gate': nrm(ks[18], (DEPTH, MOE_EXPERTS, D_MODEL, MOE_D_FF), F32) * D_MODEL ** -0.5,
        'w_up': nrm(ks[19], (DEPTH, MOE_EXPERTS, D_MODEL, MOE_D_FF), F32) * D_MODEL ** -0.5,
        'w_down': nrm(ks[20], (DEPTH, MOE_EXPERTS, MOE_D_FF, D_MODEL), F32) * MOE_D_FF ** -0.5,
        'w_ple_proj': nrm(ks[21], (DEPTH, PLE_DIM, D_MODEL), F32) * PLE_DIM ** -0.5,
        'w_ple_gate': nrm(ks[22], (DEPTH, D_MODEL, D_MODEL), F32) * D_MODEL ** -0.5,
        'final_norm_g': 1.0 + 0.02 * nrm(ks[23], (D_MODEL,), F32),
    }


def reference(x, p, mix_norm_g, w_in, conv_w, conv_b, dt_bias, a_log, d_skip, ssd_norm_g,
              w_out_a, w_out_b, w_out, ffn_norm_g, w_rg, b_rg, w_re, b_re, w_gate, w_up,
              w_down, w_ple_proj, w_ple_gate, final_norm_g):
    bsz, s, _ = x.shape
    for i in range(DEPTH):
        h = rms_norm(x, mix_norm_g[i])
        proj = h @ w_in[i]
        q = proj[..., OFF_Q:OFF_K].reshape(bsz, s, A_HEADS, A_HEAD_DIM)
        k = proj[..., OFF_K:OFF_V].reshape(bsz, s, A_HEADS, A_HEAD_DIM)
        v = proj[..., OFF_V:OFF_Z].reshape(bsz, s, A_HEADS, A_HEAD_DIM)
        z = proj[..., OFF_Z:OFF_XBC]
        xbc = proj[..., OFF_XBC:OFF_DT]
        dt_raw = proj[..., OFF_DT:OFF_GA]
        gate_a = jax.nn.sigmoid(proj[..., OFF_GA:OFF_GB])
        gate_b = jax.nn.sigmoid(proj[..., OFF_GB:IN_DIM])
        y_a = moba_attention(q, k, v).reshape(bsz, s, A_WIDTH) @ w_out_a[i]
        y_b = ssd_mixer(z, xbc, dt_raw, conv_w[i], conv_b[i], dt_bias[i], a_log[i],
                        d_skip[i], ssd_norm_g[i]) @ w_out_b[i]
        x = x + (gate_a * y_a + gate_b * y_b) @ w_out[i]
        h2 = rms_norm(x, ffn_norm_g[i])
        x = x + hier_moe(h2, w_rg[i], b_rg[i], w_re[i], b_re[i], w_gate[i], w_up[i], w_down[i])
        x = x + jax.nn.sigmoid(x @ w_ple_gate[i]) * (p[i] @ w_ple_proj[i])
    return rms_norm(x, final_norm_g)
```

```python
import numpy as np
import ml_dtypes
from contextlib import ExitStack
import concourse.bass as bass
import concourse.mybir as mybir
from concourse.bass_utils import run_bass_kernel_spmd

F32 = mybir.dt.float32
BF16 = mybir.dt.bfloat16
U8 = mybir.dt.uint8
I32 = mybir.dt.int32
AF = mybir.ActivationFunctionType
ALU = mybir.AluOpType
AX = mybir.AxisListType

S = 2048
D = 1024
NT = 16
EPS = 1e-6
IN_DIM = 6160
OFF_Q, OFF_K, OFF_V, OFF_Z, OFF_XBC, OFF_DT, OFF_GA, OFF_GB = 0, 512, 1024, 1536, 2560, 4096, 4112, 5136
NEG = -240000.0

ENGS = ("pe", "act", "dve", "pool", "sp")
import os as _os0
STRICT_SAME_ENGINE = _os0.environ.get("KSTRICT", "1") == "1"
DSIZE = {F32: 4, BF16: 2, U8: 1, I32: 4}


class V:
    __slots__ = ("ap", "keys")

    def __init__(self, ap, keys):
        self.ap = ap
        self.keys = tuple(keys)


class Tile:
    def __init__(self, name, ap, subs=None):
        self.name = name
        self.ap = ap
        self.subs = subs

    def v(self, idx=None, sub=None):
        ap = self.ap if idx is None else self.ap[idx]
        if self.subs is None:
            keys = (self.name,)
        elif sub is None:
            keys = tuple((self.name, s) for s in self.subs)
        elif isinstance(sub, (list, tuple, range)):
            keys = tuple((self.name, s) for s in sub)
        else:
            keys = ((self.name, sub),)
        return V(ap, keys)


class Sched:
    NDMA = 16

    def __init__(self):
        self.ins = []
        self.last_w = {}
        self.readers = {}
        self.out_dmas = []
        self.bounds = []
        self.region = 0
        self.flag = None

    def op(self, eng, fn, reads=(), writes=(), dma=False, prefetch=False, cost=0.3, lat=0.0):
        i = len(self.ins)
        deps = {}
        for k in reads:
            w = self.last_w.get(k)
            if w is not None:
                deps[w] = True
        for k in writes:
            w = self.last_w.get(k)
            if w is not None:
                deps.setdefault(w, False)
            for r in self.readers.get(k, ()):
                deps.setdefault(r, False)
        rec = dict(eng=eng, fn=fn, deps=deps, dma=dma, prefetch=prefetch, cost=cost, lat=lat, region=self.region)
        rec['wk'] = tuple(writes)
        rec['rk'] = tuple(reads)
        self.ins.append(rec)
        for k in reads:
            self.readers.setdefault(k, []).append(i)
        for k in writes:
            self.last_w[k] = i
            self.readers[k] = []
        return i

    def barrier(self):
        if not self.bounds or self.bounds[-1] != len(self.ins):
            self.bounds.append(len(self.ins))

    def begin_branch(self, flag_writer, flag_ap):
        self.barrier()
        self.flag = (flag_writer, flag_ap)
        self._snap = (dict(self.last_w), {k: list(v) for k, v in self.readers.items()})
        self.region = 1

    def begin_else(self):
        self.barrier()
        self.last_w = dict(self._snap[0])
        self.readers = {k: list(v) for k, v in self._snap[1].items()}
        self.region = 2

    def end_branch(self):
        self.barrier()
        self.last_w = {}
        self.readers = {}
        self.region = 3

    def _schedule(self, ids, window=128, sync=0.2):
        ins = self.ins
        idset = set(ids)
        users = {i: [] for i in ids}
        nun = {}
        for i in ids:
            n = 0
            for d in ins[i]["deps"]:
                if d in idset:
                    users[d].append(i)
                    n += 1
            nun[i] = n
        pend = {e: [i for i in ids if ins[i]["eng"] == e] for e in ENGS}
        head = {e: 0 for e in ENGS}
        done = set()
        finish = {}
        ready = {}
        free = {e: 0.0 for e in ENGS}
        order = {e: [] for e in ENGS}

        def rtime(i):
            t = 0.0
            e = ins[i]["eng"]
            for d, raw in ins[i]["deps"].items():
                if d in finish:
                    f = finish[d]
                    if ins[d]["eng"] != e or ins[d]["dma"] or raw:
                        f += sync
                    if f > t:
                        t = f
            return t

        for i in ids:
            if nun[i] == 0:
                ready[i] = rtime(i)
        left = len(ids)
        while left:
            best = None
            for e in ENGS:
                lst = pend[e]
                h = head[e]
                while h < len(lst) and lst[h] in done:
                    h += 1
                head[e] = h
                cnt = 0
                j = h
                while j < len(lst) and cnt < window:
                    i = lst[j]
                    j += 1
                    if i in done:
                        continue
                    cnt += 1
                    if i in ready:
                        r = ready[i]
                        if r < free[e]:
                            r = free[e]
                        if best is None or (r, i) < best[0]:
                            best = ((r, i), e)
            (r, i), e = best
            rec = ins[i]
            if rec["dma"]:
                free[e] = r + rec["cost"]
                finish[i] = r + rec["cost"] + rec["lat"]
            else:
                free[e] = r + rec["cost"]
                finish[i] = free[e]
            done.add(i)
            order[e].append(i)
            left -= 1
            for u in users[i]:
                nun[u] -= 1
                if nun[u] == 0:
                    ready[u] = rtime(u)
        return order, max(finish.values()) if finish else 0.0

    def emit(self, block, sems, dma_sems, reorder=True, regs=None):
        ins = self.ins
        N = self.NDMA
        bounds = [b for b in self.bounds if 0 < b < len(ins)] + [len(ins)]
        segs = {0: [], 1: [], 2: [], 3: []}
        lo = 0
        self.est = []
        for b in bounds:
            ids = list(range(lo, b))
            lo = b
            if not ids:
                continue
            reg = ins[ids[0]]["region"]
            assert all(ins[i]["region"] == reg for i in ids)
            if reorder:
                order, t = self._schedule(ids)
                self.est.append((reg, round(t)))
            else:
                order = {e: [i for i in ids if ins[i]["eng"] == e] for e in ENGS}
            segs[reg].append((ids, order))
        has_branch = bool(segs[1] or segs[2])
        extra = {}
        force_signal = set()

        def chain(seglist, prev):
            for ids, order in seglist:
                if prev is not None:
                    pl, pd = prev
                    for e in ENGS:
                        if order[e]:
                            ex = extra.setdefault(order[e][0], {})
                            for d in pl + pd:
                                ex[d] = True
                last = []
                for e in ENGS:
                    for i in reversed(order[e]):
                        if not ins[i]["dma"]:
                            last.append(i)
                            break
                if prev is not None:
                    have = {ins[i]["eng"] for i in last}
                    last += [i for i in prev[0] if ins[i]["eng"] not in have]
                prev = (last, [i for i in ids if ins[i]["dma"] and not ins[i]["prefetch"]])
            return prev
        tail0 = chain(segs[0], None)
        if has_branch:
            chain(segs[1], tail0)
            chain(segs[2], tail0)
            chain(segs[3], None)
        rstream = {r: {e: [i for ids, order in segs[r] for i in order[e]] for e in ENGS} for r in range(4)}
        if has_branch:
            for r in (1, 2):
                for e in ENGS:
                    for i in reversed(rstream[r][e]):
                        if not ins[i]["dma"]:
                            force_signal.add(i)
                            break
        dma_j = {}
        nd = {}
        for q in ("sp", "pool"):
            l0 = [i for i in rstream[0][q] if ins[i]["dma"]]
            for j, i in enumerate(l0):
                dma_j[i] = j
                if j >= N:
                    extra.setdefault(i, {})[l0[j - N]] = True
            cnt = [len(l0)]
            for r in (1, 2):
                lr = l0 + [i for i in rstream[r][q] if ins[i]["dma"]]
                for j in range(len(l0), len(lr)):
                    dma_j[lr[j]] = j
                    if j >= N:
                        extra.setdefault(lr[j], {})[lr[j - N]] = True
                cnt.append(len(lr))
            J = (max(cnt) + N - 1) // N * N
            l3 = [i for i in rstream[3][q] if ins[i]["dma"]]
            for n, i in enumerate(l3):
                dma_j[i] = J + n
                if n >= N:
                    extra.setdefault(i, {})[l3[n - N]] = True
            nd[q] = (cnt, J)
        pos = {}
        for e in ENGS:
            n0 = len(rstream[0][e])
            for n, i in enumerate(rstream[0][e]):
                pos[i] = n
            for r in (1, 2):
                for n, i in enumerate(rstream[r][e]):
                    pos[i] = n0 + n
            for n, i in enumerate(rstream[3][e]):
                pos[i] = 10 ** 7 + n
        signal = set(force_signal)
        waits = {}
        if self.flag is not None:
            signal.add(self.flag[0])

        def prune(e, stream, known, known_dma, drop_old=False):
            for i in stream:
                r = ins[i]
                final = []
                best = {}
                alld = dict(r["deps"])
                for d, raw in extra.get(i, {}).items():
                    alld[d] = alld.get(d, False) or raw
                for d, raw in alld.items():
                    rd = ins[d]
                    if drop_old and rd["region"] != 3:
                        continue
                    if rd["dma"]:
                        if d not in known_dma:
                            known_dma.add(d)
                            final.append(d)
                        continue
                    e2 = rd["eng"]
                    if e2 == e and not r["dma"] and (e == "pe" or (not raw and not STRICT_SAME_ENGINE)):
                        continue
                    if known[e2] >= pos[d]:
                        continue
                    if e2 not in best or pos[d] > pos[best[e2]]:
                        best[e2] = d
                for e2, d in best.items():
                    known[e2] = pos[d]
                    final.append(d)
                    signal.add(d)
                waits[i] = final
        for e in ENGS:
            known = {x: -1 for x in ENGS}
            kd = set()
            prune(e, rstream[0][e], known, kd)
            for r in (1, 2):
                prune(e, rstream[r][e], dict(known), set(kd))
            prune(e, rstream[3][e], {x: -1 for x in ENGS}, set(), drop_old=has_branch)
        ev = {}
        cnt_e = {}
        for e in ENGS:
            c = 0
            cs = {}
            for i in rstream[0][e]:
                if not ins[i]["dma"] and i in signal:
                    c += 1
                    ev[i] = (sems[e], c)
            cs[0] = c
            for r in (1, 2):
                c = cs[0]
                for i in rstream[r][e]:
                    if not ins[i]["dma"] and i in signal:
                        c += 1
                        ev[i] = (sems[e], c)
                cs[r] = c
            c = max(cs[1], cs[2])
            cs["t"] = c
            for i in rstream[3][e]:
                if not ins[i]["dma"] and i in signal:
                    c += 1
                    ev[i] = (sems[e], c)
            cnt_e[e] = cs
        for i, j in dma_j.items():
            ev[i] = (dma_sems[ins[i]["eng"]][j % N], 16 * (j // N + 1))
        self.stats = {e: (sum(len(rstream[r][e]) for r in range(4)), cnt_e[e]) for e in ENGS}
        self.nwaits = sum(len(v) for v in waits.values())
        self.first_sig = {e: [(i, ins[i]['wk'], ins[i]['rk']) for i in rstream[0][e] if i in ev and not ins[i]['dma']][:3] for e in ENGS}
        final_waits = [ev[i] for i in self.out_dmas]
        join_waits = []
        if has_branch:
            for e in ENGS:
                if e != "sp":
                    join_waits.append((sems[e], cnt_e[e]["t"]))
            for q in ("sp", "pool"):
                cnt, J = nd[q]
                if J:
                    for s_ in range(N):
                        join_waits.append((dma_sems[q][s_], 16 * (J // N)))

        def run_stream(e, handle, stream):
            for i in stream:
                r = ins[i]
                for d in waits[i]:
                    s, v = ev[d]
                    handle.wait_ge(s, v)
                bi = r["fn"](handle)
                if r["dma"]:
                    bi.then_inc(ev[i][0], 16)
                elif i in signal:
                    bi.then_inc(sems[e], 1)

        def pads(e, handle, r):
            if e != "sp":
                cs = cnt_e[e]
                if cs[r] > cs[0]:
                    handle.wait_ge(sems[e], cs[r])
                if cs["t"] > cs[r]:
                    handle.sem_inc(sems[e], cs["t"] - cs[r])
            if e in nd:
                cnt, J = nd[e]
                n = cnt[r]
                for s_ in range(N):
                    real = 16 * len([j for j in range(n) if j % N == s_])
                    if real:
                        handle.wait_ge(dma_sems[e][s_], real)
                    if 16 * (J // N) > real:
                        handle.sem_inc(dma_sems[e][s_], 16 * (J // N) - real)

        def run(e, handle):
            run_stream(e, handle, rstream[0][e])
            if has_branch:
                fw, fap = self.flag
                s, v = ev[fw]
                handle.wait_ge(s, v)
                handle.reg_load(regs[e], fap)
                import os as _os
                with handle.If_eq(regs[e], int(_os.environ.get('KFLAGCMP', '0'))):
                    run_stream(e, handle, rstream[1][e])
                    pads(e, handle, 1)
                with handle.Else():
                    run_stream(e, handle, rstream[2][e])
                    pads(e, handle, 2)
                for s, v in join_waits:
                    handle.wait_ge(s, v)
                run_stream(e, handle, rstream[3][e])
            if e == "sp":
                for s, v in final_waits:
                    handle.wait_ge(s, v)

        block.tensor(lambda h: run("pe", h))
        block.scalar(lambda h: run("act", h))
        block.vector(lambda h: run("dve", h))
        block.gpsimd(lambda h: run("pool", h))
        block.sync(lambda h: run("sp", h))


class Builder:
    def __init__(self, nc, arena_bytes):
        self.nc = nc
        self.s = Sched()
        self.arena_bytes = arena_bytes
        self.top = 0
        self.uid = 0
        self.ps_i = 0

    def setup(self, stack):
        nc = self.nc
        self.arena = stack.enter_context(nc.sbuf_tensor("arena", [128, self.arena_bytes], U8))
        self.psum = [stack.enter_context(nc.psum_tensor("ps%d" % i, [128, 512], F32)) for i in range(8)]
        self.psum_t = [Tile("ps%d" % i, self.psum[i][:, :]) for i in range(8)]
        self.psum_bf = [Tile("ps%d" % i, self.psum[i].bitcast(BF16)[:, :]) for i in range(8)]

    def alloc(self, name, shape, dtype, subs=None, parts=128):
        n = int(np.prod(shape)) * DSIZE[dtype]
        n = (n + 31) // 32 * 32
        off = self.top
        self.top += n
        assert self.top <= self.arena_bytes, (name, self.top)
        self.maxtop = max(getattr(self, "maxtop", 0), self.top)
        ap = self.arena[0:parts, off:off + int(np.prod(shape)) * DSIZE[dtype]].bitcast(dtype)
        if len(shape) == 2:
            ap = ap.rearrange("p (a b) -> p a b", a=shape[0])
        elif len(shape) == 3:
            ap = ap.rearrange("p (a b c) -> p a b c", a=shape[0], b=shape[1])
        elif len(shape) == 4:
            ap = ap.rearrange("p (a b c d) -> p a b c d", a=shape[0], b=shape[1], c=shape[2])
        self.uid += 1
        return Tile("%s#%d" % (name, self.uid), ap, subs)

    def alloc_at(self, name, off, shape, dtype, subs=None):
        top = self.top
        self.top = off
        t = self.alloc(name, shape, dtype, subs)
        self.top = top
        return t

    def phase(self, base):
        self.s.barrier()
        self.top = base

    def mark(self):
        return self.top

    def release(self, mark):
        self.s.barrier()
        self.top = mark

    ps_rot = list(range(8))

    def ps(self, bf=False):
        self.ps_i = (self.ps_i + 1) % len(self.ps_rot)
        i = self.ps_rot[self.ps_i]
        return (self.psum_bf if bf else self.psum_t)[i]

    def ps_fixed(self, i, bf=False):
        return (self.psum_bf if bf else self.psum_t)[i]

    def dma(self, out, in_, eng="sp", out_keys=(), in_keys=(), prefetch=False, is_out=False):
        oa = out.ap if isinstance(out, V) else out
        ia = in_.ap if isinstance(in_, V) else in_
        ok = out.keys if isinstance(out, V) else tuple(out_keys)
        ik = in_.keys if isinstance(in_, V) else tuple(in_keys)
        try:
            nb = oa.partition_size() * oa.free_size() * DSIZE[oa.dtype]
        except Exception:
            nb = 512 * 1024
        i = self.s.op(eng, lambda h: h.dma_start(out=oa, in_=ia), reads=ik, writes=ok, dma=True,
                      prefetch=prefetch, cost=(0.2 if eng == "sp" else 1.2),
                      lat=2.5 + nb / (60e3 if eng == "pool" else 150e3))
        if is_out:
            self.s.out_dmas.append(i)
        return i

    def mm(self, out, lhsT, rhs, start=True, stop=True):
        n = rhs.ap.free_size()
        c = max(64, n) / 2400.0 * (4 if rhs.ap.dtype == F32 else 1) + 0.03
        self.s.op("pe", lambda h: h.matmul(out.ap, lhsT.ap, rhs.ap, start=start, stop=stop),
                  reads=lhsT.keys + rhs.keys, writes=out.keys, cost=c)

    def transpose(self, out, in_, ident):
        self.s.op("pe", lambda h: h.transpose(out.ap, in_.ap, ident.ap),
                  reads=in_.keys + ident.keys, writes=out.keys, cost=0.1)

    def act(self, out, in_, func, bias=None, scale=1.0, accum=None, eng="act"):
        reads = in_.keys
        kw = {}
        if isinstance(bias, V):
            reads = reads + bias.keys
            kw["bias"] = bias.ap
        elif bias is not None:
            kw["bias"] = bias
        if isinstance(scale, V):
            reads = reads + scale.keys
            kw["scale"] = scale.ap
        else:
            kw["scale"] = scale
        writes = out.keys
        if accum is not None:
            writes = writes + accum.keys
            kw["accum_out"] = accum.ap
        self.s.op(eng, lambda h: h.activation(out.ap, in_.ap, func, **kw), reads=reads, writes=writes,
                  cost=0.25 + in_.ap.free_size() / 1200.0)

    def tt(self, out, in0, in1, op, eng="dve"):
        self.s.op(eng, lambda h: h.tensor_tensor(out.ap, in0.ap, in1.ap, op),
                  reads=in0.keys + in1.keys, writes=out.keys, cost=self.vcost(eng, out))

    def ts(self, out, in0, s1, op0, s2=None, op1=None, eng="dve", accum=None):
        reads = in0.keys
        a1 = s1
        a2 = s2
        if isinstance(s1, V):
            reads = reads + s1.keys
            a1 = s1.ap
        if isinstance(s2, V):
            reads = reads + s2.keys
            a2 = s2.ap
        kw = {}
        writes = out.keys
        if op1 is not None:
            kw["op1"] = op1
        if accum is not None:
            kw["accum_out"] = accum.ap
            writes = writes + accum.keys
        self.s.op(eng, lambda h: h.tensor_scalar(out.ap, in0.ap, a1, a2, op0, **kw), reads=reads, writes=writes,
                  cost=self.vcost(eng, out))

    def stt(self, out, in0, scalar, in1, op0, op1, eng="dve"):
        reads = in0.keys + in1.keys
        sc = scalar
        if isinstance(scalar, V):
            reads = reads + scalar.keys
            sc = scalar.ap
        self.s.op(eng, lambda h: h.scalar_tensor_tensor(out.ap, in0.ap, sc, in1.ap, op0, op1),
                  reads=reads, writes=out.keys, cost=self.vcost(eng, out))

    def copy(self, out, in_, eng="dve"):
        if eng == "act":
            self.s.op("act", lambda h: h.copy(out.ap, in_.ap), reads=in_.keys, writes=out.keys,
                      cost=0.25 + in_.ap.free_size() / 1200.0)
        else:
            self.s.op(eng, lambda h: h.tensor_copy(out.ap, in_.ap), reads=in_.keys, writes=out.keys,
                      cost=self.vcost(eng, out))

    def reduce(self, out, in_, op, axis=AX.X, eng="dve"):
        self.s.op(eng, lambda h: h.tensor_reduce(out.ap, in_.ap, axis, op), reads=in_.keys, writes=out.keys,
                  cost=self.vcost(eng, in_))

    def recip(self, out, in_):
        self.s.op("dve", lambda h: h.reciprocal(out.ap, in_.ap), reads=in_.keys, writes=out.keys,
                  cost=self.vcost("dve", out))

    def memset(self, out, val, eng="pool"):
        self.s.op(eng, lambda h: h.memset(out.ap, val), reads=(), writes=out.keys, cost=self.vcost(eng, out))

    @staticmethod
    def vcost(eng, v):
        n = v.ap.free_size()
        return (0.1 + n / 960.0) if eng == "dve" else (0.2 + n / 500.0)


def bcast(ap, shape_steps):
    return bass.AP(ap.tensor, ap.offset, [list(ap.ap[0])] + [list(x) for x in shape_steps])


SL = slice(None)


def ts_(i, n=128):
    return slice(i * n, (i + 1) * n)


class Ctx:
    pass


def phase_A(B, C):
    B.phase(34 * 1024)
    xt = [B.alloc("xt%d" % i, [D], F32) for i in range(2)]
    xn = [B.alloc("xn%d" % i, [D], BF16) for i in range(2)]
    junk = B.alloc("junk", [D], BF16)
    ss = [B.alloc("ss%d" % i, [1], F32) for i in range(2)]
    rs = [B.alloc("rs%d" % i, [1], F32) for i in range(2)]
    for i in range(NT):
        b = i % 2
        B.dma(xt[b].v(), C.x[ts_(i), :])
        B.act(junk.v(), xt[b].v(), AF.Square, accum=ss[b].v())
        B.act(rs[b].v(), ss[b].v(), AF.Sqrt, bias=C.eps.v(), scale=1.0 / D)
        B.recip(rs[b].v(), rs[b].v())
        B.ts(xn[b].v(), xt[b].v(), rs[b].v(), ALU.mult)
        p = B.ps(bf=True)
        for c in range(8):
            B.transpose(p.v((SL, ts_(c))), xn[b].v((SL, ts_(c))), C.ident_b.v())
        pin = V(p.ap[:, 0:1024].rearrange("p (c t) -> p c t", c=8), p.v().keys)
        gb = V(bcast(C.g1.ap, [[1, 8], [0, 128]]), C.g1.v().keys)
        B.tt(C.hT.v((SL, SL, ts_(i)), sub=i), pin, gb, ALU.mult)


def phase_B(B, C):
    hT = C.hT
    B.top = 50 * 1024
    wqkv = B.alloc("wqkv", [8, 1536], BF16, subs=[0, 1, 2])
    for j in range(3):
        B.dma(wqkv.v((SL, SL, ts_(j, 512)), sub=j),
              C.w_in[:, j * 512:(j + 1) * 512].rearrange("(k p) n -> p k n", p=128), eng="pool")
    qa = [B.alloc("qa%d" % h, [S], BF16, subs=["d", "s", "m"]) for h in range(8)]
    ka = [B.alloc("ka%d" % h, [S], BF16, subs=["d", "s"]) for h in range(8)]
    va_e = B.alloc("va_e", [NT, 4, 65], BF16, subs=list(range(NT)) + ["one"])
    va_o = B.alloc("va_o", [NT, 4, 128], BF16, subs=list(range(NT)) + ["one"])
    ksum = B.alloc("ksum", [8, 8], F32, subs=list(range(8)))
    KM = B.alloc("KM", [8, 8], BF16, subs=list(range(8)))
    MBT = B.alloc("MBT", [S], BF16, subs=list(range(NT)))
    tri = B.alloc("tri", [128], F32)
    ones_f = B.alloc("ones_f", [128], F32)
    B.dma(tri.v(), C.dram["tri_kq"])
    B.memset(ones_f.v(), 1.0)
    B.memset(va_e.v((SL, SL, SL, slice(64, 65)), sub="one"), 1.0)
    B.memset(va_o.v((SL, SL, SL, slice(0, 64)), sub="one"), 0.0)
    B.memset(va_o.v((SL, SL, SL, slice(0, 1)), sub="one"), 1.0)

    def dpart(h):
        return slice(0, 64) if h % 2 == 0 else slice(64, 128)

    def kpart(h):
        return slice(0, 76) if h % 2 == 0 else slice(0, 128)
    for h in range(8):
        a0 = 64 if h % 2 == 0 else 0
        if h % 2 == 1:
            B.memset(qa[h].v((slice(0, 64), SL), sub=["s", "m"]), 0.0)
            B.memset(ka[h].v((slice(0, 64), SL), sub="s"), 0.0)
        B.dma(qa[h].v((slice(a0, a0 + 4), SL), sub="s"), C.dram["qaug_c"][h], eng="pool")
        B.dma(ka[h].v((slice(a0, a0 + 12), SL), sub="s"), C.dram["kaug_c"], eng="pool")

    for hp in range(4):
        for tc in range(4):
            tl = range(4 * tc, 4 * tc + 4)
            p = B.ps()
            for k in range(8):
                B.mm(p.v(), wqkv.v((SL, k, ts_(hp)), sub=0), hT.v((SL, k, ts_(tc, 512)), sub=tl),
                     start=(k == 0), stop=(k == 7))
            for par in range(2):
                h = 2 * hp + par
                B.copy(qa[h].v((dpart(h), ts_(tc, 512)), sub="d"), p.v((dpart(h), SL)), eng=("act" if par == 0 else "dve"))
            p = B.ps()
            for k in range(8):
                B.mm(p.v(), wqkv.v((SL, k, slice(512 + hp * 128, 512 + hp * 128 + 128)), sub=1),
                     hT.v((SL, k, ts_(tc, 512)), sub=tl), start=(k == 0), stop=(k == 7))
            for par in range(2):
                h = 2 * hp + par
                for bb in range(2):
                    B.act(ka[h].v((dpart(h), slice(tc * 512 + bb * 256, tc * 512 + bb * 256 + 256)), sub="d"),
                          p.v((dpart(h), ts_(bb, 256))), AF.Copy,
                          accum=ksum.v((dpart(h), h, slice(2 * tc + bb, 2 * tc + bb + 1)), sub=h))
        for par in range(2):
            h = 2 * hp + par
            B.act(KM.v((dpart(h), h, SL), sub=h), ksum.v((dpart(h), h, SL), sub=h), AF.Copy, scale=1.0 / 256)

    if C.stop == 'B1':
        return
    for i in range(NT):
        p = B.ps()
        for k in range(8):
            B.mm(p.v(), hT.v((SL, k, ts_(i)), sub=i), wqkv.v((SL, k, slice(1024, 1536)), sub=2),
                 start=(k == 0), stop=(k == 7))
        pv4 = p.ap.rearrange("p (a b d) -> p a b d", a=4, b=2)
        B.copy(va_e.v((SL, i, SL, slice(0, 64)), sub=i), V(pv4[:, :, 0, :], p.v().keys), eng="dve")
        B.copy(va_o.v((SL, i, SL, slice(64, 128)), sub=i), V(pv4[:, :, 1, :], p.v().keys), eng="pool" if False else "dve")

    if C.stop == 'B2':
        return
    Gs = [B.alloc("Gs%d" % i, [64], F32) for i in range(2)]
    cmpt = [B.alloc("cmp%d" % i, [512], F32) for i in range(2)]
    rank = [B.alloc("rank%d" % i, [64], F32) for i in range(2)]
    mb = [B.alloc("mb%d" % i, [64], BF16) for i in range(2)]
    B.memset(MBT.v((slice(0, 64), slice(0, 256)), sub=[0, 1]), 0.0)
    for i in range(2, NT):
        b = i // 2
        u = i % 2
        gpe = B.ps()
        gpo = B.ps()
        for h in range(8):
            gp = gpe if h % 2 == 0 else gpo
            B.mm(gp.v((SL, slice((h // 2) * 8, (h // 2) * 8 + 8))), qa[h].v((dpart(h), ts_(i)), sub="d"),
                 KM.v((dpart(h), h, SL), sub=h))
        g4 = Gs[u].ap.rearrange("p (a b j) -> p a b j", a=4, b=2)
        B.copy(V(g4[:, :, 0, :], Gs[u].v().keys), V(gpe.ap[:, 0:32].rearrange("p (a j) -> p a j", a=4), gpe.v().keys), eng="act")
        B.copy(V(g4[:, :, 1, :], Gs[u].v().keys), V(gpo.ap[:, 0:32].rearrange("p (a j) -> p a j", a=4), gpo.v().keys), eng="act")
        gk = Gs[u].v().keys
        in0 = V(bcast(Gs[u].ap, [[8, 8], [0, b], [1, b]]), gk)
        in1 = V(bcast(Gs[u].ap, [[8, 8], [1, b], [0, b]]), gk)
        co = V(bcast(cmpt[u].ap, [[b * b, 8], [b, b], [1, b]]), cmpt[u].v().keys)
        B.tt(co, in0, in1, ALU.is_gt)
        ro = V(bcast(rank[u].ap, [[8, 8], [1, b]]), rank[u].v().keys)
        B.reduce(ro, co, ALU.add)
        B.memset(mb[u].v(), 0.0)
        mo = V(bcast(mb[u].ap, [[8, 8], [1, b]]), mb[u].v().keys)
        B.ts(mo, ro, 3.0, ALU.is_ge, NEG, ALU.mult)
        pt = B.ps(bf=True)
        B.transpose(pt.v((slice(0, 64), slice(0, 128))), mb[u].v(), C.ident_b.v())
        B.copy(MBT.v((slice(0, 64), ts_(i)), sub=i), pt.v((slice(0, 64), slice(0, 128))), eng="act")
    for h in range(8):
        a0 = 68 if h % 2 == 0 else 4
        B.dma(qa[h].v((slice(a0, a0 + 8), SL), sub="m"), MBT.v((slice(h * 8, h * 8 + 8), SL)))

    if C.stop == 'B3':
        return
    B.ps_rot = [0, 1, 2, 3, 4, 5]
    PT = [B.alloc("PT%d" % i, [512], BF16) for i in range(6)]
    tmp = [B.alloc("tmpd%d" % i, [128], F32) for i in range(3)]
    rden = [B.alloc("rden%d" % i, [512], F32) for i in range(2)]
    bcs = [B.alloc("bcs%d" % i, [512], F32) for i in range(2)]
    it = 0
    hq = 0
    for h in range(8):
        hp, par = h // 2, h % 2
        yp = dpart(h)
        dn = slice(64, 65) if par == 0 else slice(0, 1)
        op_ = slice(0, 65) if par == 0 else slice(0, 128)
        for Q in range(4):
            po = B.ps_fixed(6 + hq % 2)
            nk = 4 * (Q + 1)
            for kt in range(nk):
                m = kt - 4 * Q
                c0 = max(m, 0) * 128
                sp_ = B.ps()
                B.mm(sp_.v((SL, slice(c0, 512))), ka[h].v((kpart(h), ts_(kt))),
                     qa[h].v((kpart(h), slice(Q * 512 + c0, (Q + 1) * 512))))
                pt = PT[it % 6]
                if m >= 0:
                    t_ = tmp[it % 3]
                    B.tt(t_.v(), sp_.v((SL, slice(c0, c0 + 128))), tri.v(), ALU.add)
                    B.act(pt.v((SL, slice(c0, c0 + 128))), t_.v(), AF.Exp, scale=0.125)
                    if c0 + 128 < 512:
                        B.act(pt.v((SL, slice(c0 + 128, 512))), sp_.v((SL, slice(c0 + 128, 512))), AF.Exp, scale=0.125)
                else:
                    B.act(pt.v(), sp_.v(), AF.Exp, scale=0.125)
                vv = (va_e if par == 0 else va_o).v((SL, kt, hp, SL), sub=[kt, "one"])
                B.mm(po.v((op_, slice(c0, 512))), vv, pt.v((SL, slice(c0, 512))), start=(kt == 0), stop=(kt == nk - 1))
                it += 1
            rd = rden[hq % 2]
            B.recip(rd.v((dn, SL)), po.v((dn, SL)))
            pb = B.ps()
            if par == 0:
                B.mm(pb.v((slice(0, 64), SL)), ones_f.v((dn, slice(0, 64))), rd.v((dn, SL)))
            else:
                B.mm(pb.v(), ones_f.v((dn, SL)), rd.v((dn, SL)))
            bc = bcs[hq % 2]
            B.copy(bc.v((yp, SL)), pb.v((yp, SL)), eng="act")
            B.tt(C.yaT.v((yp, hp, ts_(Q, 512)), sub=h), po.v((yp, SL)), bc.v((yp, SL)), ALU.mult)
            hq += 1
    B.ps_rot = list(range(8))


def phase_C(B, C):
    hT = C.hT
    B.phase(82 * 1024)
    dr = C.dram
    wxbc = B.alloc("wxbc", [8, 1536], BF16, subs=list(range(6)))
    for j in range(6):
        B.dma(wxbc.v((SL, SL, ts_(j, 256)), sub=j),
              C.w_in[:, OFF_XBC + j * 256:OFF_XBC + (j + 1) * 256].rearrange("(k p) n -> p k n", p=128), eng="pool")
    wz = B.alloc("wz", [8, 1024], BF16, subs=[0, 1])
    for j in range(2):
        B.dma(wz.v((SL, SL, ts_(j, 512)), sub=j),
              C.w_in[:, OFF_Z + j * 512:OFF_Z + (j + 1) * 512].rearrange("(k p) n -> p k n", p=128), eng="pool")
    wdt = B.alloc("wdt", [8, 16], BF16)
    B.dma(wdt.v(), C.w_in[:, OFF_DT:OFF_DT + 16].rearrange("(k p) n -> p k n", p=128), eng="pool")
    Wt = B.alloc("Wt", [384], F32)
    W2 = B.alloc("W2", [384], F32)
    Esel = B.alloc("Esel", [16], F32)
    cw = B.alloc("cw", [12, 4], F32)
    cb = B.alloc("cb", [12], F32)
    dtb = B.alloc("dtb", [16], F32)
    A_bc = B.alloc("A_bc", [16], F32)
    dsk = B.alloc("dsk", [16], F32)
    ng = B.alloc("ng", [1024], F32)
    one_c = B.alloc("one_c", [1], F32)
    tri_b = B.alloc("tri_b", [128], BF16)
    B.dma(Wt.v(), dr["W_tri"])
    B.dma(W2.v(), dr["W_tri2"])
    B.dma(Esel.v((slice(0, 16), SL)), dr["Esel"])
    B.dma(cw.v(), dr["conv_w"])
    B.dma(cb.v(), dr["conv_b"])
    B.dma(dtb.v(), dr["dt_bias"])
    B.dma(A_bc.v(), dr["a_log"])
    B.dma(dsk.v(), dr["d_skip"])
    B.dma(ng.v(), dr["ssd_norm_g"])
    B.dma(tri_b.v(), dr["tri_kq"], eng="pool")
    B.memset(one_c.v(), 1.0)
    B.act(A_bc.v(), A_bc.v(), AF.Exp)
    B.ts(A_bc.v(), A_bc.v(), -1.0, ALU.mult)

    xr = [B.alloc("xr%d" % i, [259], F32) for i in range(3)]
    hist = B.alloc("hist", [12, 3], F32, subs=list(range(12)))
    acc = [B.alloc("acc%d" % i, [256], F32) for i in range(3)]
    xc = [B.alloc("xc%d" % i, [256], BF16) for i in range(3)]
    BT_l = [B.alloc("BT%d" % i, [2, 256], BF16, subs=[0, 1]) for i in range(2)]
    CT_l = [B.alloc("CT%d" % i, [2, 256], BF16, subs=[0, 1]) for i in range(2)]
    xs_tok_l = [B.alloc("xs_tok%d" % i, [2, 1024], BF16, subs=list(range(8))) for i in range(2)]
    B_tok_l = [B.alloc("B_tok%d" % i, [2, 2, 128], BF16, subs=[0, 1]) for i in range(2)]
    sz_l = [B.alloc("sz%d" % i, [2, 1024], BF16, subs=["%d%d" % (a, b) for a in range(2) for b in range(2)])
            for i in range(2)]
    xd_l = [B.alloc("xd%d" % i, [2, 16], F32) for i in range(2)]
    dt_l = [B.alloc("dt%d" % i, [2, 16], F32) for i in range(2)]
    da_l = [B.alloc("da%d" % i, [2, 16], F32) for i in range(2)]
    csT_l = [B.alloc("csT%d" % i, [256], F32) for i in range(2)]
    ncs_l = [B.alloc("ncs%d" % i, [2, 16], F32) for i in range(2)]
    ecs_l = [B.alloc("ecs%d" % i, [2, 16], F32) for i in range(2)]
    d2e_l = [B.alloc("d2e%d" % i, [2, 16], F32) for i in range(2)]
    dec_l = [B.alloc("dec%d" % i, [16], F32) for i in range(2)]
    xdt = B.alloc("xdt", [2, 1024], BF16)
    xdtd = B.alloc("xdtd", [2, 1024], BF16)
    CBT = B.alloc("CBT", [2, 384], F32, subs=[0, 1])
    LT = [B.alloc("LT%d" % i, [384], F32) for i in range(3)]
    MT = [B.alloc("MT%d" % i, [384], BF16) for i in range(4)]
    hst = B.alloc("hst", [1024], F32, subs=[0, 1])
    hsb = B.alloc("hsb", [1024], BF16, subs=[0, 1])
    t1_l = [B.alloc("t1_%d" % i, [1024], F32, subs=[0, 1]) for i in range(2)]
    u1_l = [B.alloc("u1_%d" % i, [1024], F32) for i in range(2)]
    junk = B.alloc("junkc", [512], BF16)
    ssq = B.alloc("ssq", [2], F32)
    rsd = B.alloc("rsd", [2], F32)
    ybk = B.alloc("ybk", [1024], BF16)

    B.ps_rot = [0, 1, 2, 3]
    B.memset(hist.v(), 0.0)
    itc = 0
    for c in range(8):
        T0 = 256 * c
        tiles = [2 * c, 2 * c + 1]
        BT, CT, xs_tok, B_tok, sz = BT_l[c % 2], CT_l[c % 2], xs_tok_l[c % 2], B_tok_l[c % 2], sz_l[c % 2]
        xd, dt, da, csT, ncs, ecs, d2e, dec = (xd_l[c % 2], dt_l[c % 2], da_l[c % 2], csT_l[c % 2], ncs_l[c % 2],
                                               ecs_l[c % 2], d2e_l[c % 2], dec_l[c % 2])
        for cc in range(12):
            p = B.ps()
            for k in range(8):
                B.mm(p.v((SL, slice(0, 256))), wxbc.v((SL, k, ts_(cc)), sub=cc // 2),
                     hT.v((SL, k, slice(T0, T0 + 256)), sub=tiles), start=(k == 0), stop=(k == 7))
            xr_ = xr[itc % 3]
            a_ = acc[itc % 3]
            x_ = xc[itc % 3]
            itc += 1
            B.copy(xr_.v((SL, slice(0, 3))), hist.v((SL, cc, SL), sub=cc), eng="pool")
            B.copy(xr_.v((SL, slice(3, 259))), p.v((SL, slice(0, 256))), eng="act")
            B.copy(hist.v((SL, cc, SL), sub=cc), xr_.v((SL, slice(256, 259))), eng="pool")
            B.ts(a_.v(), xr_.v((SL, slice(0, 256))), cw.v((SL, cc, slice(0, 1))), ALU.mult, 0.0, ALU.add,
                 eng="pool")
            for j in range(1, 4):
                B.stt(a_.v(), xr_.v((SL, slice(j, j + 256))), cw.v((SL, cc, slice(j, j + 1))), a_.v(),
                      ALU.mult, ALU.add)
            if cc < 8:
                B.act(x_.v(), a_.v(), AF.Silu, bias=cb.v((SL, slice(cc, cc + 1))))
                pt = B.ps(bf=True)
                for st in range(2):
                    B.transpose(pt.v((SL, ts_(st))), x_.v((SL, ts_(st))), C.ident_b.v())
                pin = V(pt.ap[:, 0:256].rearrange("p (s t) -> p s t", s=2), pt.v().keys)
                B.copy(xs_tok.v((SL, SL, ts_(cc)), sub=cc), pin, eng="dve")
            elif cc < 10:
                g = cc - 8
                B.act(BT.v((SL, g, SL), sub=g), a_.v(), AF.Silu, bias=cb.v((SL, slice(cc, cc + 1))))
                pt = B.ps(bf=True)
                for st in range(2):
                    B.transpose(pt.v((SL, ts_(st))), BT.v((SL, g, ts_(st)), sub=g), C.ident_b.v())
                pin = V(pt.ap[:, 0:256].rearrange("p (s t) -> p s t", s=2), pt.v().keys)
                B.copy(B_tok.v((SL, SL, g, SL), sub=g), pin, eng="dve")
            else:
                g = cc - 10
                B.act(CT.v((SL, g, SL), sub=g), a_.v(), AF.Silu, bias=cb.v((SL, slice(cc, cc + 1))))
        for st in range(2):
            for hf in range(2):
                p = B.ps()
                for k in range(8):
                    B.mm(p.v(), hT.v((SL, k, ts_(tiles[st])), sub=tiles[st]), wz.v((SL, k, ts_(hf, 512)), sub=hf),
                         start=(k == 0), stop=(k == 7))
                B.act(sz.v((SL, st, ts_(hf, 512)), sub="%d%d" % (st, hf)), p.v(), AF.Silu)
        for st in range(2):
            p = B.ps()
            for k in range(8):
                B.mm(p.v((SL, slice(0, 16))), hT.v((SL, k, ts_(tiles[st])), sub=tiles[st]), wdt.v((SL, k, SL)),
                     start=(k == 0), stop=(k == 7))
            B.tt(xd.v((SL, st, SL)), p.v((SL, slice(0, 16))), dtb.v(), ALU.add)
        B.act(xd.v(), xd.v(), AF.Exp)
        B.act(dt.v(), xd.v(), AF.Ln, bias=one_c.v())
        A2 = V(bcast(A_bc.ap, [[0, 2], [1, 16]]), A_bc.v().keys)
        B.tt(da.v(), dt.v(), A2, ALU.mult)
        pcs = B.ps()
        B.mm(pcs.v((slice(0, 16), slice(0, 256))), da.v((SL, 0, SL)), Wt.v((SL, slice(128, 384))), start=True, stop=False)
        B.mm(pcs.v((slice(0, 16), slice(0, 256))), da.v((SL, 1, SL)), Wt.v((SL, slice(0, 256))), start=False, stop=True)
        B.copy(csT.v((slice(0, 16), SL)), pcs.v((slice(0, 16), slice(0, 256))), eng="act")
        pct = B.ps()
        B.mm(pct.v((SL, slice(0, 16))), Wt.v((SL, slice(128, 256))), da.v((SL, 0, SL)))
        B.mm(pct.v((SL, slice(16, 32))), Wt.v((SL, slice(256, 384))), da.v((SL, 0, SL)), start=True, stop=False)
        B.mm(pct.v((SL, slice(16, 32))), Wt.v((SL, slice(128, 256))), da.v((SL, 1, SL)), start=False, stop=True)
        B.mm(pct.v((SL, slice(32, 48))), W2.v((SL, slice(128, 256))), da.v((SL, 0, SL)), start=True, stop=False)
        B.mm(pct.v((SL, slice(32, 48))), W2.v((SL, slice(0, 128))), da.v((SL, 1, SL)), start=False, stop=True)
        B.mm(pct.v((SL, slice(48, 64))), W2.v((SL, slice(128, 256))), da.v((SL, 1, SL)))
        B.mm(pct.v((SL, slice(64, 80))), Wt.v((SL, slice(256, 384))), da.v((SL, 0, SL)), start=True, stop=False)
        B.mm(pct.v((SL, slice(64, 80))), Wt.v((SL, slice(256, 384))), da.v((SL, 1, SL)), start=False, stop=True)
        B.act(ecs.v(), V(pct.ap[:, 0:32].rearrange("p (s h) -> p s h", s=2), pct.v().keys), AF.Exp)
        B.ts(ncs.v(), V(pct.ap[:, 0:32].rearrange("p (s h) -> p s h", s=2), pct.v().keys), -1.0, ALU.mult)
        B.act(d2e.v(), V(pct.ap[:, 32:64].rearrange("p (s h) -> p s h", s=2), pct.v().keys), AF.Exp)
        B.act(dec.v(), pct.v((SL, slice(64, 80))), AF.Exp)
        for st in range(2):
            xs3 = V(xs_tok.ap[:, st, :].rearrange("p (h d) -> p h d", h=16), xs_tok.v().keys)
            dtb3 = V(bcast(dt.ap[:, st, :], [[1, 16], [0, 64]]), dt.v().keys)
            xo3 = V(xdt.ap[:, st, :].rearrange("p (h d) -> p h d", h=16), xdt.v().keys)
            B.tt(xo3, xs3, dtb3, ALU.mult, eng="pool")
            if c < 7:
                d3 = V(bcast(d2e.ap[:, st, :], [[1, 16], [0, 64]]), d2e.v().keys)
                xo4 = V(xdtd.ap[:, st, :].rearrange("p (h d) -> p h d", h=16), xdtd.v().keys)
                B.tt(xo4, xo3, d3, ALU.mult, eng="pool")
        for g in range(2):
            p = B.ps()
            B.mm(p.v((SL, slice(0, 256))), BT.v((SL, g, slice(0, 128)), sub=g), CT.v((SL, g, SL), sub=g))
            B.mm(p.v((SL, slice(256, 384))), BT.v((SL, g, slice(128, 256)), sub=g), CT.v((SL, g, slice(128, 256)), sub=g))
            B.copy(CBT.v((SL, g, SL), sub=g), p.v((SL, slice(0, 384))), eng="dve")
        for h in range(16):
            g = h // 8
            pd = B.ps()
            eh = V(bcast(Esel.ap[0:16, h:h + 1], [[0, 128]]), Esel.v().keys)
            B.mm(pd.v((SL, slice(0, 256))), eh, csT.v((slice(0, 16), SL)), start=True, stop=False)
            B.mm(pd.v((SL, slice(0, 128))), C.ident_b.v(), tri_b.v(), start=False, stop=True)
            B.mm(pd.v((SL, slice(256, 384))), eh, csT.v((slice(0, 16), slice(128, 256))), start=True, stop=False)
            B.mm(pd.v((SL, slice(256, 384))), C.ident_b.v(), tri_b.v(), start=False, stop=True)
            lt_ = LT[h % 3]
            mt_ = MT[h % 4]
            B.act(lt_.v((SL, slice(0, 256))), pd.v((SL, slice(0, 256))), AF.Exp, bias=ncs.v((SL, 0, slice(h, h + 1))))
            B.act(lt_.v((SL, slice(256, 384))), pd.v((SL, slice(256, 384))), AF.Exp, bias=ncs.v((SL, 1, slice(h, h + 1))))
            B.tt(mt_.v(), lt_.v(), CBT.v((SL, g, SL), sub=g), ALU.mult)
            hc = slice(h * 64, h * 64 + 64)
            y0 = B.ps_fixed(4 + g)
            y1 = B.ps_fixed(6 + g)
            oc = slice((h % 8) * 64, (h % 8) * 64 + 64)
            B.mm(y0.v((SL, oc)), mt_.v((SL, slice(0, 128))), xdt.v((SL, 0, hc)))
            B.mm(y1.v((SL, oc)), mt_.v((SL, slice(128, 256))), xdt.v((SL, 0, hc)), start=True, stop=False)
            B.mm(y1.v((SL, oc)), mt_.v((SL, slice(256, 384))), xdt.v((SL, 1, hc)), start=False, stop=True)
        for lt in range(2):
            u_ = u1_l[lt]
            t1 = t1_l[lt]
            for g in range(2):
                yb_ = B.ps_fixed(4 + 2 * lt + g)
                hs = ts_(g, 512)
                if c > 0:
                    p = B.ps()
                    B.mm(p.v(), CT.v((SL, g, ts_(lt)), sub=g), hsb.v((SL, hs), sub=g))
                    e3 = V(bcast(ecs.ap[:, lt, g * 8:(g + 1) * 8], [[1, 8], [0, 64]]), ecs.v().keys)
                    p3 = V(p.ap.rearrange("p (h d) -> p h d", h=8), p.v().keys)
                    t13 = V(t1.ap[:, hs].rearrange("p (h d) -> p h d", h=8), t1.v(sub=g).keys)
                    B.tt(t13, p3, e3, ALU.mult)
                    B.tt(u_.v((SL, hs)), yb_.v(), t1.v((SL, hs), sub=g), ALU.add)
                else:
                    B.copy(u_.v((SL, hs)), yb_.v(), eng="dve")
            xs3b = V(xs_tok.ap[:, lt, :].rearrange("p (h d) -> p h d", h=16), xs_tok.v().keys)
            dk3 = V(bcast(dsk.ap, [[1, 16], [0, 64]]), dsk.v().keys)
            t1o = V(t1.ap.rearrange("p (h d) -> p h d", h=16), t1.v().keys)
            B.tt(t1o, xs3b, dk3, ALU.mult, eng="pool")
            B.tt(u_.v(), u_.v(), t1.v(), ALU.add, eng="pool")
            B.tt(u_.v(), u_.v(), sz.v((SL, lt, SL), sub=["%d0" % lt, "%d1" % lt]), ALU.mult)
            for g in range(2):
                B.act(junk.v(), u_.v((SL, ts_(g, 512))), AF.Square, accum=ssq.v((SL, slice(g, g + 1))))
            B.act(rsd.v(), ssq.v(), AF.Sqrt, bias=C.eps.v(), scale=1.0 / 512)
            B.recip(rsd.v(), rsd.v())
            for g in range(2):
                B.stt(ybk.v((SL, ts_(g, 512))), u_.v((SL, ts_(g, 512))), rsd.v((SL, slice(g, g + 1))),
                      ng.v((SL, ts_(g, 512))), ALU.mult, ALU.mult)
            pt = B.ps(bf=True)
            for k in range(8):
                B.transpose(pt.v((SL, ts_(k))), ybk.v((SL, ts_(k))), C.ident_b.v())
            pin = V(pt.ap[:, 0:1024].rearrange("p (c t) -> p c t", c=8), pt.v().keys)
            B.copy(C.ybT.v((SL, SL, ts_(tiles[lt])), sub=tiles[lt]), pin, eng="act")
        if c < 7:
            for g in range(2):
                hs = ts_(g, 512)
                p = B.ps()
                for lt in range(2):
                    B.mm(p.v(), B_tok.v((SL, lt, g, SL), sub=g), xdtd.v((SL, lt, hs)), start=(lt == 0), stop=(lt == 1))
                if c > 0:
                    dc3 = V(bcast(dec.ap[:, g * 8:(g + 1) * 8], [[1, 8], [0, 64]]), dec.v().keys)
                    h3 = V(hst.ap[:, hs].rearrange("p (h d) -> p h d", h=8), hst.v(sub=g).keys)
                    B.tt(h3, h3, dc3, ALU.mult)
                    B.tt(hst.v((SL, hs), sub=g), hst.v((SL, hs), sub=g), p.v(), ALU.add)
                else:
                    B.copy(hst.v((SL, hs), sub=g), p.v(), eng="dve")
                B.copy(hsb.v((SL, hs), sub=g), hst.v((SL, hs), sub=g), eng="pool")
    B.ps_rot = list(range(8))


def phase_D(B, C):
    hT, yaT, ybT, mT = C.hT, C.yaT, C.ybT, C.mT
    dr = C.dram
    B.phase(114 * 1024)
    woa = B.alloc("woa", [4, 1024], BF16, subs=[0, 1, 2, 3])
    wob = B.alloc("wob", [8, 1024], BF16, subs=[0, 1, 2, 3])
    wga = B.alloc("wga", [8, 1024], BF16, subs=[0, 1, 2, 3])
    wgb = B.alloc("wgb", [8, 1024], BF16, subs=[0, 1, 2, 3])
    for j in range(4):
        cs_ = slice(j * 256, (j + 1) * 256)
        B.dma(woa.v((SL, SL, cs_), sub=j), dr["w_out_a"][:, cs_].rearrange("(k p) n -> p k n", p=128), eng="pool")
        B.dma(wga.v((SL, SL, cs_), sub=j),
              C.w_in[:, OFF_GA + j * 256:OFF_GA + (j + 1) * 256].rearrange("(k p) n -> p k n", p=128), eng="pool")
        B.dma(wob.v((SL, SL, cs_), sub=j), dr["w_out_b"][:, cs_].rearrange("(k p) n -> p k n", p=128), eng="pool")
        B.dma(wgb.v((SL, SL, cs_), sub=j),
              C.w_in[:, OFF_GB + j * 256:OFF_GB + (j + 1) * 256].rearrange("(k p) n -> p k n", p=128), eng="pool")
    sga = B.alloc("sga", [512], F32)
    sgb = B.alloc("sgb", [512], F32)
    m1 = B.alloc("m1", [512], F32)
    m2 = B.alloc("m2", [512], F32)
    wo = B.alloc_at("wo", 180 * 1024, [8, 1024], BF16, subs=[0, 1])
    assert B.top <= 180 * 1024, B.top
    for j in range(2):
        cs_ = slice(j * 512, (j + 1) * 512)
        B.dma(wo.v((SL, SL, cs_), sub=j), dr["w_out"][:, cs_].rearrange("(k p) n -> p k n", p=128), eng="pool",
              prefetch=True)
    for cc in range(8):
        j = cc // 2
        for tc in range(4):
            tsl = ts_(tc, 512)
            tl = list(range(4 * tc, 4 * tc + 4))
            pga = B.ps()
            for k in range(8):
                B.mm(pga.v(), wga.v((SL, k, ts_(cc)), sub=j), hT.v((SL, k, tsl), sub=tl), start=(k == 0), stop=(k == 7))
            B.act(sga.v(), pga.v(), AF.Sigmoid)
            pa = B.ps()
            for pr in range(4):
                B.mm(pa.v(), woa.v((SL, pr, ts_(cc)), sub=j), yaT.v((SL, pr, tsl), sub=[2 * pr, 2 * pr + 1]),
                     start=(pr == 0), stop=(pr == 3))
            B.tt(m1.v(), pa.v(), sga.v(), ALU.mult)
            pgb = B.ps()
            for k in range(8):
                B.mm(pgb.v(), wgb.v((SL, k, ts_(cc)), sub=j), hT.v((SL, k, tsl), sub=tl), start=(k == 0), stop=(k == 7))
            B.act(sgb.v(), pgb.v(), AF.Sigmoid)
            pb = B.ps()
            for k in range(8):
                B.mm(pb.v(), wob.v((SL, k, ts_(cc)), sub=j), ybT.v((SL, k, tsl), sub=tl), start=(k == 0), stop=(k == 7))
            B.tt(m2.v(), pb.v(), sgb.v(), ALU.mult)
            B.tt(mT.v((SL, cc, tsl), sub=tl), m1.v(), m2.v(), ALU.add, eng="pool")
    B.phase(114 * 1024)
    x1 = C.x1
    xt = [B.alloc("xt%d" % i, [D], F32) for i in range(2)]
    for i in range(NT):
        b = i % 2
        B.dma(xt[b].v(), C.x[ts_(i), :])
        for hf in range(2):
            p = B.ps()
            for k in range(8):
                B.mm(p.v(), mT.v((SL, k, ts_(i)), sub=i), wo.v((SL, k, ts_(hf, 512)), sub=hf), start=(k == 0), stop=(k == 7))
            B.tt(x1.v((SL, i, ts_(hf, 512)), sub=i), p.v(), xt[b].v((SL, ts_(hf, 512))), ALU.add)


def phase_E(B, C):
    x1 = C.x1
    dr = C.dram
    K = 1024
    TG = 6
    CAP = 128 * TG
    H0 = 66 * K + 8 * K * TG
    PM0 = H0 + 32 * K
    SM0 = PM0 + 16 * K
    SC0 = SM0 + 4 * K
    B.phase(SC0)
    h2s = B.alloc_at("h2s", 66 * K, [8, 512 * TG], BF16, subs=list(range(4 * TG)))
    h2_tok = B.alloc_at("h2_tok", H0, [NT, D], BF16, subs=list(range(NT)))
    top0 = B.top
    B.top = SM0
    comb = B.alloc("comb", [NT * 16], F32)
    dest = B.alloc("dest", [NT], F32)
    destm = B.alloc("destm", [TG, NT], F32)
    cgx = B.alloc("cgx", [NT, 8], BF16)
    flag_i = B.alloc("flag_i", [1], I32)
    sel8 = B.alloc("sel8", [4], F32)
    sel8b = B.alloc("sel8b", [4], BF16)
    sid = B.alloc("sid", [4 * TG], F32)
    assert B.top <= SM0 + 2 * K, B.top
    B.top = top0
    B.dma(sel8.v((slice(0, 8), SL)), dr["sel8"])
    B.copy(sel8b.v((slice(0, 8), SL)), sel8.v((slice(0, 8), SL)), eng="dve")
    B.dma(sid.v(), dr["sid"])
    g2 = B.alloc("g2", [8], F32)
    B.dma(g2.v(), dr["ffn_norm_g"])
    g2bc = B.alloc("g2bc", [D], F32)
    B.dma(g2bc.v(), dr["ffn_norm_g_bc"])
    wr = B.alloc("wr", [8, 20], F32)
    B.dma(wr.v((SL, SL, slice(0, 4))), dr["w_rg"].rearrange("(k p) n -> p k n", p=128))
    B.dma(wr.v((SL, SL, slice(4, 20))), dr["w_re"].rearrange("(k p) n -> p k n", p=128))
    br = B.alloc("br", [20], F32)
    B.dma(br.v(), dr["b_r"])
    Wt = B.alloc("WtE", [384], F32)
    B.dma(Wt.v(), dr["W_tri"])
    gidx = B.alloc("gidx", [4], F32)
    B.dma(gidx.v(), dr["gidx4"])
    xn = [B.alloc("xnf%d" % i, [D], F32) for i in range(2)]
    h2f = [B.alloc("h2f%d" % i, [8, 128], F32) for i in range(2)]
    junk = B.alloc("junke", [D], BF16)
    sm = [B.alloc("rsm%d" % i, [2], F32) for i in range(2)]
    LG = B.alloc("LG", [NT, 20], F32, subs=list(range(NT)))
    for i in range(NT):
        b = i % 2
        ssv = sm[b].v((SL, slice(0, 1)))
        rsv = sm[b].v((SL, slice(1, 2)))
        B.act(junk.v(), x1.v((SL, i, SL), sub=i), AF.Square, accum=ssv)
        B.act(rsv, ssv, AF.Sqrt, bias=C.eps.v(), scale=1.0 / D)
        B.recip(rsv, rsv)
        B.ts(xn[b].v(), x1.v((SL, i, SL), sub=i), rsv, ALU.mult)
        B.tt(h2_tok.v((SL, i, SL), sub=i), xn[b].v(), g2bc.v(), ALU.mult, eng="pool")
        for hf in range(2):
            p = B.ps()
            for c4 in range(4):
                c = hf * 4 + c4
                B.transpose(p.v((SL, ts_(c4))), xn[b].v((SL, ts_(c))), C.ident_f.v())
            pin = V(p.ap.rearrange("p (c t) -> p c t", c=4), p.v().keys)
            gb = V(bcast(g2.ap[:, hf * 4:hf * 4 + 4], [[1, 4], [0, 128]]), g2.v().keys)
            B.tt(h2f[b].v((SL, slice(hf * 4, hf * 4 + 4), SL)), pin, gb, ALU.mult)
        pl = B.ps()
        for k in range(8):
            B.mm(pl.v((SL, slice(0, 20))), h2f[b].v((SL, k, SL)), wr.v((SL, k, SL)), start=(k == 0), stop=(k == 7))
        B.tt(LG.v((SL, i, SL), sub=i), pl.v((SL, slice(0, 20))), br.v(), ALU.add)

    def sm_(name, n):
        return B.alloc(name, [NT * n], F32)

    def b3(t, steps, off=0):
        a = t.ap
        return V(bass.AP(a.tensor, a.offset + off, [list(a.ap[0])] + [list(x) for x in steps]), t.v().keys)
    lgk = LG.v().keys
    gmax, gsh, ge, gsum, gpw, goh = sm_("gmax", 1), sm_("gsh", 4), sm_("ge", 4), sm_("gsum", 1), sm_("gpw", 1), sm_("goh", 4)
    tmpg, em, m1, oh1, em2, m2, oh2 = sm_("tmpg", 16), sm_("em", 4), sm_("m1", 1), sm_("oh1", 4), sm_("em2", 4), sm_("m2", 1), sm_("oh2", 4)
    dd, ee, w1, w2, cig, tm2 = sm_("dd", 1), sm_("ee", 1), sm_("w1", 1), sm_("w2", 1), sm_("cig", 4), sm_("tm2", 4)
    gl3 = V(bcast(LG.ap[:, 0, 0:1], [[20, NT], [1, 4]]), lgk)
    B.reduce(gmax.v(), gl3, ALU.max)
    B.tt(b3(gsh, [[4, NT], [1, 4]]), gl3, b3(gmax, [[1, NT], [0, 4]]), ALU.subtract)
    B.act(ge.v(), gsh.v(), AF.Exp)
    B.reduce(gsum.v(), b3(ge, [[4, NT], [1, 4]]), ALU.add)
    B.recip(gpw.v(), gsum.v())
    B.ts(goh.v(), gsh.v(), 0.0, ALU.is_ge)
    el3 = V(bcast(LG.ap[:, 0, 4:5], [[20, NT], [1, 4], [4, 4]]), lgk)
    B.tt(b3(tmpg, [[16, NT], [4, 4], [1, 4]]), el3, b3(goh, [[4, NT], [0, 4], [1, 4]]), ALU.mult)
    B.reduce(b3(em, [[4, NT], [1, 4]]), b3(tmpg, [[16, NT], [4, 4], [1, 4]]), ALU.add)
    B.reduce(m1.v(), b3(em, [[4, NT], [1, 4]]), ALU.max)
    B.tt(b3(oh1, [[4, NT], [1, 4]]), b3(em, [[4, NT], [1, 4]]), b3(m1, [[1, NT], [0, 4]]), ALU.is_ge)
    B.stt(em2.v(), oh1.v(), -1e30, em.v(), ALU.mult, ALU.add)
    B.reduce(m2.v(), b3(em2, [[4, NT], [1, 4]]), ALU.max)
    B.tt(b3(oh2, [[4, NT], [1, 4]]), b3(em2, [[4, NT], [1, 4]]), b3(m2, [[1, NT], [0, 4]]), ALU.is_ge)
    B.tt(dd.v(), m2.v(), m1.v(), ALU.subtract)
    B.act(ee.v(), dd.v(), AF.Exp)
    B.ts(w1.v(), ee.v(), 1.0, ALU.add)
    B.recip(w1.v(), w1.v())
    B.tt(w2.v(), ee.v(), w1.v(), ALU.mult)
    B.tt(w1.v(), w1.v(), gpw.v(), ALU.mult)
    B.tt(w2.v(), w2.v(), gpw.v(), ALU.mult)
    B.tt(b3(cig, [[4, NT], [1, 4]]), b3(oh1, [[4, NT], [1, 4]]), b3(w1, [[1, NT], [0, 4]]), ALU.mult)
    B.tt(b3(tm2, [[4, NT], [1, 4]]), b3(oh2, [[4, NT], [1, 4]]), b3(w2, [[1, NT], [0, 4]]), ALU.mult)
    B.tt(cig.v(), cig.v(), tm2.v(), ALU.add)
    B.tt(b3(comb, [[16, NT], [4, 4], [1, 4]]), b3(goh, [[4, NT], [1, 4], [0, 4]]), b3(cig, [[4, NT], [0, 4], [1, 4]]),
         ALU.mult)
    B.copy(b3(cgx, [[8, NT], [1, 4]]), b3(cig, [[4, NT], [1, 4]]), eng="dve")
    B.copy(b3(tm2, [[4, NT], [1, 4]]), b3(cgx, [[8, NT], [1, 4]]), eng="dve")
    B.tt(tm2.v(), cig.v(), tm2.v(), ALU.subtract)
    B.copy(b3(cgx, [[8, NT], [1, 4]], off=4), b3(tm2, [[4, NT], [1, 4]]), eng="dve")
    pcn = B.ps()
    for i in range(NT):
        for i2 in range(i + 1):
            lhs = Wt.v((SL, slice(256, 384))) if i2 < i else Wt.v((SL, slice(127, 255)))
            B.mm(pcn.v((SL, slice(4 * i, 4 * i + 4))), lhs, goh.v((SL, slice(4 * i2, 4 * i2 + 4))),
                 start=(i2 == 0), stop=(i2 == i))
    cnt = sm_("cnt", 4)
    B.copy(cnt.v(), pcn.v((SL, slice(0, 64))), eng="act")
    rsel, gid, ovm, ov1 = sm_("rsel", 1), sm_("gid", 1), sm_("ovm", 1), B.alloc("ov1", [1], F32)
    B.tt(tmpg.v((SL, slice(0, 64))), goh.v(), cnt.v(), ALU.mult)
    B.reduce(rsel.v(), b3(tmpg, [[4, NT], [1, 4]]), ALU.add)
    B.tt(b3(tmpg, [[4, NT], [1, 4]]), b3(goh, [[4, NT], [1, 4]]), b3(gidx, [[0, NT], [1, 4]]), ALU.mult)
    B.reduce(gid.v(), b3(tmpg, [[4, NT], [1, 4]]), ALU.add)
    B.stt(dest.v(), gid.v(), float(CAP), rsel.v(), ALU.mult, ALU.add)
    for sc in range(TG):
        B.ts(destm.v((SL, sc, SL)), dest.v(), -512.0 * sc, ALU.add)
    B.ts(ovm.v(), rsel.v(), float(CAP), ALU.is_ge)
    B.reduce(ov1.v(), ovm.v(), ALU.max)
    pov = B.ps()
    B.transpose(pov.v((slice(0, 1), slice(0, 128))), ov1.v(), C.ident_f.v())
    ovr = B.alloc("ovr", [128], F32)
    B.copy(ovr.v((slice(0, 1), SL)), pov.v((slice(0, 1), slice(0, 128))), eng="act")
    ov2 = B.alloc("ov2", [1], F32)
    B.reduce(ov2.v((slice(0, 1), SL)), ovr.v((slice(0, 1), SL)), ALU.max)
    fo, fi_ = flag_i.v((slice(0, 1), SL)), ov2.v((slice(0, 1), SL))
    fw = B.s.op("dve", lambda h: h.tensor_copy(fo.ap, fi_.ap), reads=fi_.keys, writes=fo.keys, cost=0.1)

    C.dbgt.update(dest=dest, cgx=cgx, comb=comb, rsel=rsel, gid=gid)
    B.s.begin_branch(fw, flag_i.ap[0:1, 0:1])
    B.top = SC0
    Pm = [B.alloc_at("Pm%d" % i, PM0 + i * K, [512], BF16) for i in range(NT)]
    iota = B.alloc_at("iota", SM0 + 2 * K, [512], F32)
    B.dma(iota.v(), dr["iota512"])
    cTs8 = B.alloc("cTs8", [512 * TG], BF16, subs=list(range(TG)))
    ev_ = 0
    for sc in range(TG):
        for i in range(NT):
            B.ts(Pm[i].v(), iota.v(), destm.v((SL, sc, slice(i, i + 1))), ALU.is_equal)
        for c in range(8):
            p = B.ps()
            for i in range(NT):
                B.mm(p.v(), h2_tok.v((SL, i, ts_(c)), sub=i), Pm[i].v(), start=(i == 0), stop=(i == NT - 1))
            B.copy(h2s.v((SL, c, ts_(sc, 512)), sub=range(4 * sc, 4 * sc + 4)), p.v(), eng=("act" if ev_ % 2 == 0 else "dve"))
            ev_ += 1
        p8 = B.ps()
        for i in range(NT):
            B.mm(p8.v((slice(0, 8), SL)), cgx.v((SL, i, SL)), Pm[i].v(), start=(i == 0), stop=(i == NT - 1))
        B.copy(cTs8.v((slice(0, 8), ts_(sc, 512)), sub=sc), p8.v((slice(0, 8), SL)), eng="act")
    mS = B.mark()
    B.s.barrier()
    wbuf = []
    for u in range(2):
        o = H0 + u * 24 * K
        wbuf.append((B.alloc_at("wgS%d" % u, o, [8, 512], BF16, subs=[0, 1]), B.alloc_at("wuS%d" % u, o + 8 * K, [8, 512], BF16, subs=[0, 1]),
                     B.alloc_at("wdS%d" % u, o + 16 * K, [4, 1024], BF16)))

    def load_expert(e, wb):
        wg_, wu_, wd_ = wb[e % 2]
        for j in range(2):
            B.dma(wg_.v((SL, SL, ts_(j, 256)), sub=j), dr["w_gate"][e][:, j * 256:(j + 1) * 256].rearrange("(k p) n -> p k n", p=128),
                  eng="pool", prefetch=True)
            B.dma(wu_.v((SL, SL, ts_(j, 256)), sub=j), dr["w_up"][e][:, j * 256:(j + 1) * 256].rearrange("(k p) n -> p k n", p=128),
                  eng="pool", prefetch=True)
        B.dma(wd_.v(), dr["w_down"][e].rearrange("(k p) n -> p k n", p=128), eng="pool", prefetch=True)

    load_expert(0, wbuf)
    destT = B.alloc("destT", [128], F32)
    DB = B.alloc("DB", [S], F32, subs=list(range(NT)))
    pdt = B.ps()
    B.transpose(pdt.v((slice(0, 16), slice(0, 128))), dest.v(), C.ident_f.v())
    B.copy(destT.v((slice(0, 16), SL)), pdt.v((slice(0, 16), slice(0, 128))), eng="act")
    for q4 in range(4):
        pdb = B.ps()
        for j in range(4):
            i = 4 * q4 + j
            eh = V(bcast(C.ident_f.ap[0:16, i:i + 1], [[0, 128]]), C.ident_f.v().keys)
            B.mm(pdb.v((SL, ts_(j))), eh, destT.v((slice(0, 16), SL)))
        B.copy(DB.v((SL, ts_(q4, 512)), sub=range(4 * q4, 4 * q4 + 4)), pdb.v(), eng="act")
    ysg = B.alloc("ysg", [TG, D], BF16)
    he = [B.alloc("heS", [4, CAP], BF16)]
    PTt = [B.alloc("PTt%d" % i, [TG, 128], BF16) for i in range(2)]
    sg = [B.alloc("sgS%d" % i, [512], F32) for i in range(2)]
    it = 0
    ip = 0
    ipb = 0
    B.ps_rot = [0, 1, 2, 3, 4, 5]
    for g in range(4):
        gsub = list(range(TG * g, TG * g + TG))
        for el in range(4):
            e = 4 * g + el
            if e + 1 < 16:
                load_expert(e + 1, wbuf)
            wg_, wu_, wd_ = wbuf[e % 2]
            he_ = he[0]
            for (n0, nn) in [(a, min(512, CAP - a)) for a in range(0, CAP, 512)]:
                s0 = g * CAP + n0
                pbc = B.ps_fixed(6 + ipb % 2)
                ipb += 1
                eh = V(bcast(sel8b.ap[0:8, el:el + 1], [[0, 128]]), sel8b.v().keys)
                B.mm(pbc.v((SL, slice(0, nn))), eh, cTs8.v((slice(0, 8), slice(s0, s0 + nn))))
                for fc in range(4):
                    pg = B.ps()
                    for k in range(8):
                        B.mm(pg.v((SL, slice(0, nn))), wg_.v((SL, k, ts_(fc)), sub=fc // 2), h2s.v((SL, k, slice(s0, s0 + nn)), sub=gsub),
                             start=(k == 0), stop=(k == 7))
                    pu = B.ps()
                    for k in range(8):
                        B.mm(pu.v((SL, slice(0, nn))), wu_.v((SL, k, ts_(fc)), sub=fc // 2), h2s.v((SL, k, slice(s0, s0 + nn)), sub=gsub),
                             start=(k == 0), stop=(k == 7))
                    s_ = sg[it % 2]
                    it += 1
                    B.act(s_.v((SL, slice(0, nn))), pg.v((SL, slice(0, nn))), AF.Silu)
                    B.tt(s_.v((SL, slice(0, nn))), pu.v((SL, slice(0, nn))), s_.v((SL, slice(0, nn))), ALU.mult)
                    B.tt(he_.v((SL, fc, slice(n0, n0 + nn))), s_.v((SL, slice(0, nn))), pbc.v((SL, slice(0, nn))), ALU.mult)
            for st in range(TG):
                for hf in range(2):
                    po = B.ps()
                    for fc in range(4):
                        B.mm(po.v(), he_.v((SL, fc, ts_(st))), wd_.v((SL, fc, ts_(hf, 512))), start=(fc == 0), stop=(fc == 3))
                    yv = ysg.v((SL, st, ts_(hf, 512)))
                    if el == 0:
                        B.copy(yv, po.v(), eng="act")
                    else:
                        B.tt(yv, po.v(), yv, ALU.add)
        for i in range(NT):
            pt_ = PTt[ip % 2]
            ip += 1
            d3 = V(bcast(DB.ap[:, i * 128:i * 128 + 1], [[0, TG], [1, 128]]), DB.v(sub=i).keys)
            s3 = V(bcast(sid.ap[:, TG * g:TG * g + 1], [[1, TG], [0, 128]]), sid.v().keys)
            B.tt(pt_.v(), d3, s3, ALU.is_equal)
            for hf in range(2):
                po = B.ps()
                for st in range(TG):
                    B.mm(po.v(), pt_.v((SL, st, SL)), ysg.v((SL, st, ts_(hf, 512))), start=(st == 0), stop=(st == TG - 1))
                xv = x1.v((SL, i, ts_(hf, 512)), sub=i)
                B.tt(xv, po.v(), xv, ALU.add)

    B.s.begin_else()
    B.ps_rot = list(range(8))
    B.top = SC0
    cT = B.alloc("cT", [S], F32, subs=list(range(NT)))
    for i in range(NT):
        p = B.ps(bf=True)
        for c in range(8):
            B.transpose(p.v((SL, ts_(c))), h2_tok.v((SL, i, ts_(c)), sub=i), C.ident_b.v())
        pin = V(p.ap[:, 0:1024].rearrange("p (c t) -> p c t", c=8), p.v().keys)
        B.copy(h2s.v((SL, SL, ts_(i)), sub=i), pin, eng=("act" if i % 2 == 0 else "dve"))
    for q4 in range(4):
        pc = B.ps()
        for j in range(4):
            i = q4 * 4 + j
            B.transpose(pc.v((slice(0, 16), ts_(j))), comb.v((SL, slice(i * 16, i * 16 + 16))), C.ident_f.v())
        B.copy(cT.v((slice(0, 16), ts_(q4, 512)), sub=list(range(4 * q4, 4 * q4 + 4))), pc.v((slice(0, 16), SL)), eng="act")
    B.s.barrier()
    wbufd = []
    for u in range(2):
        o = H0 + u * 24 * K
        wbufd.append((B.alloc_at("wgD%d" % u, o, [8, 512], BF16, subs=[0, 1]), B.alloc_at("wuD%d" % u, o + 8 * K, [8, 512], BF16, subs=[0, 1]),
                      B.alloc_at("wdD%d" % u, o + 16 * K, [4, 1024], BF16)))
    load_expert(0, wbufd)
    B.ps_rot = [0, 1, 2, 3, 4, 5]
    heT = [B.alloc("heT%d" % i, [4, 512], BF16) for i in range(2)]
    sgd = [B.alloc("sg%d" % i, [512], F32) for i in range(2)]
    ttd = [B.alloc("tt%d" % i, [512], F32) for i in range(2)]
    it = 0
    ic = 0
    for e in range(16):
        if e + 1 < 16:
            load_expert(e + 1, wbufd)
        wg_, wu_, wd_ = wbufd[e % 2]
        for tc in range(4):
            tsl = ts_(tc, 512)
            tl = list(range(4 * tc, 4 * tc + 4))
            pbc = B.ps_fixed(6 + ic % 2)
            eh = V(bcast(C.ident_f.ap[0:16, e:e + 1], [[0, 128]]), C.ident_f.v().keys)
            B.mm(pbc.v(), eh, cT.v((slice(0, 16), tsl), sub=tl))
            he_ = heT[ic % 2]
            ic += 1
            for fc in range(4):
                pg = B.ps()
                for k in range(8):
                    B.mm(pg.v(), wg_.v((SL, k, ts_(fc)), sub=fc // 2), h2s.v((SL, k, tsl), sub=tl), start=(k == 0), stop=(k == 7))
                pu = B.ps()
                for k in range(8):
                    B.mm(pu.v(), wu_.v((SL, k, ts_(fc)), sub=fc // 2), h2s.v((SL, k, tsl), sub=tl), start=(k == 0), stop=(k == 7))
                s_ = sgd[it % 2]
                t_ = ttd[it % 2]
                it += 1
                B.act(s_.v(), pg.v(), AF.Silu)
                B.tt(t_.v(), pu.v(), s_.v(), ALU.mult)
                B.tt(he_.v((SL, fc, SL)), t_.v(), pbc.v(), ALU.mult)
            for i4 in range(4):
                i = 4 * tc + i4
                for hf in range(2):
                    po = B.ps()
                    for fc in range(4):
                        B.mm(po.v(), he_.v((SL, fc, ts_(i4))), wd_.v((SL, fc, ts_(hf, 512))), start=(fc == 0), stop=(fc == 3))
                    xv = x1.v((SL, i, ts_(hf, 512)), sub=i)
                    B.tt(xv, po.v(), xv, ALU.add)
    B.s.end_branch()
    B.ps_rot = list(range(8))
    C.dbgt.update(h2s0=V(h2s.ap[:, 0, :], h2s.v().keys), cTs8=V(cTs8.ap[0:8, :], cTs8.v().keys), ysg=V(ysg.ap[:, 0, :], ysg.v().keys), DB=DB)


def phase_F(B, C):
    x2, x2T = C.x1, C.h2T
    dr = C.dram
    B.phase(98 * 1024)
    wpg = B.alloc("wpg", [8, 1024], BF16, subs=[0, 1])
    wpp = B.alloc("wpp", [2, 1024], BF16)
    for j in range(2):
        cs_ = slice(j * 512, (j + 1) * 512)
        B.dma(wpg.v((SL, SL, cs_), sub=j), dr["w_ple_gate"][:, cs_].rearrange("(k p) n -> p k n", p=128), eng="pool")
    B.dma(wpp.v(), dr["w_ple_proj"].rearrange("(k p) n -> p k n", p=128), eng="pool")
    gf = B.alloc("gf", [1024], F32)
    B.dma(gf.v(), dr["final_norm_g"])
    pT = B.alloc("pT", [2, S], BF16, subs=list(range(NT)))
    xb = [B.alloc("xb%d" % i, [D], BF16) for i in range(2)]
    pt_ = [B.alloc("ptl%d" % i, [256], F32) for i in range(2)]
    pb_ = [B.alloc("pbl%d" % i, [256], BF16) for i in range(2)]
    for i in range(NT):
        b = i % 2
        B.copy(xb[b].v(), x2.v((SL, i, SL), sub=i), eng="pool")
        p = B.ps(bf=True)
        for c in range(8):
            B.transpose(p.v((SL, ts_(c))), xb[b].v((SL, ts_(c))), C.ident_b.v())
        pin = V(p.ap[:, 0:1024].rearrange("p (c t) -> p c t", c=8), p.v().keys)
        B.copy(x2T.v((SL, SL, ts_(i)), sub=i), pin, eng="act")
        B.dma(pt_[b].v(), dr["p"][ts_(i), :])
        B.copy(pb_[b].v(), pt_[b].v(), eng="dve")
        p2 = B.ps(bf=True)
        for c in range(2):
            B.transpose(p2.v((SL, ts_(c))), pb_[b].v((SL, ts_(c))), C.ident_b.v())
        pin2 = V(p2.ap[:, 0:256].rearrange("p (c t) -> p c t", c=2), p2.v().keys)
        B.copy(pT.v((SL, SL, ts_(i)), sub=i), pin2, eng="dve")
    sg = [B.alloc("sgf%d" % i, [512], F32) for i in range(2)]
    x3 = [B.alloc("x3_%d" % i, [D], F32) for i in range(2)]
    ot = [B.alloc("otf%d" % i, [D], F32) for i in range(2)]
    junk = B.alloc("junkf", [D], BF16)
    ssf = [B.alloc("ssf%d" % i, [2], F32) for i in range(2)]
    it = 0
    for i in range(NT):
        b = i % 2
        for hf in range(2):
            hs = ts_(hf, 512)
            pg = B.ps()
            for k in range(8):
                B.mm(pg.v(), x2T.v((SL, k, ts_(i)), sub=i), wpg.v((SL, k, hs), sub=hf), start=(k == 0), stop=(k == 7))
            pp = B.ps()
            for k in range(2):
                B.mm(pp.v(), pT.v((SL, k, ts_(i)), sub=i), wpp.v((SL, k, hs)), start=(k == 0), stop=(k == 1))
            s_ = sg[it % 2]
            it += 1
            B.act(s_.v(), pg.v(), AF.Sigmoid)
            B.tt(s_.v(), pp.v(), s_.v(), ALU.mult)
            B.tt(x3[b].v((SL, hs)), x2.v((SL, i, hs), sub=i), s_.v(), ALU.add, eng="pool")
        B.act(junk.v(), x3[b].v(), AF.Square, accum=ssf[b].v((SL, slice(0, 1))))
        B.act(ssf[b].v((SL, slice(1, 2))), ssf[b].v((SL, slice(0, 1))), AF.Sqrt, bias=C.eps.v(), scale=1.0 / D)
        B.recip(ssf[b].v((SL, slice(1, 2))), ssf[b].v((SL, slice(1, 2))))
        B.stt(ot[b].v(), x3[b].v(), ssf[b].v((SL, slice(1, 2))), gf.v(), ALU.mult, ALU.mult)
        B.dma(C.out[ts_(i), :], ot[b].v(), is_out=True)


def build(dbg=None):
    import os
    nc = bass.Bass("TRN2", target_bir_lowering=False)
    C = Ctx()
    C.stop = os.environ.get('KSTOP', '')
    C.dbgt = {}
    C.dram = {}

    def din(name, shape, dt=F32):
        C.dram[name] = nc.dram_tensor(name, list(shape), dt, kind="ExternalInput").ap()
        return C.dram[name]

    C.x = din("x", [S, D])
    din("p", [S, 256])
    din("mix_norm_g", [128, 8])
    C.w_in = din("w_in", [D, IN_DIM])
    din("ident", [128, 128])
    din("tri_kq", [128, 128])
    din("qaug_c", [8, 4, S])
    din("kaug_c", [12, S])
    din("W_tri", [128, 384])
    din("W_tri2", [128, 384])
    din("Esel", [16, 16])
    din("conv_w", [128, 12, 4])
    din("conv_b", [128, 12])
    din("dt_bias", [128, 16])
    din("a_log", [128, 16])
    din("d_skip", [128, 16])
    din("ssd_norm_g", [128, 1024])
    din("w_out_a", [512, D])
    din("w_out_b", [D, D])
    din("w_out", [D, D])
    din("ffn_norm_g", [128, 8])
    din("w_rg", [D, 4])
    din("w_re", [D, 16])
    din("b_r", [128, 20])
    din("w_gate", [16, D, 512])
    din("w_up", [16, D, 512])
    din("w_down", [16, 512, D])
    din("w_ple_proj", [256, D])
    din("w_ple_gate", [D, D])
    din("final_norm_g", [128, D])
    din("ffn_norm_g_bc", [128, D])
    din("sel8", [8, 4])
    din("sid", [128, 24])
    din("gidx4", [128, 4])
    din("iota512", [128, 512])
    C.out = nc.dram_tensor("out", [S, D], F32, kind="ExternalOutput").ap()
    dbg_out = None
    if dbg is not None:
        dbg_out = nc.dram_tensor("dbg", list(dbg[1]), F32, kind="ExternalOutput").ap()

    B = Builder(nc, 212480)
    with ExitStack() as stack:
        B.setup(stack)
        sems = {e: stack.enter_context(nc.semaphore("sem_" + e)) for e in ENGS if e != "sp"}
        dma_sems = {q: [stack.enter_context(nc.semaphore("dsem_%s%d" % (q, i))) for i in range(Sched.NDMA)]
                    for q in ("sp", "pool")}
        eng_h = {"pe": nc.tensor, "act": nc.scalar, "dve": nc.vector, "pool": nc.gpsimd, "sp": nc.sync}
        regs = {e: stack.enter_context(eng_h[e].register("brflag_" + e)) for e in ENGS}
        block = stack.enter_context(nc.Block())

        ident_f = B.alloc("ident_f", [128], F32)
        C.ident_b = B.alloc("ident_b", [128], BF16)
        C.g1 = B.alloc("g1", [8], F32)
        B.dma(ident_f.v(), C.dram["ident"])
        B.dma(C.g1.v(), C.dram["mix_norm_g"])
        B.copy(C.ident_b.v(), ident_f.v(), eng="dve")
        C.ident_f = ident_f
        C.eps = B.alloc("eps", [1], F32)
        B.memset(C.eps.v(), EPS)
        assert B.top <= 2048
        K = 1024
        C.hT = B.alloc_at("hT", 2 * K, [8, S], BF16, subs=list(range(NT)))
        C.yaT = B.alloc_at("yaT", 34 * K, [4, S], BF16, subs=list(range(8)))
        C.ybT = B.alloc_at("ybT", 50 * K, [8, S], BF16, subs=list(range(NT)))
        C.mT = B.alloc_at("mT", 82 * K, [8, S], BF16, subs=list(range(NT)))
        C.x1 = B.alloc_at("x1", 2 * K, [NT, D], F32, subs=list(range(NT)))
        C.h2T = B.alloc_at("h2T", 66 * K, [8, S], BF16, subs=list(range(NT)))
        phase_A(B, C)
        if C.stop != "noB":
            phase_B(B, C)
        phase_C(B, C)
        phase_D(B, C)
        if dbg is not None and dbg[0] == "x1":
            B.phase(130 * K)
            for i in range(NT):
                B.dma(dbg_out[ts_(i), :], C.x1.v((SL, i, SL), sub=i), is_out=True)
        phase_E(B, C)
        if dbg is not None and dbg[0] in C.dbgt:
            B.phase(166 * K)
            dv = C.dbgt[dbg[0]]
            B.dma(dbg_out, dv if isinstance(dv, V) else dv.v(), is_out=True, eng="pool")
        if dbg is not None and dbg[0] == "x2":
            B.phase(130 * K)
            for i in range(NT):
                B.dma(dbg_out[ts_(i), :], C.x1.v((SL, i, SL), sub=i), is_out=True)
        phase_F(B, C)
        import time as _t
        _t0 = _t.time()
        B.s.emit(block, sems, dma_sems, reorder=(os.environ.get("KNOREORDER", "") == ""), regs=regs)
        if os.environ.get("KVERB"):
            print("emit s", _t.time() - _t0, "est us per segment", B.s.est, "stats", B.s.stats, "maxtop", B.maxtop)
    return nc, B


def host_consts():
    c = {}
    c["ident"] = np.eye(128, dtype=np.float32)
    s_ = np.arange(128)[:, None]
    t_ = np.arange(128)[None, :]
    c["tri_kq"] = np.where(t_ >= s_, 0.0, NEG).astype(np.float32)
    t = np.arange(S)
    bq = (t // 256).astype(np.float32)
    rq = (t % 256).astype(np.float32)
    qa = np.zeros((8, 4, S), np.float32)
    for h in range(8):
        sl = 2.0 ** (-(h + 1))
        qa[h, 0] = -8.0 * sl * 256.0 * bq
        qa[h, 1] = -8.0 * sl * rq
        qa[h, 2] = 8.0 * sl * 256.0
        qa[h, 3] = 8.0 * sl
    c["qaug_c"] = qa
    ka = np.zeros((12, S), np.float32)
    ka[0] = 1.0
    ka[1] = 1.0
    ka[2] = bq
    ka[3] = rq
    for j in range(8):
        ka[4 + j] = (bq == j).astype(np.float32)
    c["kaug_c"] = ka
    xx = np.arange(384)[None, :]
    ss_ = np.arange(128)[:, None]
    W = ((xx - 128) >= ss_).astype(np.float32)
    c["W_tri"] = W
    c["W_tri2"] = (1.0 - W).astype(np.float32)
    c["Esel"] = np.eye(16, dtype=np.float32)
    c["sel8"] = np.tile(np.eye(4, dtype=np.float32), (2, 1))
    c["sid"] = (np.arange(24)[None, :] * 128 + np.arange(128)[:, None]).astype(np.float32)
    c["gidx4"] = np.broadcast_to(np.arange(4, dtype=np.float32)[None, :], (128, 4)).copy()
    c["iota512"] = np.broadcast_to(np.arange(512, dtype=np.float32)[None, :], (128, 512)).copy()
    return c


def bc128(v):
    v = np.asarray(v, np.float32).reshape(1, -1)
    return np.ascontiguousarray(np.broadcast_to(v, (128, v.shape[1])))


def kernel(_dbg=None, **inputs):
    x = np.asarray(inputs["x"], dtype=np.float32)
    p = np.asarray(inputs["p"], dtype=np.float32)[0]
    nc, B = build(_dbg)
    f = lambda n: np.ascontiguousarray(np.asarray(inputs[n], np.float32)[0])
    shared = dict(host_consts())
    shared.update({
        "mix_norm_g": np.ascontiguousarray(f("mix_norm_g").reshape(8, 128).T),
        "w_in": f("w_in"),
        "conv_w": np.ascontiguousarray(f("conv_w").reshape(4, 12, 128).transpose(2, 1, 0)),
        "conv_b": np.ascontiguousarray(f("conv_b").reshape(12, 128).T),
        "dt_bias": bc128(f("dt_bias")), "a_log": bc128(f("a_log")),
        "d_skip": bc128(f("d_skip")), "ssd_norm_g": bc128(f("ssd_norm_g")),
        "w_out_a": f("w_out_a"), "w_out_b": f("w_out_b"), "w_out": f("w_out"),
        "ffn_norm_g": np.ascontiguousarray(f("ffn_norm_g").reshape(8, 128).T),
        "ffn_norm_g_bc": bc128(f("ffn_norm_g")),
        "w_rg": f("w_rg"), "w_re": f("w_re"),
        "b_r": bc128(np.concatenate([f("b_rg"), f("b_re")])),
        "w_gate": f("w_gate"), "w_up": f("w_up"), "w_down": f("w_down"),
        "w_ple_proj": f("w_ple_proj"), "w_ple_gate": f("w_ple_gate"),
        "final_norm_g": bc128(np.asarray(inputs["final_norm_g"], np.float32)),
    })
    in_maps = []
    for c in range(8):
        m = {"x": np.ascontiguousarray(x[c]), "p": np.ascontiguousarray(p[c])}
        m.update(shared)
        in_maps.append(m)
    res = run_bass_kernel_spmd(nc, in_maps, core_ids=list(range(8)))
    if _dbg is not None:
        return res.results[0]["dbg"]
    return np.stack([r["out"] for r in res.results], axis=0)
```

```python
import numpy as np
import ml_dtypes
from contextlib import ExitStack
import concourse.bass as bass
import concourse.mybir as mybir
from concourse.bass_utils import run_bass_kernel_spmd

F32 = mybir.dt.float32
BF16 = mybir.dt.bfloat16
U8 = mybir.dt.uint8
I32 = mybir.dt.int32
AF = mybir.ActivationFunctionType
ALU = mybir.AluOpType
AX = mybir.AxisListType

S = 2048
D = 1024
NT = 16
EPS = 1e-6
IN_DIM = 6160
OFF_Q, OFF_K, OFF_V, OFF_Z, OFF_XBC, OFF_DT, OFF_GA, OFF_GB = 0, 512, 1024, 1536, 2560, 4096, 4112, 5136
NEG = -240000.0

ENGS = ("pe", "act", "dve", "pool", "sp")
import os as _os0
STRICT_SAME_ENGINE = _os0.environ.get("KSTRICT", "1") == "1"
DSIZE = {F32: 4, BF16: 2, U8: 1, I32: 4}


class V:
    __slots__ = ("ap", "keys")

    def __init__(self, ap, keys):
        self.ap = ap
        self.keys = tuple(keys)


class Tile:
    def __init__(self, name, ap, subs=None):
        self.name = name
        self.ap = ap
        self.subs = subs

    def v(self, idx=None, sub=None):
        ap = self.ap if idx is None else self.ap[idx]
        if self.subs is None:
            keys = (self.name,)
        elif sub is None:
            keys = tuple((self.name, s) for s in self.subs)
        elif isinstance(sub, (list, tuple, range)):
            keys = tuple((self.name, s) for s in sub)
        else:
            keys = ((self.name, sub),)
        return V(ap, keys)


class Sched:
    NDMA = 16

    def __init__(self):
        self.ins = []
        self.last_w = {}
        self.readers = {}
        self.out_dmas = []
        self.bounds = []
        self.region = 0
        self.flag = None

    def op(self, eng, fn, reads=(), writes=(), dma=False, prefetch=False, cost=0.3, lat=0.0):
        i = len(self.ins)
        deps = {}
        for k in reads:
            w = self.last_w.get(k)
            if w is not None:
                deps[w] = True
        for k in writes:
            w = self.last_w.get(k)
            if w is not None:
                deps.setdefault(w, False)
            for r in self.readers.get(k, ()):
                deps.setdefault(r, False)
        rec = dict(eng=eng, fn=fn, deps=deps, dma=dma, prefetch=prefetch, cost=cost, lat=lat, region=self.region)
        rec['wk'] = tuple(writes)
        rec['rk'] = tuple(reads)
        self.ins.append(rec)
        for k in reads:
            self.readers.setdefault(k, []).append(i)
        for k in writes:
            self.last_w[k] = i
            self.readers[k] = []
        return i

    def barrier(self):
        if not self.bounds or self.bounds[-1] != len(self.ins):
            self.bounds.append(len(self.ins))

    def begin_branch(self, flag_writer, flag_ap):
        self.barrier()
        self.flag = (flag_writer, flag_ap)
        self._snap = (dict(self.last_w), {k: list(v) for k, v in self.readers.items()})
        self.region = 1

    def begin_else(self):
        self.barrier()
        self.last_w = dict(self._snap[0])
        self.readers = {k: list(v) for k, v in self._snap[1].items()}
        self.region = 2

    def end_branch(self):
        self.barrier()
        self.last_w = {}
        self.readers = {}
        self.region = 3

    def _schedule(self, ids, window=128, sync=0.2):
        ins = self.ins
        idset = set(ids)
        users = {i: [] for i in ids}
        nun = {}
        for i in ids:
            n = 0
            for d in ins[i]["deps"]:
                if d in idset:
                    users[d].append(i)
                    n += 1
            nun[i] = n
        pend = {e: [i for i in ids if ins[i]["eng"] == e] for e in ENGS}
        head = {e: 0 for e in ENGS}
        done = set()
        finish = {}
        ready = {}
        free = {e: 0.0 for e in ENGS}
        order = {e: [] for e in ENGS}

        def rtime(i):
            t = 0.0
            e = ins[i]["eng"]
            for d, raw in ins[i]["deps"].items():
                if d in finish:
                    f = finish[d]
                    if ins[d]["eng"] != e or ins[d]["dma"] or raw:
                        f += sync
                    if f > t:
                        t = f
            return t

        for i in ids:
            if nun[i] == 0:
                ready[i] = rtime(i)
        left = len(ids)
        while left:
            best = None
            for e in ENGS:
                lst = pend[e]
                h = head[e]
                while h < len(lst) and lst[h] in done:
                    h += 1
                head[e] = h
                cnt = 0
                j = h
                while j < len(lst) and cnt < window:
                    i = lst[j]
                    j += 1
                    if i in done:
                        continue
                    cnt += 1
                    if i in ready:
                        r = ready[i]
                        if r < free[e]:
                            r = free[e]
                        if best is None or (r, i) < best[0]:
                            best = ((r, i), e)
            (r, i), e = best
            rec = ins[i]
            if rec["dma"]:
                free[e] = r + rec["cost"]
                finish[i] = r + rec["cost"] + rec["lat"]
            else:
                free[e] = r + rec["cost"]
                finish[i] = free[e]
            done.add(i)
            order[e].append(i)
            left -= 1
            for u in users[i]:
                nun[u] -= 1
                if nun[u] == 0:
                    ready[u] = rtime(u)
        return order, max(finish.values()) if finish else 0.0

    def emit(self, block, sems, dma_sems, reorder=True, regs=None):
        ins = self.ins
        N = self.NDMA
        bounds = [b for b in self.bounds if 0 < b < len(ins)] + [len(ins)]
        segs = {0: [], 1: [], 2: [], 3: []}
        lo = 0
        self.est = []
        for b in bounds:
            ids = list(range(lo, b))
            lo = b
            if not ids:
                continue
            reg = ins[ids[0]]["region"]
            assert all(ins[i]["region"] == reg for i in ids)
            if reorder:
                order, t = self._schedule(ids)
                self.est.append((reg, round(t)))
            else:
                order = {e: [i for i in ids if ins[i]["eng"] == e] for e in ENGS}
            segs[reg].append((ids, order))
        has_branch = bool(segs[1] or segs[2])
        extra = {}
        force_signal = set()

        def chain(seglist, prev):
            for ids, order in seglist:
                if prev is not None:
                    pl, pd = prev
                    for e in ENGS:
                        if order[e]:
                            ex = extra.setdefault(order[e][0], {})
                            for d in pl + pd:
                                ex[d] = True
                last = []
                for e in ENGS:
                    for i in reversed(order[e]):
                        if not ins[i]["dma"]:
                            last.append(i)
                            break
                if prev is not None:
                    have = {ins[i]["eng"] for i in last}
                    last += [i for i in prev[0] if ins[i]["eng"] not in have]
                prev = (last, [i for i in ids if ins[i]["dma"] and not ins[i]["prefetch"]])
            return prev
        tail0 = chain(segs[0], None)
        if has_branch:
            chain(segs[1], tail0)
            chain(segs[2], tail0)
            chain(segs[3], None)
        rstream = {r: {e: [i for ids, order in segs[r] for i in order[e]] for e in ENGS} for r in range(4)}
        if has_branch:
            for r in (1, 2):
                for e in ENGS:
                    for i in reversed(rstream[r][e]):
                        if not ins[i]["dma"]:
                            force_signal.add(i)
                            break
        dma_j = {}
        nd = {}
        for q in ("sp", "pool"):
            l0 = [i for i in rstream[0][q] if ins[i]["dma"]]
            for j, i in enumerate(l0):
                dma_j[i] = j
                if j >= N:
                    extra.setdefault(i, {})[l0[j - N]] = True
            cnt = [len(l0)]
            for r in (1, 2):
                lr = l0 + [i for i in rstream[r][q] if ins[i]["dma"]]
                for j in range(len(l0), len(lr)):
                    dma_j[lr[j]] = j
                    if j >= N:
                        extra.setdefault(lr[j], {})[lr[j - N]] = True
                cnt.append(len(lr))
            J = (max(cnt) + N - 1) // N * N
            l3 = [i for i in rstream[3][q] if ins[i]["dma"]]
            for n, i in enumerate(l3):
                dma_j[i] = J + n
                if n >= N:
                    extra.setdefault(i, {})[l3[n - N]] = True
            nd[q] = (cnt, J)
        pos = {}
        for e in ENGS:
            n0 = len(rstream[0][e])
            for n, i in enumerate(rstream[0][e]):
                pos[i] = n
            for r in (1, 2):
                for n, i in enumerate(rstream[r][e]):
                    pos[i] = n0 + n
            for n, i in enumerate(rstream[3][e]):
                pos[i] = 10 ** 7 + n
        signal = set(force_signal)
        waits = {}
        if self.flag is not None:
            signal.add(self.flag[0])

        def prune(e, stream, known, known_dma, drop_old=False):
            for i in stream:
                r = ins[i]
                final = []
                best = {}
                alld = dict(r["deps"])
                for d, raw in extra.get(i, {}).items():
                    alld[d] = alld.get(d, False) or raw
                for d, raw in alld.items():
                    rd = ins[d]
                    if drop_old and rd["region"] != 3:
                        continue
                    if rd["dma"]:
                        if d not in known_dma:
                            known_dma.add(d)
                            final.append(d)
                        continue
                    e2 = rd["eng"]
                    if e2 == e and not r["dma"] and (e == "pe" or (not raw and not STRICT_SAME_ENGINE)):
                        continue
                    if known[e2] >= pos[d]:
                        continue
                    if e2 not in best or pos[d] > pos[best[e2]]:
                        best[e2] = d
                for e2, d in best.items():
                    known[e2] = pos[d]
                    final.append(d)
                    signal.add(d)
                waits[i] = final
        for e in ENGS:
            known = {x: -1 for x in ENGS}
            kd = set()
            prune(e, rstream[0][e], known, kd)
            for r in (1, 2):
                prune(e, rstream[r][e], dict(known), set(kd))
            prune(e, rstream[3][e], {x: -1 for x in ENGS}, set(), drop_old=has_branch)
        ev = {}
        cnt_e = {}
        for e in ENGS:
            c = 0
            cs = {}
            for i in rstream[0][e]:
                if not ins[i]["dma"] and i in signal:
                    c += 1
                    ev[i] = (sems[e], c)
            cs[0] = c
            for r in (1, 2):
                c = cs[0]
                for i in rstream[r][e]:
                    if not ins[i]["dma"] and i in signal:
                        c += 1
                        ev[i] = (sems[e], c)
                cs[r] = c
            c = max(cs[1], cs[2])
            cs["t"] = c
            for i in rstream[3][e]:
                if not ins[i]["dma"] and i in signal:
                    c += 1
                    ev[i] = (sems[e], c)
            cnt_e[e] = cs
        for i, j in dma_j.items():
            ev[i] = (dma_sems[ins[i]["eng"]][j % N], 16 * (j // N + 1))
        self.stats = {e: (sum(len(rstream[r][e]) for r in range(4)), cnt_e[e]) for e in ENGS}
        self.nwaits = sum(len(v) for v in waits.values())
        self.first_sig = {e: [(i, ins[i]['wk'], ins[i]['rk']) for i in rstream[0][e] if i in ev and not ins[i]['dma']][:3] for e in ENGS}
        final_waits = [ev[i] for i in self.out_dmas]
        join_waits = []
        if has_branch:
            for e in ENGS:
                if e != "sp":
                    join_waits.append((sems[e], cnt_e[e]["t"]))
            for q in ("sp", "pool"):
                cnt, J = nd[q]
                if J:
                    for s_ in range(N):
                        join_waits.append((dma_sems[q][s_], 16 * (J // N)))

        def run_stream(e, handle, stream):
            for i in stream:
                r = ins[i]
                for d in waits[i]:
                    s, v = ev[d]
                    handle.wait_ge(s, v)
                bi = r["fn"](handle)
                if r["dma"]:
                    bi.then_inc(ev[i][0], 16)
                elif i in signal:
                    bi.then_inc(sems[e], 1)

        def pads(e, handle, r):
            if e != "sp":
                cs = cnt_e[e]
                if cs[r] > cs[0]:
                    handle.wait_ge(sems[e], cs[r])
                if cs["t"] > cs[r]:
                    handle.sem_inc(sems[e], cs["t"] - cs[r])
            if e in nd:
                cnt, J = nd[e]
                n = cnt[r]
                for s_ in range(N):
                    real = 16 * len([j for j in range(n) if j % N == s_])
                    if real:
                        handle.wait_ge(dma_sems[e][s_], real)
                    if 16 * (J // N) > real:
                        handle.sem_inc(dma_sems[e][s_], 16 * (J // N) - real)

        def run(e, handle):
            run_stream(e, handle, rstream[0][e])
            if has_branch:
                fw, fap = self.flag
                s, v = ev[fw]
                handle.wait_ge(s, v)
                handle.reg_load(regs[e], fap)
                import os as _os
                with handle.If_eq(regs[e], int(_os.environ.get('KFLAGCMP', '0'))):
                    run_stream(e, handle, rstream[1][e])
                    pads(e, handle, 1)
                with handle.Else():
                    run_stream(e, handle, rstream[2][e])
                    pads(e, handle, 2)
                for s, v in join_waits:
                    handle.wait_ge(s, v)
                run_stream(e, handle, rstream[3][e])
            if e == "sp":
                for s, v in final_waits:
                    handle.wait_ge(s, v)

        block.tensor(lambda h: run("pe", h))
        block.scalar(lambda h: run("act", h))
        block.vector(lambda h: run("dve", h))
        block.gpsimd(lambda h: run("pool", h))
        block.sync(lambda h: run("sp", h))


class Builder:
    def __init__(self, nc, arena_bytes):
        self.nc = nc
        self.s = Sched()
        self.arena_bytes = arena_bytes
        self.top = 0
        self.uid = 0
        self.ps_i = 0

    def setup(self, stack):
        nc = self.nc
        self.arena = stack.enter_context(nc.sbuf_tensor("arena", [128, self.arena_bytes], U8))
        self.psum = [stack.enter_context(nc.psum_tensor("ps%d" % i, [128, 512], F32)) for i in range(8)]
        self.psum_t = [Tile("ps%d" % i, self.psum[i][:, :]) for i in range(8)]
        self.psum_bf = [Tile("ps%d" % i, self.psum[i].bitcast(BF16)[:, :]) for i in range(8)]

    def alloc(self, name, shape, dtype, subs=None, parts=128):
        n = int(np.prod(shape)) * DSIZE[dtype]
        n = (n + 31) // 32 * 32
        off = self.top
        self.top += n
        assert self.top <= self.arena_bytes, (name, self.top)
        self.maxtop = max(getattr(self, "maxtop", 0), self.top)
        ap = self.arena[0:parts, off:off + int(np.prod(shape)) * DSIZE[dtype]].bitcast(dtype)
        if len(shape) == 2:
            ap = ap.rearrange("p (a b) -> p a b", a=shape[0])
        elif len(shape) == 3:
            ap = ap.rearrange("p (a b c) -> p a b c", a=shape[0], b=shape[1])
        elif len(shape) == 4:
            ap = ap.rearrange("p (a b c d) -> p a b c d", a=shape[0], b=shape[1], c=shape[2])
        self.uid += 1
        return Tile("%s#%d" % (name, self.uid), ap, subs)

    def alloc_at(self, name, off, shape, dtype, subs=None):
        top = self.top
        self.top = off
        t = self.alloc(name, shape, dtype, subs)
        self.top = top
        return t

    def phase(self, base):
        self.s.barrier()
        self.top = base

    def mark(self):
        return self.top

    def release(self, mark):
        self.s.barrier()
        self.top = mark

    ps_rot = list(range(8))

    def ps(self, bf=False):
        self.ps_i = (self.ps_i + 1) % len(self.ps_rot)
        i = self.ps_rot[self.ps_i]
        return (self.psum_bf if bf else self.psum_t)[i]

    def ps_fixed(self, i, bf=False):
        return (self.psum_bf if bf else self.psum_t)[i]

    def dma(self, out, in_, eng="sp", out_keys=(), in_keys=(), prefetch=False, is_out=False):
        oa = out.ap if isinstance(out, V) else out
        ia = in_.ap if isinstance(in_, V) else in_
        ok = out.keys if isinstance(out, V) else tuple(out_keys)
        ik = in_.keys if isinstance(in_, V) else tuple(in_keys)
        try:
            nb = oa.partition_size() * oa.free_size() * DSIZE[oa.dtype]
        except Exception:
            nb = 512 * 1024
        i = self.s.op(eng, lambda h: h.dma_start(out=oa, in_=ia), reads=ik, writes=ok, dma=True,
                      prefetch=prefetch, cost=(0.2 if eng == "sp" else 1.2),
                      lat=2.5 + nb / (60e3 if eng == "pool" else 150e3))
        if is_out:
            self.s.out_dmas.append(i)
        return i

    def mm(self, out, lhsT, rhs, start=True, stop=True):
        n = rhs.ap.free_size()
        c = max(64, n) / 2400.0 * (4 if rhs.ap.dtype == F32 else 1) + 0.03
        self.s.op("pe", lambda h: h.matmul(out.ap, lhsT.ap, rhs.ap, start=start, stop=stop),
                  reads=lhsT.keys + rhs.keys, writes=out.keys, cost=c)

    def transpose(self, out, in_, ident):
        self.s.op("pe", lambda h: h.transpose(out.ap, in_.ap, ident.ap),
                  reads=in_.keys + ident.keys, writes=out.keys, cost=0.1)

    def act(self, out, in_, func, bias=None, scale=1.0, accum=None, eng="act"):
        reads = in_.keys
        kw = {}
        if isinstance(bias, V):
            reads = reads + bias.keys
            kw["bias"] = bias.ap
        elif bias is not None:
            kw["bias"] = bias
        if isinstance(scale, V):
            reads = reads + scale.keys
            kw["scale"] = scale.ap
        else:
            kw["scale"] = scale
        writes = out.keys
        if accum is not None:
            writes = writes + accum.keys
            kw["accum_out"] = accum.ap
        self.s.op(eng, lambda h: h.activation(out.ap, in_.ap, func, **kw), reads=reads, writes=writes,
                  cost=0.25 + in_.ap.free_size() / 1200.0)

    def tt(self, out, in0, in1, op, eng="dve"):
        self.s.op(eng, lambda h: h.tensor_tensor(out.ap, in0.ap, in1.ap, op),
                  reads=in0.keys + in1.keys, writes=out.keys, cost=self.vcost(eng, out))

    def ts(self, out, in0, s1, op0, s2=None, op1=None, eng="dve", accum=None):
        reads = in0.keys
        a1 = s1
        a2 = s2
        if isinstance(s1, V):
            reads = reads + s1.keys
            a1 = s1.ap
        if isinstance(s2, V):
            reads = reads + s2.keys
            a2 = s2.ap
        kw = {}
        writes = out.keys
        if op1 is not None:
            kw["op1"] = op1
        if accum is not None:
            kw["accum_out"] = accum.ap
            writes = writes + accum.keys
        self.s.op(eng, lambda h: h.tensor_scalar(out.ap, in0.ap, a1, a2, op0, **kw), reads=reads, writes=writes,
                  cost=self.vcost(eng, out))

    def stt(self, out, in0, scalar, in1, op0, op1, eng="dve"):
        reads = in0.keys + in1.keys
        sc = scalar
        if isinstance(scalar, V):
            reads = reads + scalar.keys
            sc = scalar.ap
        self.s.op(eng, lambda h: h.scalar_tensor_tensor(out.ap, in0.ap, sc, in1.ap, op0, op1),
                  reads=reads, writes=out.keys, cost=self.vcost(eng, out))

    def copy(self, out, in_, eng="dve"):
        if eng == "act":
            self.s.op("act", lambda h: h.copy(out.ap, in_.ap), reads=in_.keys, writes=out.keys,
                      cost=0.25 + in_.ap.free_size() / 1200.0)
        else:
            self.s.op(eng, lambda h: h.tensor_copy(out.ap, in_.ap), reads=in_.keys, writes=out.keys,
                      cost=self.vcost(eng, out))

    def reduce(self, out, in_, op, axis=AX.X, eng="dve"):
        self.s.op(eng, lambda h: h.tensor_reduce(out.ap, in_.ap, axis, op), reads=in_.keys, writes=out.keys,
                  cost=self.vcost(eng, in_))

    def recip(self, out, in_):
        self.s.op("dve", lambda h: h.reciprocal(out.ap, in_.ap), reads=in_.keys, writes=out.keys,
                  cost=self.vcost("dve", out))

    def memset(self, out, val, eng="pool"):
        self.s.op(eng, lambda h: h.memset(out.ap, val), reads=(), writes=out.keys, cost=self.vcost(eng, out))

    @staticmethod
    def vcost(eng, v):
        n = v.ap.free_size()
        return (0.1 + n / 960.0) if eng == "dve" else (0.2 + n / 500.0)


def bcast(ap, shape_steps):
    return bass.AP(ap.tensor, ap.offset, [list(ap.ap[0])] + [list(x) for x in shape_steps])


SL = slice(None)


def ts_(i, n=128):
    return slice(i * n, (i + 1) * n)


class Ctx:
    pass


def phase_A(B, C):
    B.phase(34 * 1024)
    xt = [B.alloc("xt%d" % i, [D], F32) for i in range(2)]
    xn = [B.alloc("xn%d" % i, [D], BF16) for i in range(2)]
    junk = B.alloc("junk", [D], BF16)
    ss = [B.alloc("ss%d" % i, [1], F32) for i in range(2)]
    rs = [B.alloc("rs%d" % i, [1], F32) for i in range(2)]
    for i in range(NT):
        b = i % 2
        B.dma(xt[b].v(), C.x[ts_(i), :])
        B.act(junk.v(), xt[b].v(), AF.Square, accum=ss[b].v())
        B.act(rs[b].v(), ss[b].v(), AF.Sqrt, bias=C.eps.v(), scale=1.0 / D)
        B.recip(rs[b].v(), rs[b].v())
        B.ts(xn[b].v(), xt[b].v(), rs[b].v(), ALU.mult)
        p = B.ps(bf=True)
        for c in range(8):
            B.transpose(p.v((SL, ts_(c))), xn[b].v((SL, ts_(c))), C.ident_b.v())
        pin = V(p.ap[:, 0:1024].rearrange("p (c t) -> p c t", c=8), p.v().keys)
        gb = V(bcast(C.g1.ap, [[1, 8], [0, 128]]), C.g1.v().keys)
        B.tt(C.hT.v((SL, SL, ts_(i)), sub=i), pin, gb, ALU.mult)


def phase_B(B, C):
    hT = C.hT
    B.top = 50 * 1024
    wqkv = B.alloc("wqkv", [8, 1536], BF16, subs=list(range(9)))
    for j in range(4):
        for qk in range(2):
            c0_ = qk * 512 + j * 128
            B.dma(wqkv.v((SL, SL, slice(c0_, c0_ + 128)), sub=qk * 4 + j),
                  C.w_in[:, c0_:c0_ + 128].rearrange("(k p) n -> p k n", p=128), eng="pool")
    B.dma(wqkv.v((SL, SL, slice(1024, 1536)), sub=8), C.w_in[:, 1024:1536].rearrange("(k p) n -> p k n", p=128), eng="pool")
    qa = [B.alloc("qa%d" % h, [S], BF16, subs=["d", "s", "m"]) for h in range(8)]
    ka = [B.alloc("ka%d" % h, [S], BF16, subs=["d", "s"]) for h in range(8)]
    va_e = B.alloc("va_e", [NT, 4, 65], BF16, subs=list(range(NT)) + ["one"])
    va_o = B.alloc("va_o", [NT, 4, 128], BF16, subs=list(range(NT)) + ["one"])
    ksum = B.alloc("ksum", [8, 8], F32, subs=list(range(8)))
    KM = B.alloc("KM", [8, 8], BF16, subs=list(range(8)))
    MBT = B.alloc("MBT", [S], BF16, subs=list(range(NT)))
    tri = B.alloc("tri", [128], F32)
    ones_f = B.alloc("ones_f", [128], F32)
    B.dma(tri.v(), C.dram["tri_kq"])
    B.memset(ones_f.v(), 1.0)
    B.memset(va_e.v((SL, SL, SL, slice(64, 65)), sub="one"), 1.0)
    B.memset(va_o.v((SL, SL, SL, slice(0, 64)), sub="one"), 0.0)
    B.memset(va_o.v((SL, SL, SL, slice(0, 1)), sub="one"), 1.0)

    def dpart(h):
        return slice(0, 64) if h % 2 == 0 else slice(64, 128)

    def kpart(h):
        return slice(0, 76) if h % 2 == 0 else slice(0, 128)
    for h in range(8):
        a0 = 64 if h % 2 == 0 else 0
        if h % 2 == 1:
            B.memset(qa[h].v((slice(0, 64), SL), sub=["s", "m"]), 0.0)
            B.memset(ka[h].v((slice(0, 64), SL), sub="s"), 0.0)
        B.dma(qa[h].v((slice(a0, a0 + 4), SL), sub="s"), C.dram["qaug_c"][h], eng="pool")
        B.dma(ka[h].v((slice(a0, a0 + 12), SL), sub="s"), C.dram["kaug_c"], eng="pool")

    for hp in range(4):
        for tc in range(4):
            tl = range(4 * tc, 4 * tc + 4)
            p = B.ps()
            for k in range(8):
                B.mm(p.v(), wqkv.v((SL, k, ts_(hp)), sub=hp), hT.v((SL, k, ts_(tc, 512)), sub=tl),
                     start=(k == 0), stop=(k == 7))
            for par in range(2):
                h = 2 * hp + par
                B.copy(qa[h].v((dpart(h), ts_(tc, 512)), sub="d"), p.v((dpart(h), SL)), eng=("act" if par == 0 else "dve"))
            p = B.ps()
            for k in range(8):
                B.mm(p.v(), wqkv.v((SL, k, slice(512 + hp * 128, 512 + hp * 128 + 128)), sub=4 + hp),
                     hT.v((SL, k, ts_(tc, 512)), sub=tl), start=(k == 0), stop=(k == 7))
            for par in range(2):
                h = 2 * hp + par
                for bb in range(2):
                    B.act(ka[h].v((dpart(h), slice(tc * 512 + bb * 256, tc * 512 + bb * 256 + 256)), sub="d"),
                          p.v((dpart(h), ts_(bb, 256))), AF.Copy,
                          accum=ksum.v((dpart(h), h, slice(2 * tc + bb, 2 * tc + bb + 1)), sub=h))
        for par in range(2):
            h = 2 * hp + par
            B.act(KM.v((dpart(h), h, SL), sub=h), ksum.v((dpart(h), h, SL), sub=h), AF.Copy, scale=1.0 / 256)

    if C.stop == 'B1':
        return
    for i in range(NT):
        p = B.ps()
        for k in range(8):
            B.mm(p.v(), hT.v((SL, k, ts_(i)), sub=i), wqkv.v((SL, k, slice(1024, 1536)), sub=8),
                 start=(k == 0), stop=(k == 7))
        pv4 = p.ap.rearrange("p (a b d) -> p a b d", a=4, b=2)
        B.copy(va_e.v((SL, i, SL, slice(0, 64)), sub=i), V(pv4[:, :, 0, :], p.v().keys), eng="dve")
        B.copy(va_o.v((SL, i, SL, slice(64, 128)), sub=i), V(pv4[:, :, 1, :], p.v().keys), eng="pool" if False else "dve")

    if C.stop == 'B2':
        return
    Gs = [B.alloc("Gs%d" % i, [64], F32) for i in range(2)]
    cmpt = [B.alloc("cmp%d" % i, [512], F32) for i in range(2)]
    rank = [B.alloc("rank%d" % i, [64], F32) for i in range(2)]
    mb = [B.alloc("mb%d" % i, [64], BF16) for i in range(2)]
    B.memset(MBT.v((slice(0, 64), slice(0, 256)), sub=[0, 1]), 0.0)
    for i in range(2, NT):
        b = i // 2
        u = i % 2
        gpe = B.ps()
        gpo = B.ps()
        for h in range(8):
            gp = gpe if h % 2 == 0 else gpo
            B.mm(gp.v((SL, slice((h // 2) * 8, (h // 2) * 8 + 8))), qa[h].v((dpart(h), ts_(i)), sub="d"),
                 KM.v((dpart(h), h, SL), sub=h))
        g4 = Gs[u].ap.rearrange("p (a b j) -> p a b j", a=4, b=2)
        B.copy(V(g4[:, :, 0, :], Gs[u].v().keys), V(gpe.ap[:, 0:32].rearrange("p (a j) -> p a j", a=4), gpe.v().keys), eng="act")
        B.copy(V(g4[:, :, 1, :], Gs[u].v().keys), V(gpo.ap[:, 0:32].rearrange("p (a j) -> p a j", a=4), gpo.v().keys), eng="act")
        gk = Gs[u].v().keys
        in0 = V(bcast(Gs[u].ap, [[8, 8], [0, b], [1, b]]), gk)
        in1 = V(bcast(Gs[u].ap, [[8, 8], [1, b], [0, b]]), gk)
        co = V(bcast(cmpt[u].ap, [[b * b, 8], [b, b], [1, b]]), cmpt[u].v().keys)
        B.tt(co, in0, in1, ALU.is_gt)
        ro = V(bcast(rank[u].ap, [[8, 8], [1, b]]), rank[u].v().keys)
        B.reduce(ro, co, ALU.add)
        B.memset(mb[u].v(), 0.0)
        mo = V(bcast(mb[u].ap, [[8, 8], [1, b]]), mb[u].v().keys)
        B.ts(mo, ro, 3.0, ALU.is_ge, NEG, ALU.mult)
        pt = B.ps(bf=True)
        B.transpose(pt.v((slice(0, 64), slice(0, 128))), mb[u].v(), C.ident_b.v())
        B.copy(MBT.v((slice(0, 64), ts_(i)), sub=i), pt.v((slice(0, 64), slice(0, 128))), eng="act")
    for h in range(8):
        a0 = 68 if h % 2 == 0 else 4
        B.dma(qa[h].v((slice(a0, a0 + 8), SL), sub="m"), MBT.v((slice(h * 8, h * 8 + 8), SL)))

    if C.stop == 'B3':
        return
    B.ps_rot = [0, 1, 2, 3, 4, 5]
    PT = [B.alloc("PT%d" % i, [512], BF16) for i in range(6)]
    tmp = [B.alloc("tmpd%d" % i, [128], F32) for i in range(3)]
    rden = [B.alloc("rden%d" % i, [512], F32) for i in range(2)]
    bcs = [B.alloc("bcs%d" % i, [512], F32) for i in range(2)]
    it = 0
    hq = 0
    for h in range(8):
        hp, par = h // 2, h % 2
        yp = dpart(h)
        dn = slice(64, 65) if par == 0 else slice(0, 1)
        op_ = slice(0, 65) if par == 0 else slice(0, 128)
        for Q in range(4):
            po = B.ps_fixed(6 + hq % 2)
            nk = 4 * (Q + 1)
            for kt in range(nk):
                m = kt - 4 * Q
                c0 = max(m, 0) * 128
                sp_ = B.ps()
                B.mm(sp_.v((SL, slice(c0, 512))), ka[h].v((kpart(h), ts_(kt))),
                     qa[h].v((kpart(h), slice(Q * 512 + c0, (Q + 1) * 512))))
                pt = PT[it % 6]
                if m >= 0:
                    t_ = tmp[it % 3]
                    B.tt(t_.v(), sp_.v((SL, slice(c0, c0 + 128))), tri.v(), ALU.add)
                    B.act(pt.v((SL, slice(c0, c0 + 128))), t_.v(), AF.Exp, scale=0.125)
                    if c0 + 128 < 512:
                        B.act(pt.v((SL, slice(c0 + 128, 512))), sp_.v((SL, slice(c0 + 128, 512))), AF.Exp, scale=0.125)
                else:
                    B.act(pt.v(), sp_.v(), AF.Exp, scale=0.125)
                vv = (va_e if par == 0 else va_o).v((SL, kt, hp, SL), sub=[kt, "one"])
                B.mm(po.v((op_, slice(c0, 512))), vv, pt.v((SL, slice(c0, 512))), start=(kt == 0), stop=(kt == nk - 1))
                it += 1
            rd = rden[hq % 2]
            B.recip(rd.v((dn, SL)), po.v((dn, SL)))
            pb = B.ps()
            if par == 0:
                B.mm(pb.v((slice(0, 64), SL)), ones_f.v((dn, slice(0, 64))), rd.v((dn, SL)))
            else:
                B.mm(pb.v(), ones_f.v((dn, SL)), rd.v((dn, SL)))
            bc = bcs[hq % 2]
            B.copy(bc.v((yp, SL)), pb.v((yp, SL)), eng="act")
            B.tt(C.yaT.v((yp, hp, ts_(Q, 512)), sub=h), po.v((yp, SL)), bc.v((yp, SL)), ALU.mult)
            hq += 1
    B.ps_rot = list(range(8))


def phase_C(B, C):
    hT = C.hT
    B.phase(82 * 1024)
    dr = C.dram
    wxbc = B.alloc("wxbc", [8, 1536], BF16, subs=list(range(6)))
    for j in range(6):
        B.dma(wxbc.v((SL, SL, ts_(j, 256)), sub=j),
              C.w_in[:, OFF_XBC + j * 256:OFF_XBC + (j + 1) * 256].rearrange("(k p) n -> p k n", p=128), eng="pool")
    wz = B.alloc("wz", [8, 1024], BF16, subs=[0, 1])
    for j in range(2):
        B.dma(wz.v((SL, SL, ts_(j, 512)), sub=j),
              C.w_in[:, OFF_Z + j * 512:OFF_Z + (j + 1) * 512].rearrange("(k p) n -> p k n", p=128), eng="pool")
    wdt = B.alloc("wdt", [8, 16], BF16)
    B.dma(wdt.v(), C.w_in[:, OFF_DT:OFF_DT + 16].rearrange("(k p) n -> p k n", p=128), eng="pool")
    Wt = B.alloc("Wt", [384], F32)
    W2 = B.alloc("W2", [384], F32)
    Esel = B.alloc("Esel", [16], F32)
    cw = B.alloc("cw", [12, 4], F32)
    cb = B.alloc("cb", [12], F32)
    dtb = B.alloc("dtb", [16], F32)
    A_bc = B.alloc("A_bc", [16], F32)
    dsk = B.alloc("dsk", [16], F32)
    ng = B.alloc("ng", [1024], F32)
    one_c = B.alloc("one_c", [1], F32)
    tri_b = B.alloc("tri_b", [128], BF16)
    B.dma(Wt.v(), dr["W_tri"])
    B.dma(W2.v(), dr["W_tri2"])
    B.dma(Esel.v((slice(0, 16), SL)), dr["Esel"])
    B.dma(cw.v(), dr["conv_w"])
    B.dma(cb.v(), dr["conv_b"])
    B.dma(dtb.v(), dr["dt_bias"])
    B.dma(A_bc.v(), dr["a_log"])
    B.dma(dsk.v(), dr["d_skip"])
    B.dma(ng.v(), dr["ssd_norm_g"])
    B.dma(tri_b.v(), dr["tri_kq"], eng="pool")
    B.memset(one_c.v(), 1.0)
    B.act(A_bc.v(), A_bc.v(), AF.Exp)
    B.ts(A_bc.v(), A_bc.v(), -1.0, ALU.mult)

    xr = [B.alloc("xr%d" % i, [259], F32) for i in range(3)]
    hist = B.alloc("hist", [12, 3], F32, subs=list(range(12)))
    acc = [B.alloc("acc%d" % i, [256], F32) for i in range(3)]
    xc = [B.alloc("xc%d" % i, [256], BF16) for i in range(3)]
    BT_l = [B.alloc("BT%d" % i, [2, 256], BF16, subs=[0, 1]) for i in range(2)]
    CT_l = [B.alloc("CT%d" % i, [2, 256], BF16, subs=[0, 1]) for i in range(2)]
    xs_tok_l = [B.alloc("xs_tok%d" % i, [2, 1024], BF16, subs=list(range(8))) for i in range(2)]
    B_tok_l = [B.alloc("B_tok%d" % i, [2, 2, 128], BF16, subs=[0, 1]) for i in range(2)]
    sz_l = [B.alloc("sz%d" % i, [2, 1024], BF16, subs=["%d%d" % (a, b) for a in range(2) for b in range(2)])
            for i in range(2)]
    xd_l = [B.alloc("xd%d" % i, [2, 16], F32) for i in range(2)]
    dt_l = [B.alloc("dt%d" % i, [2, 16], F32) for i in range(2)]
    da_l = [B.alloc("da%d" % i, [2, 16], F32) for i in range(2)]
    csT_l = [B.alloc("csT%d" % i, [256], F32) for i in range(2)]
    ncs_l = [B.alloc("ncs%d" % i, [2, 16], F32) for i in range(2)]
    ecs_l = [B.alloc("ecs%d" % i, [2, 16], F32) for i in range(2)]
    d2e_l = [B.alloc("d2e%d" % i, [2, 16], F32) for i in range(2)]
    dec_l = [B.alloc("dec%d" % i, [16], F32) for i in range(2)]
    xdt = B.alloc("xdt", [2, 1024], BF16)
    xdtd = B.alloc("xdtd", [2, 1024], BF16)
    CBT = B.alloc("CBT", [2, 384], F32, subs=[0, 1])
    LT = [B.alloc("LT%d" % i, [384], F32) for i in range(3)]
    MT = [B.alloc("MT%d" % i, [384], BF16) for i in range(4)]
    hst = B.alloc("hst", [1024], F32, subs=[0, 1])
    hsb = B.alloc("hsb", [1024], BF16, subs=[0, 1])
    t1_l = [B.alloc("t1_%d" % i, [1024], F32, subs=[0, 1]) for i in range(2)]
    u1_l = [B.alloc("u1_%d" % i, [1024], F32) for i in range(2)]
    junk = B.alloc("junkc", [512], BF16)
    ssq = B.alloc("ssq", [2], F32)
    rsd = B.alloc("rsd", [2], F32)
    ybk = B.alloc("ybk", [1024], BF16)

    B.ps_rot = [0, 1, 2, 3]
    B.memset(hist.v(), 0.0)
    itc = 0
    for c in range(8):
        T0 = 256 * c
        tiles = [2 * c, 2 * c + 1]
        BT, CT, xs_tok, B_tok, sz = BT_l[c % 2], CT_l[c % 2], xs_tok_l[c % 2], B_tok_l[c % 2], sz_l[c % 2]
        xd, dt, da, csT, ncs, ecs, d2e, dec = (xd_l[c % 2], dt_l[c % 2], da_l[c % 2], csT_l[c % 2], ncs_l[c % 2],
                                               ecs_l[c % 2], d2e_l[c % 2], dec_l[c % 2])
        for cc in range(12):
            p = B.ps()
            for k in range(8):
                B.mm(p.v((SL, slice(0, 256))), wxbc.v((SL, k, ts_(cc)), sub=cc // 2),
                     hT.v((SL, k, slice(T0, T0 + 256)), sub=tiles), start=(k == 0), stop=(k == 7))
            xr_ = xr[itc % 3]
            a_ = acc[itc % 3]
            x_ = xc[itc % 3]
            itc += 1
            B.copy(xr_.v((SL, slice(0, 3))), hist.v((SL, cc, SL), sub=cc), eng="pool")
            B.copy(xr_.v((SL, slice(3, 259))), p.v((SL, slice(0, 256))), eng="act")
            B.copy(hist.v((SL, cc, SL), sub=cc), xr_.v((SL, slice(256, 259))), eng="pool")
            B.ts(a_.v(), xr_.v((SL, slice(0, 256))), cw.v((SL, cc, slice(0, 1))), ALU.mult, 0.0, ALU.add,
                 eng="pool")
            for j in range(1, 4):
                B.stt(a_.v(), xr_.v((SL, slice(j, j + 256))), cw.v((SL, cc, slice(j, j + 1))), a_.v(),
                      ALU.mult, ALU.add)
            if cc < 8:
                B.act(x_.v(), a_.v(), AF.Silu, bias=cb.v((SL, slice(cc, cc + 1))))
                pt = B.ps(bf=True)
                for st in range(2):
                    B.transpose(pt.v((SL, ts_(st))), x_.v((SL, ts_(st))), C.ident_b.v())
                pin = V(pt.ap[:, 0:256].rearrange("p (s t) -> p s t", s=2), pt.v().keys)
                B.copy(xs_tok.v((SL, SL, ts_(cc)), sub=cc), pin, eng="dve")
            elif cc < 10:
                g = cc - 8
                B.act(BT.v((SL, g, SL), sub=g), a_.v(), AF.Silu, bias=cb.v((SL, slice(cc, cc + 1))))
                pt = B.ps(bf=True)
                for st in range(2):
                    B.transpose(pt.v((SL, ts_(st))), BT.v((SL, g, ts_(st)), sub=g), C.ident_b.v())
                pin = V(pt.ap[:, 0:256].rearrange("p (s t) -> p s t", s=2), pt.v().keys)
                B.copy(B_tok.v((SL, SL, g, SL), sub=g), pin, eng="dve")
            else:
                g = cc - 10
                B.act(CT.v((SL, g, SL), sub=g), a_.v(), AF.Silu, bias=cb.v((SL, slice(cc, cc + 1))))
        for st in range(2):
            for hf in range(2):
                p = B.ps()
                for k in range(8):
                    B.mm(p.v(), hT.v((SL, k, ts_(tiles[st])), sub=tiles[st]), wz.v((SL, k, ts_(hf, 512)), sub=hf),
                         start=(k == 0), stop=(k == 7))
                B.act(sz.v((SL, st, ts_(hf, 512)), sub="%d%d" % (st, hf)), p.v(), AF.Silu)
        for st in range(2):
            p = B.ps()
            for k in range(8):
                B.mm(p.v((SL, slice(0, 16))), hT.v((SL, k, ts_(tiles[st])), sub=tiles[st]), wdt.v((SL, k, SL)),
                     start=(k == 0), stop=(k == 7))
            B.tt(xd.v((SL, st, SL)), p.v((SL, slice(0, 16))), dtb.v(), ALU.add)
        B.act(xd.v(), xd.v(), AF.Exp)
        B.act(dt.v(), xd.v(), AF.Ln, bias=one_c.v())
        A2 = V(bcast(A_bc.ap, [[0, 2], [1, 16]]), A_bc.v().keys)
        B.tt(da.v(), dt.v(), A2, ALU.mult)
        pcs = B.ps()
        B.mm(pcs.v((slice(0, 16), slice(0, 256))), da.v((SL, 0, SL)), Wt.v((SL, slice(128, 384))), start=True, stop=False)
        B.mm(pcs.v((slice(0, 16), slice(0, 256))), da.v((SL, 1, SL)), Wt.v((SL, slice(0, 256))), start=False, stop=True)
        B.copy(csT.v((slice(0, 16), SL)), pcs.v((slice(0, 16), slice(0, 256))), eng="act")
        pct = B.ps()
        B.mm(pct.v((SL, slice(0, 16))), Wt.v((SL, slice(128, 256))), da.v((SL, 0, SL)))
        B.mm(pct.v((SL, slice(16, 32))), Wt.v((SL, slice(256, 384))), da.v((SL, 0, SL)), start=True, stop=False)
        B.mm(pct.v((SL, slice(16, 32))), Wt.v((SL, slice(128, 256))), da.v((SL, 1, SL)), start=False, stop=True)
        B.mm(pct.v((SL, slice(32, 48))), W2.v((SL, slice(128, 256))), da.v((SL, 0, SL)), start=True, stop=False)
        B.mm(pct.v((SL, slice(32, 48))), W2.v((SL, slice(0, 128))), da.v((SL, 1, SL)), start=False, stop=True)
        B.mm(pct.v((SL, slice(48, 64))), W2.v((SL, slice(128, 256))), da.v((SL, 1, SL)))
        B.mm(pct.v((SL, slice(64, 80))), Wt.v((SL, slice(256, 384))), da.v((SL, 0, SL)), start=True, stop=False)
        B.mm(pct.v((SL, slice(64, 80))), Wt.v((SL, slice(256, 384))), da.v((SL, 1, SL)), start=False, stop=True)
        B.act(ecs.v(), V(pct.ap[:, 0:32].rearrange("p (s h) -> p s h", s=2), pct.v().keys), AF.Exp)
        B.ts(ncs.v(), V(pct.ap[:, 0:32].rearrange("p (s h) -> p s h", s=2), pct.v().keys), -1.0, ALU.mult)
        B.act(d2e.v(), V(pct.ap[:, 32:64].rearrange("p (s h) -> p s h", s=2), pct.v().keys), AF.Exp)
        B.act(dec.v(), pct.v((SL, slice(64, 80))), AF.Exp)
        for st in range(2):
            xs3 = V(xs_tok.ap[:, st, :].rearrange("p (h d) -> p h d", h=16), xs_tok.v().keys)
            dtb3 = V(bcast(dt.ap[:, st, :], [[1, 16], [0, 64]]), dt.v().keys)
            xo3 = V(xdt.ap[:, st, :].rearrange("p (h d) -> p h d", h=16), xdt.v().keys)
            B.tt(xo3, xs3, dtb3, ALU.mult, eng="pool")
            if c < 7:
                d3 = V(bcast(d2e.ap[:, st, :], [[1, 16], [0, 64]]), d2e.v().keys)
                xo4 = V(xdtd.ap[:, st, :].rearrange("p (h d) -> p h d", h=16), xdtd.v().keys)
                B.tt(xo4, xo3, d3, ALU.mult, eng="pool")
        for g in range(2):
            p = B.ps()
            B.mm(p.v((SL, slice(0, 256))), BT.v((SL, g, slice(0, 128)), sub=g), CT.v((SL, g, SL), sub=g))
            B.mm(p.v((SL, slice(256, 384))), BT.v((SL, g, slice(128, 256)), sub=g), CT.v((SL, g, slice(128, 256)), sub=g))
            B.copy(CBT.v((SL, g, SL), sub=g), p.v((SL, slice(0, 384))), eng="dve")
        for h in range(16):
            g = h // 8
            pd = B.ps()
            eh = V(bcast(Esel.ap[0:16, h:h + 1], [[0, 128]]), Esel.v().keys)
            B.mm(pd.v((SL, slice(0, 256))), eh, csT.v((slice(0, 16), SL)), start=True, stop=False)
            B.mm(pd.v((SL, slice(0, 128))), C.ident_b.v(), tri_b.v(), start=False, stop=True)
            B.mm(pd.v((SL, slice(256, 384))), eh, csT.v((slice(0, 16), slice(128, 256))), start=True, stop=False)
            B.mm(pd.v((SL, slice(256, 384))), C.ident_b.v(), tri_b.v(), start=False, stop=True)
            lt_ = LT[h % 3]
            mt_ = MT[h % 4]
            B.act(lt_.v((SL, slice(0, 256))), pd.v((SL, slice(0, 256))), AF.Exp, bias=ncs.v((SL, 0, slice(h, h + 1))))
            B.act(lt_.v((SL, slice(256, 384))), pd.v((SL, slice(256, 384))), AF.Exp, bias=ncs.v((SL, 1, slice(h, h + 1))))
            B.tt(mt_.v(), lt_.v(), CBT.v((SL, g, SL), sub=g), ALU.mult)
            hc = slice(h * 64, h * 64 + 64)
            y0 = B.ps_fixed(4 + g)
            y1 = B.ps_fixed(6 + g)
            oc = slice((h % 8) * 64, (h % 8) * 64 + 64)
            B.mm(y0.v((SL, oc)), mt_.v((SL, slice(0, 128))), xdt.v((SL, 0, hc)))
            B.mm(y1.v((SL, oc)), mt_.v((SL, slice(128, 256))), xdt.v((SL, 0, hc)), start=True, stop=False)
            B.mm(y1.v((SL, oc)), mt_.v((SL, slice(256, 384))), xdt.v((SL, 1, hc)), start=False, stop=True)
        for lt in range(2):
            u_ = u1_l[lt]
            t1 = t1_l[lt]
            for g in range(2):
                yb_ = B.ps_fixed(4 + 2 * lt + g)
                hs = ts_(g, 512)
                if c > 0:
                    p = B.ps()
                    B.mm(p.v(), CT.v((SL, g, ts_(lt)), sub=g), hsb.v((SL, hs), sub=g))
                    e3 = V(bcast(ecs.ap[:, lt, g * 8:(g + 1) * 8], [[1, 8], [0, 64]]), ecs.v().keys)
                    p3 = V(p.ap.rearrange("p (h d) -> p h d", h=8), p.v().keys)
                    t13 = V(t1.ap[:, hs].rearrange("p (h d) -> p h d", h=8), t1.v(sub=g).keys)
                    B.tt(t13, p3, e3, ALU.mult)
                    B.tt(u_.v((SL, hs)), yb_.v(), t1.v((SL, hs), sub=g), ALU.add)
                else:
                    B.copy(u_.v((SL, hs)), yb_.v(), eng="dve")
            xs3b = V(xs_tok.ap[:, lt, :].rearrange("p (h d) -> p h d", h=16), xs_tok.v().keys)
            dk3 = V(bcast(dsk.ap, [[1, 16], [0, 64]]), dsk.v().keys)
            t1o = V(t1.ap.rearrange("p (h d) -> p h d", h=16), t1.v().keys)
            B.tt(t1o, xs3b, dk3, ALU.mult, eng="pool")
            B.tt(u_.v(), u_.v(), t1.v(), ALU.add, eng="pool")
            B.tt(u_.v(), u_.v(), sz.v((SL, lt, SL), sub=["%d0" % lt, "%d1" % lt]), ALU.mult)
            for g in range(2):
                B.act(junk.v(), u_.v((SL, ts_(g, 512))), AF.Square, accum=ssq.v((SL, slice(g, g + 1))))
            B.act(rsd.v(), ssq.v(), AF.Sqrt, bias=C.eps.v(), scale=1.0 / 512)
            B.recip(rsd.v(), rsd.v())
            for g in range(2):
                B.stt(ybk.v((SL, ts_(g, 512))), u_.v((SL, ts_(g, 512))), rsd.v((SL, slice(g, g + 1))),
                      ng.v((SL, ts_(g, 512))), ALU.mult, ALU.mult)
            pt = B.ps(bf=True)
            for k in range(8):
                B.transpose(pt.v((SL, ts_(k))), ybk.v((SL, ts_(k))), C.ident_b.v())
            pin = V(pt.ap[:, 0:1024].rearrange("p (c t) -> p c t", c=8), pt.v().keys)
            B.copy(C.ybT.v((SL, SL, ts_(tiles[lt])), sub=tiles[lt]), pin, eng="act")
        if c < 7:
            for g in range(2):
                hs = ts_(g, 512)
                p = B.ps()
                for lt in range(2):
                    B.mm(p.v(), B_tok.v((SL, lt, g, SL), sub=g), xdtd.v((SL, lt, hs)), start=(lt == 0), stop=(lt == 1))
                if c > 0:
                    dc3 = V(bcast(dec.ap[:, g * 8:(g + 1) * 8], [[1, 8], [0, 64]]), dec.v().keys)
                    h3 = V(hst.ap[:, hs].rearrange("p (h d) -> p h d", h=8), hst.v(sub=g).keys)
                    B.tt(h3, h3, dc3, ALU.mult)
                    B.tt(hst.v((SL, hs), sub=g), hst.v((SL, hs), sub=g), p.v(), ALU.add)
                else:
                    B.copy(hst.v((SL, hs), sub=g), p.v(), eng="dve")
                B.copy(hsb.v((SL, hs), sub=g), hst.v((SL, hs), sub=g), eng="pool")
    B.ps_rot = list(range(8))


def phase_D(B, C):
    hT, yaT, ybT, mT = C.hT, C.yaT, C.ybT, C.mT
    dr = C.dram
    B.phase(114 * 1024)
    woa = B.alloc("woa", [4, 1024], BF16, subs=[0, 1, 2, 3])
    wob = B.alloc("wob", [8, 1024], BF16, subs=[0, 1, 2, 3])
    wga = B.alloc("wga", [8, 1024], BF16, subs=[0, 1, 2, 3])
    wgb = B.alloc("wgb", [8, 1024], BF16, subs=[0, 1, 2, 3])
    for j in range(4):
        cs_ = slice(j * 256, (j + 1) * 256)
        B.dma(woa.v((SL, SL, cs_), sub=j), dr["w_out_a"][:, cs_].rearrange("(k p) n -> p k n", p=128), eng="pool")
        B.dma(wga.v((SL, SL, cs_), sub=j),
              C.w_in[:, OFF_GA + j * 256:OFF_GA + (j + 1) * 256].rearrange("(k p) n -> p k n", p=128), eng="pool")
        B.dma(wob.v((SL, SL, cs_), sub=j), dr["w_out_b"][:, cs_].rearrange("(k p) n -> p k n", p=128), eng="pool")
        B.dma(wgb.v((SL, SL, cs_), sub=j),
              C.w_in[:, OFF_GB + j * 256:OFF_GB + (j + 1) * 256].rearrange("(k p) n -> p k n", p=128), eng="pool")
    sga = B.alloc("sga", [512], F32)
    sgb = B.alloc("sgb", [512], F32)
    m1 = B.alloc("m1", [512], F32)
    m2 = B.alloc("m2", [512], F32)
    wo = B.alloc_at("wo", 180 * 1024, [8, 1024], BF16, subs=[0, 1])
    assert B.top <= 180 * 1024, B.top
    for j in range(2):
        cs_ = slice(j * 512, (j + 1) * 512)
        B.dma(wo.v((SL, SL, cs_), sub=j), dr["w_out"][:, cs_].rearrange("(k p) n -> p k n", p=128), eng="pool",
              prefetch=True)
    for cc in range(8):
        j = cc // 2
        for tc in range(4):
            tsl = ts_(tc, 512)
            tl = list(range(4 * tc, 4 * tc + 4))
            pga = B.ps()
            for k in range(8):
                B.mm(pga.v(), wga.v((SL, k, ts_(cc)), sub=j), hT.v((SL, k, tsl), sub=tl), start=(k == 0), stop=(k == 7))
            B.act(sga.v(), pga.v(), AF.Sigmoid)
            pa = B.ps()
            for pr in range(4):
                B.mm(pa.v(), woa.v((SL, pr, ts_(cc)), sub=j), yaT.v((SL, pr, tsl), sub=[2 * pr, 2 * pr + 1]),
                     start=(pr == 0), stop=(pr == 3))
            B.tt(m1.v(), pa.v(), sga.v(), ALU.mult)
            pgb = B.ps()
            for k in range(8):
                B.mm(pgb.v(), wgb.v((SL, k, ts_(cc)), sub=j), hT.v((SL, k, tsl), sub=tl), start=(k == 0), stop=(k == 7))
            B.act(sgb.v(), pgb.v(), AF.Sigmoid)
            pb = B.ps()
            for k in range(8):
                B.mm(pb.v(), wob.v((SL, k, ts_(cc)), sub=j), ybT.v((SL, k, tsl), sub=tl), start=(k == 0), stop=(k == 7))
            B.tt(m2.v(), pb.v(), sgb.v(), ALU.mult)
            B.tt(mT.v((SL, cc, tsl), sub=tl), m1.v(), m2.v(), ALU.add, eng="pool")
    B.phase(114 * 1024)
    x1 = C.x1
    xt = [B.alloc("xt%d" % i, [D], F32) for i in range(2)]
    for i in range(NT):
        b = i % 2
        B.dma(xt[b].v(), C.x[ts_(i), :])
        for hf in range(2):
            p = B.ps()
            for k in range(8):
                B.mm(p.v(), mT.v((SL, k, ts_(i)), sub=i), wo.v((SL, k, ts_(hf, 512)), sub=hf), start=(k == 0), stop=(k == 7))
            B.tt(x1.v((SL, i, ts_(hf, 512)), sub=i), p.v(), xt[b].v((SL, ts_(hf, 512))), ALU.add)


def phase_E(B, C):
    x1 = C.x1
    dr = C.dram
    K = 1024
    TG = 6
    CAP = 128 * TG
    H0 = 66 * K + 8 * K * TG
    PM0 = H0 + 32 * K
    SM0 = PM0 + 16 * K
    SC0 = SM0 + 4 * K
    B.phase(SC0)
    h2s = B.alloc_at("h2s", 66 * K, [8, 512 * TG], BF16, subs=list(range(4 * TG)))
    h2_tok = B.alloc_at("h2_tok", H0, [NT, D], BF16, subs=list(range(NT)))
    top0 = B.top
    B.top = SM0
    comb = B.alloc("comb", [NT * 16], F32)
    dest = B.alloc("dest", [NT], F32)
    destm = B.alloc("destm", [TG, NT], F32)
    cgx = B.alloc("cgx", [NT, 8], BF16)
    flag_i = B.alloc("flag_i", [1], I32)
    sel8 = B.alloc("sel8", [4], F32)
    sel8b = B.alloc("sel8b", [4], BF16)
    sid = B.alloc("sid", [4 * TG], F32)
    assert B.top <= SM0 + 2 * K, B.top
    B.top = top0
    B.dma(sel8.v((slice(0, 8), SL)), dr["sel8"])
    B.copy(sel8b.v((slice(0, 8), SL)), sel8.v((slice(0, 8), SL)), eng="dve")
    B.dma(sid.v(), dr["sid"])
    g2 = B.alloc("g2", [8], F32)
    B.dma(g2.v(), dr["ffn_norm_g"])
    g2bc = B.alloc("g2bc", [D], F32)
    B.dma(g2bc.v(), dr["ffn_norm_g_bc"])
    wr = B.alloc("wr", [8, 20], F32)
    B.dma(wr.v((SL, SL, slice(0, 4))), dr["w_rg"].rearrange("(k p) n -> p k n", p=128))
    B.dma(wr.v((SL, SL, slice(4, 20))), dr["w_re"].rearrange("(k p) n -> p k n", p=128))
    br = B.alloc("br", [20], F32)
    B.dma(br.v(), dr["b_r"])
    Wt = B.alloc("WtE", [384], F32)
    B.dma(Wt.v(), dr["W_tri"])
    gidx = B.alloc("gidx", [4], F32)
    B.dma(gidx.v(), dr["gidx4"])
    xn = [B.alloc("xnf%d" % i, [D], F32) for i in range(2)]
    h2f = [B.alloc("h2f%d" % i, [8, 128], F32) for i in range(2)]
    junk = B.alloc("junke", [D], BF16)
    sm = [B.alloc("rsm%d" % i, [2], F32) for i in range(2)]
    LG = B.alloc("LG", [NT, 20], F32, subs=list(range(NT)))
    for i in range(NT):
        b = i % 2
        ssv = sm[b].v((SL, slice(0, 1)))
        rsv = sm[b].v((SL, slice(1, 2)))
        B.act(junk.v(), x1.v((SL, i, SL), sub=i), AF.Square, accum=ssv)
        B.act(rsv, ssv, AF.Sqrt, bias=C.eps.v(), scale=1.0 / D)
        B.recip(rsv, rsv)
        B.ts(xn[b].v(), x1.v((SL, i, SL), sub=i), rsv, ALU.mult)
        B.tt(h2_tok.v((SL, i, SL), sub=i), xn[b].v(), g2bc.v(), ALU.mult, eng="pool")
        for hf in range(2):
            p = B.ps()
            for c4 in range(4):
                c = hf * 4 + c4
                B.transpose(p.v((SL, ts_(c4))), xn[b].v((SL, ts_(c))), C.ident_f.v())
            pin = V(p.ap.rearrange("p (c t) -> p c t", c=4), p.v().keys)
            gb = V(bcast(g2.ap[:, hf * 4:hf * 4 + 4], [[1, 4], [0, 128]]), g2.v().keys)
            B.tt(h2f[b].v((SL, slice(hf * 4, hf * 4 + 4), SL)), pin, gb, ALU.mult)
        pl = B.ps()
        for k in range(8):
            B.mm(pl.v((SL, slice(0, 20))), h2f[b].v((SL, k, SL)), wr.v((SL, k, SL)), start=(k == 0), stop=(k == 7))
        B.tt(LG.v((SL, i, SL), sub=i), pl.v((SL, slice(0, 20))), br.v(), ALU.add)

    def sm_(name, n):
        return B.alloc(name, [NT * n], F32)

    def b3(t, steps, off=0):
        a = t.ap
        return V(bass.AP(a.tensor, a.offset + off, [list(a.ap[0])] + [list(x) for x in steps]), t.v().keys)
    lgk = LG.v().keys
    gmax, gsh, ge, gsum, gpw, goh = sm_("gmax", 1), sm_("gsh", 4), sm_("ge", 4), sm_("gsum", 1), sm_("gpw", 1), sm_("goh", 4)
    tmpg, em, m1, oh1, em2, m2, oh2 = sm_("tmpg", 16), sm_("em", 4), sm_("m1", 1), sm_("oh1", 4), sm_("em2", 4), sm_("m2", 1), sm_("oh2", 4)
    dd, ee, w1, w2, cig, tm2 = sm_("dd", 1), sm_("ee", 1), sm_("w1", 1), sm_("w2", 1), sm_("cig", 4), sm_("tm2", 4)
    gl3 = V(bcast(LG.ap[:, 0, 0:1], [[20, NT], [1, 4]]), lgk)
    B.reduce(gmax.v(), gl3, ALU.max)
    B.tt(b3(gsh, [[4, NT], [1, 4]]), gl3, b3(gmax, [[1, NT], [0, 4]]), ALU.subtract)
    B.act(ge.v(), gsh.v(), AF.Exp)
    B.reduce(gsum.v(), b3(ge, [[4, NT], [1, 4]]), ALU.add)
    B.recip(gpw.v(), gsum.v())
    B.ts(goh.v(), gsh.v(), 0.0, ALU.is_ge)
    el3 = V(bcast(LG.ap[:, 0, 4:5], [[20, NT], [1, 4], [4, 4]]), lgk)
    B.tt(b3(tmpg, [[16, NT], [4, 4], [1, 4]]), el3, b3(goh, [[4, NT], [0, 4], [1, 4]]), ALU.mult)
    B.reduce(b3(em, [[4, NT], [1, 4]]), b3(tmpg, [[16, NT], [4, 4], [1, 4]]), ALU.add)
    B.reduce(m1.v(), b3(em, [[4, NT], [1, 4]]), ALU.max)
    B.tt(b3(oh1, [[4, NT], [1, 4]]), b3(em, [[4, NT], [1, 4]]), b3(m1, [[1, NT], [0, 4]]), ALU.is_ge)
    B.stt(em2.v(), oh1.v(), -1e30, em.v(), ALU.mult, ALU.add)
    B.reduce(m2.v(), b3(em2, [[4, NT], [1, 4]]), ALU.max)
    B.tt(b3(oh2, [[4, NT], [1, 4]]), b3(em2, [[4, NT], [1, 4]]), b3(m2, [[1, NT], [0, 4]]), ALU.is_ge)
    B.tt(dd.v(), m2.v(), m1.v(), ALU.subtract)
    B.act(ee.v(), dd.v(), AF.Exp)
    B.ts(w1.v(), ee.v(), 1.0, ALU.add)
    B.recip(w1.v(), w1.v())
    B.tt(w2.v(), ee.v(), w1.v(), ALU.mult)
    B.tt(w1.v(), w1.v(), gpw.v(), ALU.mult)
    B.tt(w2.v(), w2.v(), gpw.v(), ALU.mult)
    B.tt(b3(cig, [[4, NT], [1, 4]]), b3(oh1, [[4, NT], [1, 4]]), b3(w1, [[1, NT], [0, 4]]), ALU.mult)
    B.tt(b3(tm2, [[4, NT], [1, 4]]), b3(oh2, [[4, NT], [1, 4]]), b3(w2, [[1, NT], [0, 4]]), ALU.mult)
    B.tt(cig.v(), cig.v(), tm2.v(), ALU.add)
    B.tt(b3(comb, [[16, NT], [4, 4], [1, 4]]), b3(goh, [[4, NT], [1, 4], [0, 4]]), b3(cig, [[4, NT], [0, 4], [1, 4]]),
         ALU.mult)
    B.copy(b3(cgx, [[8, NT], [1, 4]]), b3(cig, [[4, NT], [1, 4]]), eng="dve")
    B.copy(b3(tm2, [[4, NT], [1, 4]]), b3(cgx, [[8, NT], [1, 4]]), eng="dve")
    B.tt(tm2.v(), cig.v(), tm2.v(), ALU.subtract)
    B.copy(b3(cgx, [[8, NT], [1, 4]], off=4), b3(tm2, [[4, NT], [1, 4]]), eng="dve")
    pcn = B.ps()
    for i in range(NT):
        for i2 in range(i + 1):
            lhs = Wt.v((SL, slice(256, 384))) if i2 < i else Wt.v((SL, slice(127, 255)))
            B.mm(pcn.v((SL, slice(4 * i, 4 * i + 4))), lhs, goh.v((SL, slice(4 * i2, 4 * i2 + 4))),
                 start=(i2 == 0), stop=(i2 == i))
    cnt = sm_("cnt", 4)
    B.copy(cnt.v(), pcn.v((SL, slice(0, 64))), eng="act")
    rsel, gid, ovm, ov1 = sm_("rsel", 1), sm_("gid", 1), sm_("ovm", 1), B.alloc("ov1", [1], F32)
    B.tt(tmpg.v((SL, slice(0, 64))), goh.v(), cnt.v(), ALU.mult)
    B.reduce(rsel.v(), b3(tmpg, [[4, NT], [1, 4]]), ALU.add)
    B.tt(b3(tmpg, [[4, NT], [1, 4]]), b3(goh, [[4, NT], [1, 4]]), b3(gidx, [[0, NT], [1, 4]]), ALU.mult)
    B.reduce(gid.v(), b3(tmpg, [[4, NT], [1, 4]]), ALU.add)
    B.stt(dest.v(), gid.v(), float(CAP), rsel.v(), ALU.mult, ALU.add)
    for sc in range(TG):
        B.ts(destm.v((SL, sc, SL)), dest.v(), -512.0 * sc, ALU.add)
    B.ts(ovm.v(), rsel.v(), float(CAP), ALU.is_ge)
    B.reduce(ov1.v(), ovm.v(), ALU.max)
    pov = B.ps()
    B.transpose(pov.v((slice(0, 1), slice(0, 128))), ov1.v(), C.ident_f.v())
    ovr = B.alloc("ovr", [128], F32)
    B.copy(ovr.v((slice(0, 1), SL)), pov.v((slice(0, 1), slice(0, 128))), eng="act")
    ov2 = B.alloc("ov2", [1], F32)
    B.reduce(ov2.v((slice(0, 1), SL)), ovr.v((slice(0, 1), SL)), ALU.max)
    fo, fi_ = flag_i.v((slice(0, 1), SL)), ov2.v((slice(0, 1), SL))
    fw = B.s.op("dve", lambda h: h.tensor_copy(fo.ap, fi_.ap), reads=fi_.keys, writes=fo.keys, cost=0.1)

    C.dbgt.update(dest=dest, cgx=cgx, comb=comb, rsel=rsel, gid=gid)
    B.s.begin_branch(fw, flag_i.ap[0:1, 0:1])
    B.top = SC0
    Pm = [B.alloc_at("Pm%d" % i, PM0 + i * K, [512], BF16) for i in range(NT)]
    iota = B.alloc_at("iota", SM0 + 2 * K, [512], F32)
    B.dma(iota.v(), dr["iota512"])
    cTs8 = B.alloc("cTs8", [512 * TG], BF16, subs=list(range(TG)))
    ev_ = 0
    for sc in range(TG):
        for i in range(NT):
            B.ts(Pm[i].v(), iota.v(), destm.v((SL, sc, slice(i, i + 1))), ALU.is_equal)
        for c in range(8):
            p = B.ps()
            for i in range(NT):
                B.mm(p.v(), h2_tok.v((SL, i, ts_(c)), sub=i), Pm[i].v(), start=(i == 0), stop=(i == NT - 1))
            B.copy(h2s.v((SL, c, ts_(sc, 512)), sub=range(4 * sc, 4 * sc + 4)), p.v(), eng=("act" if ev_ % 2 == 0 else "dve"))
            ev_ += 1
        p8 = B.ps()
        for i in range(NT):
            B.mm(p8.v((slice(0, 8), SL)), cgx.v((SL, i, SL)), Pm[i].v(), start=(i == 0), stop=(i == NT - 1))
        B.copy(cTs8.v((slice(0, 8), ts_(sc, 512)), sub=sc), p8.v((slice(0, 8), SL)), eng="act")
    mS = B.mark()
    B.s.barrier()
    wbuf = []
    for u in range(2):
        o = H0 + u * 24 * K
        wbuf.append((B.alloc_at("wgS%d" % u, o, [8, 512], BF16, subs=[0, 1]), B.alloc_at("wuS%d" % u, o + 8 * K, [8, 512], BF16, subs=[0, 1]),
                     B.alloc_at("wdS%d" % u, o + 16 * K, [4, 1024], BF16)))

    def load_expert(e, wb):
        wg_, wu_, wd_ = wb[e % 2]
        for j in range(2):
            B.dma(wg_.v((SL, SL, ts_(j, 256)), sub=j), dr["w_gate"][e][:, j * 256:(j + 1) * 256].rearrange("(k p) n -> p k n", p=128),
                  eng="pool", prefetch=True)
            B.dma(wu_.v((SL, SL, ts_(j, 256)), sub=j), dr["w_up"][e][:, j * 256:(j + 1) * 256].rearrange("(k p) n -> p k n", p=128),
                  eng="pool", prefetch=True)
        B.dma(wd_.v(), dr["w_down"][e].rearrange("(k p) n -> p k n", p=128), eng="pool", prefetch=True)

    load_expert(0, wbuf)
    destT = B.alloc("destT", [128], F32)
    DB = B.alloc("DB", [S], F32, subs=list(range(NT)))
    pdt = B.ps()
    B.transpose(pdt.v((slice(0, 16), slice(0, 128))), dest.v(), C.ident_f.v())
    B.copy(destT.v((slice(0, 16), SL)), pdt.v((slice(0, 16), slice(0, 128))), eng="act")
    for q4 in range(4):
        pdb = B.ps()
        for j in range(4):
            i = 4 * q4 + j
            eh = V(bcast(C.ident_f.ap[0:16, i:i + 1], [[0, 128]]), C.ident_f.v().keys)
            B.mm(pdb.v((SL, ts_(j))), eh, destT.v((slice(0, 16), SL)))
        B.copy(DB.v((SL, ts_(q4, 512)), sub=range(4 * q4, 4 * q4 + 4)), pdb.v(), eng="act")
    ysg = B.alloc("ysg", [TG, D], BF16)
    he = [B.alloc("heS", [4, CAP], BF16)]
    PTt = [B.alloc("PTt%d" % i, [TG, 128], BF16) for i in range(2)]
    sg = [B.alloc("sgS%d" % i, [512], F32) for i in range(2)]
    it = 0
    ip = 0
    ipb = 0
    B.ps_rot = [0, 1, 2, 3, 4, 5]
    for g in range(4):
        gsub = list(range(TG * g, TG * g + TG))
        for el in range(4):
            e = 4 * g + el
            if e + 1 < 16:
                load_expert(e + 1, wbuf)
            wg_, wu_, wd_ = wbuf[e % 2]
            he_ = he[0]
            for (n0, nn) in [(a, min(512, CAP - a)) for a in range(0, CAP, 512)]:
                s0 = g * CAP + n0
                pbc = B.ps_fixed(6 + ipb % 2)
                ipb += 1
                eh = V(bcast(sel8b.ap[0:8, el:el + 1], [[0, 128]]), sel8b.v().keys)
                B.mm(pbc.v((SL, slice(0, nn))), eh, cTs8.v((slice(0, 8), slice(s0, s0 + nn))))
                for fc in range(4):
                    pg = B.ps()
                    for k in range(8):
                        B.mm(pg.v((SL, slice(0, nn))), wg_.v((SL, k, ts_(fc)), sub=fc // 2), h2s.v((SL, k, slice(s0, s0 + nn)), sub=gsub),
                             start=(k == 0), stop=(k == 7))
                    pu = B.ps()
                    for k in range(8):
                        B.mm(pu.v((SL, slice(0, nn))), wu_.v((SL, k, ts_(fc)), sub=fc // 2), h2s.v((SL, k, slice(s0, s0 + nn)), sub=gsub),
                             start=(k == 0), stop=(k == 7))
                    s_ = sg[it % 2]
                    it += 1
                    B.act(s_.v((SL, slice(0, nn))), pg.v((SL, slice(0, nn))), AF.Silu)
                    B.tt(s_.v((SL, slice(0, nn))), pu.v((SL, slice(0, nn))), s_.v((SL, slice(0, nn))), ALU.mult)
                    B.tt(he_.v((SL, fc, slice(n0, n0 + nn))), s_.v((SL, slice(0, nn))), pbc.v((SL, slice(0, nn))), ALU.mult)
            for st in range(TG):
                for hf in range(2):
                    po = B.ps()
                    for fc in range(4):
                        B.mm(po.v(), he_.v((SL, fc, ts_(st))), wd_.v((SL, fc, ts_(hf, 512))), start=(fc == 0), stop=(fc == 3))
                    yv = ysg.v((SL, st, ts_(hf, 512)))
                    if el == 0:
                        B.copy(yv, po.v(), eng="act")
                    else:
                        B.tt(yv, po.v(), yv, ALU.add)
        for i in range(NT):
            pt_ = PTt[ip % 2]
            ip += 1
            d3 = V(bcast(DB.ap[:, i * 128:i * 128 + 1], [[0, TG], [1, 128]]), DB.v(sub=i).keys)
            s3 = V(bcast(sid.ap[:, TG * g:TG * g + 1], [[1, TG], [0, 128]]), sid.v().keys)
            B.tt(pt_.v(), d3, s3, ALU.is_equal)
            for hf in range(2):
                po = B.ps()
                for st in range(TG):
                    B.mm(po.v(), pt_.v((SL, st, SL)), ysg.v((SL, st, ts_(hf, 512))), start=(st == 0), stop=(st == TG - 1))
                xv = x1.v((SL, i, ts_(hf, 512)), sub=i)
                B.tt(xv, po.v(), xv, ALU.add)

    B.s.begin_else()
    B.ps_rot = list(range(8))
    B.top = SC0
    cT = B.alloc("cT", [S], F32, subs=list(range(NT)))
    for i in range(NT):
        p = B.ps(bf=True)
        for c in range(8):
            B.transpose(p.v((SL, ts_(c))), h2_tok.v((SL, i, ts_(c)), sub=i), C.ident_b.v())
        pin = V(p.ap[:, 0:1024].rearrange("p (c t) -> p c t", c=8), p.v().keys)
        B.copy(h2s.v((SL, SL, ts_(i)), sub=i), pin, eng=("act" if i % 2 == 0 else "dve"))
    for q4 in range(4):
        pc = B.ps()
        for j in range(4):
            i = q4 * 4 + j
            B.transpose(pc.v((slice(0, 16), ts_(j))), comb.v((SL, slice(i * 16, i * 16 + 16))), C.ident_f.v())
        B.copy(cT.v((slice(0, 16), ts_(q4, 512)), sub=list(range(4 * q4, 4 * q4 + 4))), pc.v((slice(0, 16), SL)), eng="act")
    B.s.barrier()
    wbufd = []
    for u in range(2):
        o = H0 + u * 24 * K
        wbufd.append((B.alloc_at("wgD%d" % u, o, [8, 512], BF16, subs=[0, 1]), B.alloc_at("wuD%d" % u, o + 8 * K, [8, 512], BF16, subs=[0, 1]),
                      B.alloc_at("wdD%d" % u, o + 16 * K, [4, 1024], BF16)))
    load_expert(0, wbufd)
    B.ps_rot = [0, 1, 2, 3, 4, 5]
    heT = [B.alloc("heT%d" % i, [4, 512], BF16) for i in range(2)]
    sgd = [B.alloc("sg%d" % i, [512], F32) for i in range(2)]
    ttd = [B.alloc("tt%d" % i, [512], F32) for i in range(2)]
    it = 0
    ic = 0
    for e in range(16):
        if e + 1 < 16:
            load_expert(e + 1, wbufd)
        wg_, wu_, wd_ = wbufd[e % 2]
        for tc in range(4):
            tsl = ts_(tc, 512)
            tl = list(range(4 * tc, 4 * tc + 4))
            pbc = B.ps_fixed(6 + ic % 2)
            eh = V(bcast(C.ident_f.ap[0:16, e:e + 1], [[0, 128]]), C.ident_f.v().keys)
            B.mm(pbc.v(), eh, cT.v((slice(0, 16), tsl), sub=tl))
            he_ = heT[ic % 2]
            ic += 1
            for fc in range(4):
                pg = B.ps()
                for k in range(8):
                    B.mm(pg.v(), wg_.v((SL, k, ts_(fc)), sub=fc // 2), h2s.v((SL, k, tsl), sub=tl), start=(k == 0), stop=(k == 7))
                pu = B.ps()
                for k in range(8):
                    B.mm(pu.v(), wu_.v((SL, k, ts_(fc)), sub=fc // 2), h2s.v((SL, k, tsl), sub=tl), start=(k == 0), stop=(k == 7))
                s_ = sgd[it % 2]
                t_ = ttd[it % 2]
                it += 1
                B.act(s_.v(), pg.v(), AF.Silu)
                B.tt(t_.v(), pu.v(), s_.v(), ALU.mult)
                B.tt(he_.v((SL, fc, SL)), t_.v(), pbc.v(), ALU.mult)
            for i4 in range(4):
                i = 4 * tc + i4
                for hf in range(2):
                    po = B.ps()
                    for fc in range(4):
                        B.mm(po.v(), he_.v((SL, fc, ts_(i4))), wd_.v((SL, fc, ts_(hf, 512))), start=(fc == 0), stop=(fc == 3))
                    xv = x1.v((SL, i, ts_(hf, 512)), sub=i)
                    B.tt(xv, po.v(), xv, ALU.add)
    B.s.end_branch()
    B.ps_rot = list(range(8))
    C.dbgt.update(h2s0=V(h2s.ap[:, 0, :], h2s.v().keys), cTs8=V(cTs8.ap[0:8, :], cTs8.v().keys), ysg=V(ysg.ap[:, 0, :], ysg.v().keys), DB=DB)


def phase_F(B, C):
    x2, x2T = C.x1, C.h2T
    dr = C.dram
    B.phase(98 * 1024)
    wpg = B.alloc("wpg", [8, 1024], BF16, subs=[0, 1, 2, 3])
    wpp = B.alloc("wpp", [2, 1024], BF16)
    for j in range(4):
        cs_ = slice(j * 256, (j + 1) * 256)
        B.dma(wpg.v((SL, SL, cs_), sub=j), dr["w_ple_gate"][:, cs_].rearrange("(k p) n -> p k n", p=128), eng="pool")
    B.dma(wpp.v(), dr["w_ple_proj"].rearrange("(k p) n -> p k n", p=128), eng="pool")
    gf = B.alloc("gf", [1024], F32)
    B.dma(gf.v(), dr["final_norm_g"])
    pT = B.alloc("pT", [2, S], BF16, subs=list(range(NT)))
    xb = [B.alloc("xb%d" % i, [D], BF16) for i in range(2)]
    pt_ = [B.alloc("ptl%d" % i, [256], F32) for i in range(2)]
    pb_ = [B.alloc("pbl%d" % i, [256], BF16) for i in range(2)]
    for i in range(NT):
        b = i % 2
        B.copy(xb[b].v(), x2.v((SL, i, SL), sub=i), eng="pool")
        p = B.ps(bf=True)
        for c in range(8):
            B.transpose(p.v((SL, ts_(c))), xb[b].v((SL, ts_(c))), C.ident_b.v())
        pin = V(p.ap[:, 0:1024].rearrange("p (c t) -> p c t", c=8), p.v().keys)
        B.copy(x2T.v((SL, SL, ts_(i)), sub=i), pin, eng="act")
        B.dma(pt_[b].v(), dr["p"][ts_(i), :])
        B.copy(pb_[b].v(), pt_[b].v(), eng="dve")
        p2 = B.ps(bf=True)
        for c in range(2):
            B.transpose(p2.v((SL, ts_(c))), pb_[b].v((SL, ts_(c))), C.ident_b.v())
        pin2 = V(p2.ap[:, 0:256].rearrange("p (c t) -> p c t", c=2), p2.v().keys)
        B.copy(pT.v((SL, SL, ts_(i)), sub=i), pin2, eng="dve")
    sg = [B.alloc("sgf%d" % i, [512], F32) for i in range(2)]
    x3 = [B.alloc("x3_%d" % i, [D], F32) for i in range(2)]
    ot = [B.alloc("otf%d" % i, [D], F32) for i in range(2)]
    junk = B.alloc("junkf", [D], BF16)
    ssf = [B.alloc("ssf%d" % i, [2], F32) for i in range(2)]
    it = 0
    for i in range(NT):
        b = i % 2
        for hf in range(2):
            hs = ts_(hf, 512)
            pg = B.ps()
            for k in range(8):
                B.mm(pg.v(), x2T.v((SL, k, ts_(i)), sub=i), wpg.v((SL, k, hs), sub=[2 * hf, 2 * hf + 1]), start=(k == 0), stop=(k == 7))
            pp = B.ps()
            for k in range(2):
                B.mm(pp.v(), pT.v((SL, k, ts_(i)), sub=i), wpp.v((SL, k, hs)), start=(k == 0), stop=(k == 1))
            s_ = sg[it % 2]
            it += 1
            B.act(s_.v(), pg.v(), AF.Sigmoid)
            B.tt(s_.v(), pp.v(), s_.v(), ALU.mult)
            B.tt(x3[b].v((SL, hs)), x2.v((SL, i, hs), sub=i), s_.v(), ALU.add, eng="pool")
        B.act(junk.v(), x3[b].v(), AF.Square, accum=ssf[b].v((SL, slice(0, 1))))
        B.act(ssf[b].v((SL, slice(1, 2))), ssf[b].v((SL, slice(0, 1))), AF.Sqrt, bias=C.eps.v(), scale=1.0 / D)
        B.recip(ssf[b].v((SL, slice(1, 2))), ssf[b].v((SL, slice(1, 2))))
        B.stt(ot[b].v(), x3[b].v(), ssf[b].v((SL, slice(1, 2))), gf.v(), ALU.mult, ALU.mult)
        B.dma(C.out[ts_(i), :], ot[b].v(), is_out=True)


def build(dbg=None):
    import os
    nc = bass.Bass("TRN2", target_bir_lowering=False)
    C = Ctx()
    C.stop = os.environ.get('KSTOP', '')
    C.dbgt = {}
    C.dram = {}

    def din(name, shape, dt=F32):
        C.dram[name] = nc.dram_tensor(name, list(shape), dt, kind="ExternalInput").ap()
        return C.dram[name]

    C.x = din("x", [S, D])
    din("p", [S, 256])
    din("mix_norm_g", [128, 8])
    C.w_in = din("w_in", [D, IN_DIM])
    din("ident", [128, 128])
    din("tri_kq", [128, 128])
    din("qaug_c", [8, 4, S])
    din("kaug_c", [12, S])
    din("W_tri", [128, 384])
    din("W_tri2", [128, 384])
    din("Esel", [16, 16])
    din("conv_w", [128, 12, 4])
    din("conv_b", [128, 12])
    din("dt_bias", [128, 16])
    din("a_log", [128, 16])
    din("d_skip", [128, 16])
    din("ssd_norm_g", [128, 1024])
    din("w_out_a", [512, D])
    din("w_out_b", [D, D])
    din("w_out", [D, D])
    din("ffn_norm_g", [128, 8])
    din("w_rg", [D, 4])
    din("w_re", [D, 16])
    din("b_r", [128, 20])
    din("w_gate", [16, D, 512])
    din("w_up", [16, D, 512])
    din("w_down", [16, 512, D])
    din("w_ple_proj", [256, D])
    din("w_ple_gate", [D, D])
    din("final_norm_g", [128, D])
    din("ffn_norm_g_bc", [128, D])
    din("sel8", [8, 4])
    din("sid", [128, 24])
    din("gidx4", [128, 4])
    din("iota512", [128, 512])
    C.out = nc.dram_tensor("out", [S, D], F32, kind="ExternalOutput").ap()
    dbg_out = None
    if dbg is not None:
        dbg_out = nc.dram_tensor("dbg", list(dbg[1]), F32, kind="ExternalOutput").ap()

    B = Builder(nc, 212480)
    with ExitStack() as stack:
        B.setup(stack)
        sems = {e: stack.enter_context(nc.semaphore("sem_" + e)) for e in ENGS if e != "sp"}
        dma_sems = {q: [stack.enter_context(nc.semaphore("dsem_%s%d" % (q, i))) for i in range(Sched.NDMA)]
                    for q in ("sp", "pool")}
        eng_h = {"pe": nc.tensor, "act": nc.scalar, "dve": nc.vector, "pool": nc.gpsimd, "sp": nc.sync}
        regs = {e: stack.enter_context(eng_h[e].register("brflag_" + e)) for e in ENGS}
        block = stack.enter_context(nc.Block())

        ident_f = B.alloc("ident_f", [128], F32)
        C.ident_b = B.alloc("ident_b", [128], BF16)
        C.g1 = B.alloc("g1", [8], F32)
        B.dma(ident_f.v(), C.dram["ident"])
        B.dma(C.g1.v(), C.dram["mix_norm_g"])
        B.copy(C.ident_b.v(), ident_f.v(), eng="dve")
        C.ident_f = ident_f
        C.eps = B.alloc("eps", [1], F32)
        B.memset(C.eps.v(), EPS)
        assert B.top <= 2048
        K = 1024
        C.hT = B.alloc_at("hT", 2 * K, [8, S], BF16, subs=list(range(NT)))
        C.yaT = B.alloc_at("yaT", 34 * K, [4, S], BF16, subs=list(range(8)))
        C.ybT = B.alloc_at("ybT", 50 * K, [8, S], BF16, subs=list(range(NT)))
        C.mT = B.alloc_at("mT", 82 * K, [8, S], BF16, subs=list(range(NT)))
        C.x1 = B.alloc_at("x1", 2 * K, [NT, D], F32, subs=list(range(NT)))
        C.h2T = B.alloc_at("h2T", 66 * K, [8, S], BF16, subs=list(range(NT)))
        phase_A(B, C)
        if C.stop != "noB":
            phase_B(B, C)
        phase_C(B, C)
        phase_D(B, C)
        if dbg is not None and dbg[0] == "x1":
            B.phase(130 * K)
            for i in range(NT):
                B.dma(dbg_out[ts_(i), :], C.x1.v((SL, i, SL), sub=i), is_out=True)
        phase_E(B, C)
        if dbg is not None and dbg[0] in C.dbgt:
            B.phase(166 * K)
            dv = C.dbgt[dbg[0]]
            B.dma(dbg_out, dv if isinstance(dv, V) else dv.v(), is_out=True, eng="pool")
        if dbg is not None and dbg[0] == "x2":
            B.phase(130 * K)
            for i in range(NT):
                B.dma(dbg_out[ts_(i), :], C.x1.v((SL, i, SL), sub=i), is_out=True)
        phase_F(B, C)
        import time as _t
        _t0 = _t.time()
        B.s.emit(block, sems, dma_sems, reorder=(os.environ.get("KNOREORDER", "") == ""), regs=regs)
        if os.environ.get("KVERB"):
            print("emit s", _t.time() - _t0, "est us per segment", B.s.est, "stats", B.s.stats, "maxtop", B.maxtop)
    return nc, B


def host_consts():
    c = {}
    c["ident"] = np.eye(128, dtype=np.float32)
    s_ = np.arange(128)[:, None]
    t_ = np.arange(128)[None, :]
    c["tri_kq"] = np.where(t_ >= s_, 0.0, NEG).astype(np.float32)
    t = np.arange(S)
    bq = (t // 256).astype(np.float32)
    rq = (t % 256).astype(np.float32)
    qa = np.zeros((8, 4, S), np.float32)
    for h in range(8):
        sl = 2.0 ** (-(h + 1))
        qa[h, 0] = -8.0 * sl * 256.0 * bq
        qa[h, 1] = -8.0 * sl * rq
        qa[h, 2] = 8.0 * sl * 256.0
        qa[h, 3] = 8.0 * sl
    c["qaug_c"] = qa
    ka = np.zeros((12, S), np.float32)
    ka[0] = 1.0
    ka[1] = 1.0
    ka[2] = bq
    ka[3] = rq
    for j in range(8):
        ka[4 + j] = (bq == j).astype(np.float32)
    c["kaug_c"] = ka
    xx = np.arange(384)[None, :]
    ss_ = np.arange(128)[:, None]
    W = ((xx - 128) >= ss_).astype(np.float32)
    c["W_tri"] = W
    c["W_tri2"] = (1.0 - W).astype(np.float32)
    c["Esel"] = np.eye(16, dtype=np.float32)
    c["sel8"] = np.tile(np.eye(4, dtype=np.float32), (2, 1))
    c["sid"] = (np.arange(24)[None, :] * 128 + np.arange(128)[:, None]).astype(np.float32)
    c["gidx4"] = np.broadcast_to(np.arange(4, dtype=np.float32)[None, :], (128, 4)).copy()
    c["iota512"] = np.broadcast_to(np.arange(512, dtype=np.float32)[None, :], (128, 512)).copy()
    return c


def bc128(v):
    v = np.asarray(v, np.float32).reshape(1, -1)
    return np.ascontiguousarray(np.broadcast_to(v, (128, v.shape[1])))


def kernel(_dbg=None, **inputs):
    x = np.asarray(inputs["x"], dtype=np.float32)
    p = np.asarray(inputs["p"], dtype=np.float32)[0]
    nc, B = build(_dbg)
    f = lambda n: np.ascontiguousarray(np.asarray(inputs[n], np.float32)[0])
    shared = dict(host_consts())
    shared.update({
        "mix_norm_g": np.ascontiguousarray(f("mix_norm_g").reshape(8, 128).T),
        "w_in": f("w_in"),
        "conv_w": np.ascontiguousarray(f("conv_w").reshape(4, 12, 128).transpose(2, 1, 0)),
        "conv_b": np.ascontiguousarray(f("conv_b").reshape(12, 128).T),
        "dt_bias": bc128(f("dt_bias")), "a_log": bc128(f("a_log")),
        "d_skip": bc128(f("d_skip")), "ssd_norm_g": bc128(f("ssd_norm_g")),
        "w_out_a": f("w_out_a"), "w_out_b": f("w_out_b"), "w_out": f("w_out"),
        "ffn_norm_g": np.ascontiguousarray(f("ffn_norm_g").reshape(8, 128).T),
        "ffn_norm_g_bc": bc128(f("ffn_norm_g")),
        "w_rg": f("w_rg"), "w_re": f("w_re"),
        "b_r": bc128(np.concatenate([f("b_rg"), f("b_re")])),
        "w_gate": f("w_gate"), "w_up": f("w_up"), "w_down": f("w_down"),
        "w_ple_proj": f("w_ple_proj"), "w_ple_gate": f("w_ple_gate"),
        "final_norm_g": bc128(np.asarray(inputs["final_norm_g"], np.float32)),
    })
    in_maps = []
    for c in range(8):
        m = {"x": np.ascontiguousarray(x[c]), "p": np.ascontiguousarray(p[c])}
        m.update(shared)
        in_maps.append(m)
    res = run_bass_kernel_spmd(nc, in_maps, core_ids=list(range(8)))
    if _dbg is not None:
        return res.results[0]["dbg"]
    return np.stack([r["out"] for r in res.results], axis=0)
```

```python
import numpy as np
import ml_dtypes
from contextlib import ExitStack
import concourse.bass as bass
import concourse.mybir as mybir
from concourse.bass_utils import run_bass_kernel_spmd

F32 = mybir.dt.float32
BF16 = mybir.dt.bfloat16
U8 = mybir.dt.uint8
I32 = mybir.dt.int32
AF = mybir.ActivationFunctionType
ALU = mybir.AluOpType
AX = mybir.AxisListType

S = 2048
D = 1024
NT = 16
EPS = 1e-6
IN_DIM = 6160
OFF_Q, OFF_K, OFF_V, OFF_Z, OFF_XBC, OFF_DT, OFF_GA, OFF_GB = 0, 512, 1024, 1536, 2560, 4096, 4112, 5136
NEG = -240000.0

ENGS = ("pe", "act", "dve", "pool", "sp")
import os as _os0
STRICT_SAME_ENGINE = _os0.environ.get("KSTRICT", "1") == "1"
DSIZE = {F32: 4, BF16: 2, U8: 1, I32: 4}


class V:
    __slots__ = ("ap", "keys")

    def __init__(self, ap, keys):
        self.ap = ap
        self.keys = tuple(keys)


class Tile:
    def __init__(self, name, ap, subs=None):
        self.name = name
        self.ap = ap
        self.subs = subs

    def v(self, idx=None, sub=None):
        ap = self.ap if idx is None else self.ap[idx]
        if self.subs is None:
            keys = (self.name,)
        elif sub is None:
            keys = tuple((self.name, s) for s in self.subs)
        elif isinstance(sub, (list, tuple, range)):
            keys = tuple((self.name, s) for s in sub)
        else:
            keys = ((self.name, sub),)
        return V(ap, keys)


class Sched:
    NDMA = 16

    def __init__(self):
        self.ins = []
        self.last_w = {}
        self.readers = {}
        self.out_dmas = []
        self.bounds = []
        self.region = 0
        self.flag = None

    def op(self, eng, fn, reads=(), writes=(), dma=False, prefetch=False, cost=0.3, lat=0.0):
        i = len(self.ins)
        deps = {}
        for k in reads:
            w = self.last_w.get(k)
            if w is not None:
                deps[w] = True
        for k in writes:
            w = self.last_w.get(k)
            if w is not None:
                deps.setdefault(w, False)
            for r in self.readers.get(k, ()):
                deps.setdefault(r, False)
        rec = dict(eng=eng, fn=fn, deps=deps, dma=dma, prefetch=prefetch, cost=cost, lat=lat, region=self.region)
        rec['wk'] = tuple(writes)
        rec['rk'] = tuple(reads)
        self.ins.append(rec)
        for k in reads:
            self.readers.setdefault(k, []).append(i)
        for k in writes:
            self.last_w[k] = i
            self.readers[k] = []
        return i

    def barrier(self):
        if not self.bounds or self.bounds[-1] != len(self.ins):
            self.bounds.append(len(self.ins))

    def begin_branch(self, flag_writer, flag_ap):
        self.barrier()
        self.flag = (flag_writer, flag_ap)
        self._snap = (dict(self.last_w), {k: list(v) for k, v in self.readers.items()})
        self.region = 1

    def begin_else(self):
        self.barrier()
        self.last_w = dict(self._snap[0])
        self.readers = {k: list(v) for k, v in self._snap[1].items()}
        self.region = 2

    def end_branch(self):
        self.barrier()
        self.last_w = {}
        self.readers = {}
        self.region = 3

    def _schedule(self, ids, window=128, sync=0.2):
        ins = self.ins
        idset = set(ids)
        users = {i: [] for i in ids}
        nun = {}
        for i in ids:
            n = 0
            for d in ins[i]["deps"]:
                if d in idset:
                    users[d].append(i)
                    n += 1
            nun[i] = n
        pend = {e: [i for i in ids if ins[i]["eng"] == e] for e in ENGS}
        head = {e: 0 for e in ENGS}
        done = set()
        finish = {}
        ready = {}
        free = {e: 0.0 for e in ENGS}
        order = {e: [] for e in ENGS}

        def rtime(i):
            t = 0.0
            e = ins[i]["eng"]
            for d, raw in ins[i]["deps"].items():
                if d in finish:
                    f = finish[d]
                    if ins[d]["eng"] != e or ins[d]["dma"] or raw:
                        f += sync
                    if f > t:
                        t = f
            return t

        for i in ids:
            if nun[i] == 0:
                ready[i] = rtime(i)
        left = len(ids)
        while left:
            best = None
            for e in ENGS:
                lst = pend[e]
                h = head[e]
                while h < len(lst) and lst[h] in done:
                    h += 1
                head[e] = h
                cnt = 0
                j = h
                while j < len(lst) and cnt < window:
                    i = lst[j]
                    j += 1
                    if i in done:
                        continue
                    cnt += 1
                    if i in ready:
                        r = ready[i]
                        if r < free[e]:
                            r = free[e]
                        if best is None or (r, i) < best[0]:
                            best = ((r, i), e)
            (r, i), e = best
            rec = ins[i]
            if rec["dma"]:
                free[e] = r + rec["cost"]
                finish[i] = r + rec["cost"] + rec["lat"]
            else:
                free[e] = r + rec["cost"]
                finish[i] = free[e]
            done.add(i)
            order[e].append(i)
            left -= 1
            for u in users[i]:
                nun[u] -= 1
                if nun[u] == 0:
                    ready[u] = rtime(u)
        return order, max(finish.values()) if finish else 0.0

    def emit(self, block, sems, dma_sems, reorder=True, regs=None):
        ins = self.ins
        N = self.NDMA
        bounds = [b for b in self.bounds if 0 < b < len(ins)] + [len(ins)]
        segs = {0: [], 1: [], 2: [], 3: []}
        lo = 0
        self.est = []
        for b in bounds:
            ids = list(range(lo, b))
            lo = b
            if not ids:
                continue
            reg = ins[ids[0]]["region"]
            assert all(ins[i]["region"] == reg for i in ids)
            if reorder:
                order, t = self._schedule(ids)
                self.est.append((reg, round(t)))
            else:
                order = {e: [i for i in ids if ins[i]["eng"] == e] for e in ENGS}
            segs[reg].append((ids, order))
        has_branch = bool(segs[1] or segs[2])
        extra = {}
        force_signal = set()

        def chain(seglist, prev):
            for ids, order in seglist:
                if prev is not None:
                    pl, pd = prev
                    for e in ENGS:
                        if order[e]:
                            ex = extra.setdefault(order[e][0], {})
                            for d in pl + pd:
                                ex[d] = True
                last = []
                for e in ENGS:
                    for i in reversed(order[e]):
                        if not ins[i]["dma"]:
                            last.append(i)
                            break
                if prev is not None:
                    have = {ins[i]["eng"] for i in last}
                    last += [i for i in prev[0] if ins[i]["eng"] not in have]
                prev = (last, [i for i in ids if ins[i]["dma"] and not ins[i]["prefetch"]])
            return prev
        tail0 = chain(segs[0], None)
        if has_branch:
            chain(segs[1], tail0)
            chain(segs[2], tail0)
            chain(segs[3], None)
        rstream = {r: {e: [i for ids, order in segs[r] for i in order[e]] for e in ENGS} for r in range(4)}
        if has_branch:
            for r in (1, 2):
                for e in ENGS:
                    for i in reversed(rstream[r][e]):
                        if not ins[i]["dma"]:
                            force_signal.add(i)
                            break
        dma_j = {}
        nd = {}
        for q in ("sp", "pool"):
            l0 = [i for i in rstream[0][q] if ins[i]["dma"]]
            for j, i in enumerate(l0):
                dma_j[i] = j
                if j >= N:
                    extra.setdefault(i, {})[l0[j - N]] = True
            cnt = [len(l0)]
            for r in (1, 2):
                lr = l0 + [i for i in rstream[r][q] if ins[i]["dma"]]
                for j in range(len(l0), len(lr)):
                    dma_j[lr[j]] = j
                    if j >= N:
                        extra.setdefault(lr[j], {})[lr[j - N]] = True
                cnt.append(len(lr))
            J = (max(cnt) + N - 1) // N * N
            l3 = [i for i in rstream[3][q] if ins[i]["dma"]]
            for n, i in enumerate(l3):
                dma_j[i] = J + n
                if n >= N:
                    extra.setdefault(i, {})[l3[n - N]] = True
            nd[q] = (cnt, J)
        pos = {}
        for e in ENGS:
            n0 = len(rstream[0][e])
            for n, i in enumerate(rstream[0][e]):
                pos[i] = n
            for r in (1, 2):
                for n, i in enumerate(rstream[r][e]):
                    pos[i] = n0 + n
            for n, i in enumerate(rstream[3][e]):
                pos[i] = 10 ** 7 + n
        signal = set(force_signal)
        waits = {}
        if self.flag is not None:
            signal.add(self.flag[0])

        def prune(e, stream, known, known_dma, drop_old=False):
            for i in stream:
                r = ins[i]
                final = []
                best = {}
                alld = dict(r["deps"])
                for d, raw in extra.get(i, {}).items():
                    alld[d] = alld.get(d, False) or raw
                for d, raw in alld.items():
                    rd = ins[d]
                    if drop_old and rd["region"] != 3:
                        continue
                    if rd["dma"]:
                        if d not in known_dma:
                            known_dma.add(d)
                            final.append(d)
                        continue
                    e2 = rd["eng"]
                    if e2 == e and not r["dma"] and (e == "pe" or (not raw and not STRICT_SAME_ENGINE)):
                        continue
                    if known[e2] >= pos[d]:
                        continue
                    if e2 not in best or pos[d] > pos[best[e2]]:
                        best[e2] = d
                for e2, d in best.items():
                    known[e2] = pos[d]
                    final.append(d)
                    signal.add(d)
                waits[i] = final
        for e in ENGS:
            known = {x: -1 for x in ENGS}
            kd = set()
            prune(e, rstream[0][e], known, kd)
            for r in (1, 2):
                prune(e, rstream[r][e], dict(known), set(kd))
            prune(e, rstream[3][e], {x: -1 for x in ENGS}, set(), drop_old=has_branch)
        ev = {}
        cnt_e = {}
        for e in ENGS:
            c = 0
            cs = {}
            for i in rstream[0][e]:
                if not ins[i]["dma"] and i in signal:
                    c += 1
                    ev[i] = (sems[e], c)
            cs[0] = c
            for r in (1, 2):
                c = cs[0]
                for i in rstream[r][e]:
                    if not ins[i]["dma"] and i in signal:
                        c += 1
                        ev[i] = (sems[e], c)
                cs[r] = c
            c = max(cs[1], cs[2])
            cs["t"] = c
            for i in rstream[3][e]:
                if not ins[i]["dma"] and i in signal:
                    c += 1
                    ev[i] = (sems[e], c)
            cnt_e[e] = cs
        for i, j in dma_j.items():
            ev[i] = (dma_sems[ins[i]["eng"]][j % N], 16 * (j // N + 1))
        self.stats = {e: (sum(len(rstream[r][e]) for r in range(4)), cnt_e[e]) for e in ENGS}
        self.nwaits = sum(len(v) for v in waits.values())
        self.first_sig = {e: [(i, ins[i]['wk'], ins[i]['rk']) for i in rstream[0][e] if i in ev and not ins[i]['dma']][:3] for e in ENGS}
        final_waits = [ev[i] for i in self.out_dmas]
        join_waits = []
        if has_branch:
            for e in ENGS:
                if e != "sp":
                    join_waits.append((sems[e], cnt_e[e]["t"]))
            for q in ("sp", "pool"):
                cnt, J = nd[q]
                if J:
                    for s_ in range(N):
                        join_waits.append((dma_sems[q][s_], 16 * (J // N)))

        def run_stream(e, handle, stream):
            for i in stream:
                r = ins[i]
                for d in waits[i]:
                    s, v = ev[d]
                    handle.wait_ge(s, v)
                bi = r["fn"](handle)
                if r["dma"]:
                    bi.then_inc(ev[i][0], 16)
                elif i in signal:
                    bi.then_inc(sems[e], 1)

        def pads(e, handle, r):
            if e != "sp":
                cs = cnt_e[e]
                if cs[r] > cs[0]:
                    handle.wait_ge(sems[e], cs[r])
                if cs["t"] > cs[r]:
                    handle.sem_inc(sems[e], cs["t"] - cs[r])
            if e in nd:
                cnt, J = nd[e]
                n = cnt[r]
                for s_ in range(N):
                    real = 16 * len([j for j in range(n) if j % N == s_])
                    if real:
                        handle.wait_ge(dma_sems[e][s_], real)
                    if 16 * (J // N) > real:
                        handle.sem_inc(dma_sems[e][s_], 16 * (J // N) - real)

        def run(e, handle):
            run_stream(e, handle, rstream[0][e])
            if has_branch:
                fw, fap = self.flag
                s, v = ev[fw]
                handle.wait_ge(s, v)
                handle.reg_load(regs[e], fap)
                import os as _os
                with handle.If_eq(regs[e], int(_os.environ.get('KFLAGCMP', '0'))):
                    run_stream(e, handle, rstream[1][e])
                    pads(e, handle, 1)
                with handle.Else():
                    run_stream(e, handle, rstream[2][e])
                    pads(e, handle, 2)
                for s, v in join_waits:
                    handle.wait_ge(s, v)
                run_stream(e, handle, rstream[3][e])
            if e == "sp":
                for s, v in final_waits:
                    handle.wait_ge(s, v)

        block.tensor(lambda h: run("pe", h))
        block.scalar(lambda h: run("act", h))
        block.vector(lambda h: run("dve", h))
        block.gpsimd(lambda h: run("pool", h))
        block.sync(lambda h: run("sp", h))


class Builder:
    def __init__(self, nc, arena_bytes):
        self.nc = nc
        self.s = Sched()
        self.arena_bytes = arena_bytes
        self.top = 0
        self.uid = 0
        self.ps_i = 0

    def setup(self, stack):
        nc = self.nc
        self.arena = stack.enter_context(nc.sbuf_tensor("arena", [128, self.arena_bytes], U8))
        self.psum = [stack.enter_context(nc.psum_tensor("ps%d" % i, [128, 512], F32)) for i in range(8)]
        self.psum_t = [Tile("ps%d" % i, self.psum[i][:, :]) for i in range(8)]
        self.psum_bf = [Tile("ps%d" % i, self.psum[i].bitcast(BF16)[:, :]) for i in range(8)]

    def alloc(self, name, shape, dtype, subs=None, parts=128):
        n = int(np.prod(shape)) * DSIZE[dtype]
        n = (n + 31) // 32 * 32
        off = self.top
        self.top += n
        assert self.top <= self.arena_bytes, (name, self.top)
        self.maxtop = max(getattr(self, "maxtop", 0), self.top)
        ap = self.arena[0:parts, off:off + int(np.prod(shape)) * DSIZE[dtype]].bitcast(dtype)
        if len(shape) == 2:
            ap = ap.rearrange("p (a b) -> p a b", a=shape[0])
        elif len(shape) == 3:
            ap = ap.rearrange("p (a b c) -> p a b c", a=shape[0], b=shape[1])
        elif len(shape) == 4:
            ap = ap.rearrange("p (a b c d) -> p a b c d", a=shape[0], b=shape[1], c=shape[2])
        self.uid += 1
        return Tile("%s#%d" % (name, self.uid), ap, subs)

    def alloc_at(self, name, off, shape, dtype, subs=None):
        top = self.top
        self.top = off
        t = self.alloc(name, shape, dtype, subs)
        self.top = top
        return t

    def phase(self, base):
        self.s.barrier()
        self.top = base

    def mark(self):
        return self.top

    def release(self, mark):
        self.s.barrier()
        self.top = mark

    ps_rot = list(range(8))

    def ps(self, bf=False):
        self.ps_i = (self.ps_i + 1) % len(self.ps_rot)
        i = self.ps_rot[self.ps_i]
        return (self.psum_bf if bf else self.psum_t)[i]

    def ps_fixed(self, i, bf=False):
        return (self.psum_bf if bf else self.psum_t)[i]

    def dma(self, out, in_, eng="sp", out_keys=(), in_keys=(), prefetch=False, is_out=False):
        oa = out.ap if isinstance(out, V) else out
        ia = in_.ap if isinstance(in_, V) else in_
        ok = out.keys if isinstance(out, V) else tuple(out_keys)
        ik = in_.keys if isinstance(in_, V) else tuple(in_keys)
        try:
            nb = oa.partition_size() * oa.free_size() * DSIZE[oa.dtype]
        except Exception:
            nb = 512 * 1024
        i = self.s.op(eng, lambda h: h.dma_start(out=oa, in_=ia), reads=ik, writes=ok, dma=True,
                      prefetch=prefetch, cost=(0.2 if eng == "sp" else 1.2),
                      lat=2.5 + nb / (60e3 if eng == "pool" else 150e3))
        if is_out:
            self.s.out_dmas.append(i)
        return i

    def mm(self, out, lhsT, rhs, start=True, stop=True):
        n = rhs.ap.free_size()
        c = max(64, n) / 2400.0 * (4 if rhs.ap.dtype == F32 else 1) + 0.03
        self.s.op("pe", lambda h: h.matmul(out.ap, lhsT.ap, rhs.ap, start=start, stop=stop),
                  reads=lhsT.keys + rhs.keys, writes=out.keys, cost=c)

    def transpose(self, out, in_, ident):
        self.s.op("pe", lambda h: h.transpose(out.ap, in_.ap, ident.ap),
                  reads=in_.keys + ident.keys, writes=out.keys, cost=0.1)

    def act(self, out, in_, func, bias=None, scale=1.0, accum=None, eng="act"):
        reads = in_.keys
        kw = {}
        if isinstance(bias, V):
            reads = reads + bias.keys
            kw["bias"] = bias.ap
        elif bias is not None:
            kw["bias"] = bias
        if isinstance(scale, V):
            reads = reads + scale.keys
            kw["scale"] = scale.ap
        else:
            kw["scale"] = scale
        writes = out.keys
        if accum is not None:
            writes = writes + accum.keys
            kw["accum_out"] = accum.ap
        self.s.op(eng, lambda h: h.activation(out.ap, in_.ap, func, **kw), reads=reads, writes=writes,
                  cost=0.25 + in_.ap.free_size() / 1200.0)

    def tt(self, out, in0, in1, op, eng="dve"):
        self.s.op(eng, lambda h: h.tensor_tensor(out.ap, in0.ap, in1.ap, op),
                  reads=in0.keys + in1.keys, writes=out.keys, cost=self.vcost(eng, out))

    def ts(self, out, in0, s1, op0, s2=None, op1=None, eng="dve", accum=None):
        reads = in0.keys
        a1 = s1
        a2 = s2
        if isinstance(s1, V):
            reads = reads + s1.keys
            a1 = s1.ap
        if isinstance(s2, V):
            reads = reads + s2.keys
            a2 = s2.ap
        kw = {}
        writes = out.keys
        if op1 is not None:
            kw["op1"] = op1
        if accum is not None:
            kw["accum_out"] = accum.ap
            writes = writes + accum.keys
        self.s.op(eng, lambda h: h.tensor_scalar(out.ap, in0.ap, a1, a2, op0, **kw), reads=reads, writes=writes,
                  cost=self.vcost(eng, out))

    def stt(self, out, in0, scalar, in1, op0, op1, eng="dve"):
        reads = in0.keys + in1.keys
        sc = scalar
        if isinstance(scalar, V):
            reads = reads + scalar.keys
            sc = scalar.ap
        self.s.op(eng, lambda h: h.scalar_tensor_tensor(out.ap, in0.ap, sc, in1.ap, op0, op1),
                  reads=reads, writes=out.keys, cost=self.vcost(eng, out))

    def copy(self, out, in_, eng="dve"):
        if eng == "act":
            self.s.op("act", lambda h: h.copy(out.ap, in_.ap), reads=in_.keys, writes=out.keys,
                      cost=0.25 + in_.ap.free_size() / 1200.0)
        else:
            self.s.op(eng, lambda h: h.tensor_copy(out.ap, in_.ap), reads=in_.keys, writes=out.keys,
                      cost=self.vcost(eng, out))

    def reduce(self, out, in_, op, axis=AX.X, eng="dve"):
        self.s.op(eng, lambda h: h.tensor_reduce(out.ap, in_.ap, axis, op), reads=in_.keys, writes=out.keys,
                  cost=self.vcost(eng, in_))

    def recip(self, out, in_):
        self.s.op("dve", lambda h: h.reciprocal(out.ap, in_.ap), reads=in_.keys, writes=out.keys,
                  cost=self.vcost("dve", out))

    def memset(self, out, val, eng="pool"):
        self.s.op(eng, lambda h: h.memset(out.ap, val), reads=(), writes=out.keys, cost=self.vcost(eng, out))

    @staticmethod
    def vcost(eng, v):
        n = v.ap.free_size()
        return (0.1 + n / 960.0) if eng == "dve" else (0.2 + n / 500.0)


def bcast(ap, shape_steps):
    return bass.AP(ap.tensor, ap.offset, [list(ap.ap[0])] + [list(x) for x in shape_steps])


SL = slice(None)


def ts_(i, n=128):
    return slice(i * n, (i + 1) * n)


class Ctx:
    pass


def phase_A(B, C):
    B.phase(34 * 1024)
    xt = [B.alloc("xt%d" % i, [D], F32) for i in range(2)]
    xn = [B.alloc("xn%d" % i, [D], BF16) for i in range(2)]
    junk = B.alloc("junk", [D], BF16)
    ss = [B.alloc("ss%d" % i, [1], F32) for i in range(2)]
    rs = [B.alloc("rs%d" % i, [1], F32) for i in range(2)]
    for i in range(NT):
        b = i % 2
        B.dma(xt[b].v(), C.x[ts_(i), :])
        B.act(junk.v(), xt[b].v(), AF.Square, accum=ss[b].v())
        B.act(rs[b].v(), ss[b].v(), AF.Sqrt, bias=C.eps.v(), scale=1.0 / D)
        B.recip(rs[b].v(), rs[b].v())
        B.ts(xn[b].v(), xt[b].v(), rs[b].v(), ALU.mult)
        p = B.ps(bf=True)
        for c in range(8):
            B.transpose(p.v((SL, ts_(c))), xn[b].v((SL, ts_(c))), C.ident_b.v())
        pin = V(p.ap[:, 0:1024].rearrange("p (c t) -> p c t", c=8), p.v().keys)
        gb = V(bcast(C.g1.ap, [[1, 8], [0, 128]]), C.g1.v().keys)
        B.tt(C.hT.v((SL, SL, ts_(i)), sub=i), pin, gb, ALU.mult)


def phase_B(B, C):
    hT = C.hT
    B.top = 50 * 1024
    wqkv = B.alloc("wqkv", [8, 1536], BF16, subs=[0, 1, 2])
    for j in range(3):
        B.dma(wqkv.v((SL, SL, ts_(j, 512)), sub=j),
              C.w_in[:, j * 512:(j + 1) * 512].rearrange("(k p) n -> p k n", p=128), eng="pool")
    qa = [B.alloc("qa%d" % h, [S], BF16, subs=["d", "s", "m"]) for h in range(8)]
    ka = [B.alloc("ka%d" % h, [S], BF16, subs=["d", "s"]) for h in range(8)]
    va_e = B.alloc("va_e", [NT, 4, 65], BF16, subs=list(range(NT)) + ["one"])
    va_o = B.alloc("va_o", [NT, 4, 128], BF16, subs=list(range(NT)) + ["one"])
    ksum = B.alloc("ksum", [8, 8], F32, subs=list(range(8)))
    KM = B.alloc("KM", [8, 8], BF16, subs=list(range(8)))
    MBT = B.alloc("MBT", [S], BF16, subs=list(range(NT)))
    tri = B.alloc("tri", [128], F32)
    ones_f = B.alloc("ones_f", [128], F32)
    B.dma(tri.v(), C.dram["tri_kq"])
    B.memset(ones_f.v(), 1.0)
    B.memset(va_e.v((SL, SL, SL, slice(64, 65)), sub="one"), 1.0)
    B.memset(va_o.v((SL, SL, SL, slice(0, 64)), sub="one"), 0.0)
    B.memset(va_o.v((SL, SL, SL, slice(0, 1)), sub="one"), 1.0)

    def dpart(h):
        return slice(0, 64) if h % 2 == 0 else slice(64, 128)

    def kpart(h):
        return slice(0, 76) if h % 2 == 0 else slice(0, 128)
    for h in range(8):
        a0 = 64 if h % 2 == 0 else 0
        if h % 2 == 1:
            B.memset(qa[h].v((slice(0, 64), SL), sub=["s", "m"]), 0.0)
            B.memset(ka[h].v((slice(0, 64), SL), sub="s"), 0.0)
        B.dma(qa[h].v((slice(a0, a0 + 4), SL), sub="s"), C.dram["qaug_c"][h], eng="pool")
        B.dma(ka[h].v((slice(a0, a0 + 12), SL), sub="s"), C.dram["kaug_c"], eng="pool")

    for hp in range(4):
        for tc in range(4):
            tl = range(4 * tc, 4 * tc + 4)
            p = B.ps()
            for k in range(8):
                B.mm(p.v(), wqkv.v((SL, k, ts_(hp)), sub=0), hT.v((SL, k, ts_(tc, 512)), sub=tl),
                     start=(k == 0), stop=(k == 7))
            for par in range(2):
                h = 2 * hp + par
                B.copy(qa[h].v((dpart(h), ts_(tc, 512)), sub="d"), p.v((dpart(h), SL)), eng=("act" if par == 0 else "dve"))
            p = B.ps()
            for k in range(8):
                B.mm(p.v(), wqkv.v((SL, k, slice(512 + hp * 128, 512 + hp * 128 + 128)), sub=1),
                     hT.v((SL, k, ts_(tc, 512)), sub=tl), start=(k == 0), stop=(k == 7))
            for par in range(2):
                h = 2 * hp + par
                for bb in range(2):
                    B.act(ka[h].v((dpart(h), slice(tc * 512 + bb * 256, tc * 512 + bb * 256 + 256)), sub="d"),
                          p.v((dpart(h), ts_(bb, 256))), AF.Copy,
                          accum=ksum.v((dpart(h), h, slice(2 * tc + bb, 2 * tc + bb + 1)), sub=h))
        for par in range(2):
            h = 2 * hp + par
            B.act(KM.v((dpart(h), h, SL), sub=h), ksum.v((dpart(h), h, SL), sub=h), AF.Copy, scale=1.0 / 256)

    if C.stop == 'B1':
        return
    for i in range(NT):
        p = B.ps()
        for k in range(8):
            B.mm(p.v(), hT.v((SL, k, ts_(i)), sub=i), wqkv.v((SL, k, slice(1024, 1536)), sub=2),
                 start=(k == 0), stop=(k == 7))
        pv4 = p.ap.rearrange("p (a b d) -> p a b d", a=4, b=2)
        B.copy(va_e.v((SL, i, SL, slice(0, 64)), sub=i), V(pv4[:, :, 0, :], p.v().keys), eng="dve")
        B.copy(va_o.v((SL, i, SL, slice(64, 128)), sub=i), V(pv4[:, :, 1, :], p.v().keys), eng="pool" if False else "dve")

    if C.stop == 'B2':
        return
    Gs = [B.alloc("Gs%d" % i, [64], F32) for i in range(2)]
    cmpt = [B.alloc("cmp%d" % i, [512], F32) for i in range(2)]
    rank = [B.alloc("rank%d" % i, [64], F32) for i in range(2)]
    mb = [B.alloc("mb%d" % i, [64], BF16) for i in range(2)]
    B.memset(MBT.v((slice(0, 64), slice(0, 256)), sub=[0, 1]), 0.0)
    for i in range(2, NT):
        b = i // 2
        u = i % 2
        gpe = B.ps()
        gpo = B.ps()
        for h in range(8):
            gp = gpe if h % 2 == 0 else gpo
            B.mm(gp.v((SL, slice((h // 2) * 8, (h // 2) * 8 + 8))), qa[h].v((dpart(h), ts_(i)), sub="d"),
                 KM.v((dpart(h), h, SL), sub=h))
        g4 = Gs[u].ap.rearrange("p (a b j) -> p a b j", a=4, b=2)
        B.copy(V(g4[:, :, 0, :], Gs[u].v().keys), V(gpe.ap[:, 0:32].rearrange("p (a j) -> p a j", a=4), gpe.v().keys), eng="act")
        B.copy(V(g4[:, :, 1, :], Gs[u].v().keys), V(gpo.ap[:, 0:32].rearrange("p (a j) -> p a j", a=4), gpo.v().keys), eng="act")
        gk = Gs[u].v().keys
        in0 = V(bcast(Gs[u].ap, [[8, 8], [0, b], [1, b]]), gk)
        in1 = V(bcast(Gs[u].ap, [[8, 8], [1, b], [0, b]]), gk)
        co = V(bcast(cmpt[u].ap, [[b * b, 8], [b, b], [1, b]]), cmpt[u].v().keys)
        B.tt(co, in0, in1, ALU.is_gt)
        ro = V(bcast(rank[u].ap, [[8, 8], [1, b]]), rank[u].v().keys)
        B.reduce(ro, co, ALU.add)
        B.memset(mb[u].v(), 0.0)
        mo = V(bcast(mb[u].ap, [[8, 8], [1, b]]), mb[u].v().keys)
        B.ts(mo, ro, 3.0, ALU.is_ge, NEG, ALU.mult)
        pt = B.ps(bf=True)
        B.transpose(pt.v((slice(0, 64), slice(0, 128))), mb[u].v(), C.ident_b.v())
        B.copy(MBT.v((slice(0, 64), ts_(i)), sub=i), pt.v((slice(0, 64), slice(0, 128))), eng="act")
    for h in range(8):
        a0 = 68 if h % 2 == 0 else 4
        B.dma(qa[h].v((slice(a0, a0 + 8), SL), sub="m"), MBT.v((slice(h * 8, h * 8 + 8), SL)))

    if C.stop == 'B3':
        return
    B.ps_rot = [0, 1, 2, 3, 4, 5]
    PT = [B.alloc("PT%d" % i, [512], BF16) for i in range(6)]
    tmp = [B.alloc("tmpd%d" % i, [128], F32) for i in range(3)]
    rden = [B.alloc("rden%d" % i, [512], F32) for i in range(2)]
    bcs = [B.alloc("bcs%d" % i, [512], F32) for i in range(2)]
    it = 0
    hq = 0
    for h in range(8):
        hp, par = h // 2, h % 2
        yp = dpart(h)
        dn = slice(64, 65) if par == 0 else slice(0, 1)
        op_ = slice(0, 65) if par == 0 else slice(0, 128)
        for Q in range(4):
            po = B.ps_fixed(6 + hq % 2)
            nk = 4 * (Q + 1)
            for kt in range(nk):
                m = kt - 4 * Q
                c0 = max(m, 0) * 128
                sp_ = B.ps()
                B.mm(sp_.v((SL, slice(c0, 512))), ka[h].v((kpart(h), ts_(kt))),
                     qa[h].v((kpart(h), slice(Q * 512 + c0, (Q + 1) * 512))))
                pt = PT[it % 6]
                if m >= 0:
                    t_ = tmp[it % 3]
                    B.tt(t_.v(), sp_.v((SL, slice(c0, c0 + 128))), tri.v(), ALU.add)
                    B.act(pt.v((SL, slice(c0, c0 + 128))), t_.v(), AF.Exp, scale=0.125)
                    if c0 + 128 < 512:
                        B.act(pt.v((SL, slice(c0 + 128, 512))), sp_.v((SL, slice(c0 + 128, 512))), AF.Exp, scale=0.125)
                else:
                    B.act(pt.v(), sp_.v(), AF.Exp, scale=0.125)
                vv = (va_e if par == 0 else va_o).v((SL, kt, hp, SL), sub=[kt, "one"])
                B.mm(po.v((op_, slice(c0, 512))), vv, pt.v((SL, slice(c0, 512))), start=(kt == 0), stop=(kt == nk - 1))
                it += 1
            rd = rden[hq % 2]
            B.recip(rd.v((dn, SL)), po.v((dn, SL)))
            pb = B.ps()
            if par == 0:
                B.mm(pb.v((slice(0, 64), SL)), ones_f.v((dn, slice(0, 64))), rd.v((dn, SL)))
            else:
                B.mm(pb.v(), ones_f.v((dn, SL)), rd.v((dn, SL)))
            bc = bcs[hq % 2]
            B.copy(bc.v((yp, SL)), pb.v((yp, SL)), eng="act")
            B.tt(C.yaT.v((yp, hp, ts_(Q, 512)), sub=h), po.v((yp, SL)), bc.v((yp, SL)), ALU.mult)
            hq += 1
    B.ps_rot = list(range(8))


def phase_C(B, C):
    hT = C.hT
    B.phase(82 * 1024)
    dr = C.dram
    wxbc = B.alloc("wxbc", [8, 1536], BF16, subs=list(range(6)))
    for j in range(6):
        B.dma(wxbc.v((SL, SL, ts_(j, 256)), sub=j),
              C.w_in[:, OFF_XBC + j * 256:OFF_XBC + (j + 1) * 256].rearrange("(k p) n -> p k n", p=128), eng="pool")
    wz = B.alloc("wz", [8, 1024], BF16, subs=[0, 1])
    for j in range(2):
        B.dma(wz.v((SL, SL, ts_(j, 512)), sub=j),
              C.w_in[:, OFF_Z + j * 512:OFF_Z + (j + 1) * 512].rearrange("(k p) n -> p k n", p=128), eng="pool")
    wdt = B.alloc("wdt", [8, 16], BF16)
    B.dma(wdt.v(), C.w_in[:, OFF_DT:OFF_DT + 16].rearrange("(k p) n -> p k n", p=128), eng="pool")
    Wt = B.alloc("Wt", [384], F32)
    W2 = B.alloc("W2", [384], F32)
    Esel = B.alloc("Esel", [16], F32)
    cw = B.alloc("cw", [12, 4], F32)
    cb = B.alloc("cb", [12], F32)
    dtb = B.alloc("dtb", [16], F32)
    A_bc = B.alloc("A_bc", [16], F32)
    dsk = B.alloc("dsk", [16], F32)
    ng = B.alloc("ng", [1024], F32)
    one_c = B.alloc("one_c", [1], F32)
    tri_b = B.alloc("tri_b", [128], BF16)
    B.dma(Wt.v(), dr["W_tri"])
    B.dma(W2.v(), dr["W_tri2"])
    B.dma(Esel.v((slice(0, 16), SL)), dr["Esel"])
    B.dma(cw.v(), dr["conv_w"])
    B.dma(cb.v(), dr["conv_b"])
    B.dma(dtb.v(), dr["dt_bias"])
    B.dma(A_bc.v(), dr["a_log"])
    B.dma(dsk.v(), dr["d_skip"])
    B.dma(ng.v(), dr["ssd_norm_g"])
    B.dma(tri_b.v(), dr["tri_kq"], eng="pool")
    B.memset(one_c.v(), 1.0)
    B.act(A_bc.v(), A_bc.v(), AF.Exp)
    B.ts(A_bc.v(), A_bc.v(), -1.0, ALU.mult)

    xr = [B.alloc("xr%d" % i, [259], F32) for i in range(3)]
    hist = B.alloc("hist", [12, 3], F32, subs=list(range(12)))
    acc = [B.alloc("acc%d" % i, [256], F32) for i in range(3)]
    xc = [B.alloc("xc%d" % i, [256], BF16) for i in range(3)]
    BT_l = [B.alloc("BT%d" % i, [2, 256], BF16, subs=[0, 1]) for i in range(2)]
    CT_l = [B.alloc("CT%d" % i, [2, 256], BF16, subs=[0, 1]) for i in range(2)]
    xs_tok_l = [B.alloc("xs_tok%d" % i, [2, 1024], BF16, subs=list(range(8))) for i in range(2)]
    B_tok_l = [B.alloc("B_tok%d" % i, [2, 2, 128], BF16, subs=[0, 1]) for i in range(2)]
    sz_l = [B.alloc("sz%d" % i, [2, 1024], BF16, subs=["%d%d" % (a, b) for a in range(2) for b in range(2)])
            for i in range(2)]
    xd_l = [B.alloc("xd%d" % i, [2, 16], F32) for i in range(2)]
    dt_l = [B.alloc("dt%d" % i, [2, 16], F32) for i in range(2)]
    da_l = [B.alloc("da%d" % i, [2, 16], F32) for i in range(2)]
    csT_l = [B.alloc("csT%d" % i, [256], F32) for i in range(2)]
    ncs_l = [B.alloc("ncs%d" % i, [2, 16], F32) for i in range(2)]
    ecs_l = [B.alloc("ecs%d" % i, [2, 16], F32) for i in range(2)]
    d2e_l = [B.alloc("d2e%d" % i, [2, 16], F32) for i in range(2)]
    dec_l = [B.alloc("dec%d" % i, [16], F32) for i in range(2)]
    xdt = B.alloc("xdt", [2, 1024], BF16)
    xdtd = B.alloc("xdtd", [2, 1024], BF16)
    CBT = B.alloc("CBT", [2, 384], F32, subs=[0, 1])
    LT = [B.alloc("LT%d" % i, [384], F32) for i in range(3)]
    MT = [B.alloc("MT%d" % i, [384], BF16) for i in range(4)]
    hst = B.alloc("hst", [1024], F32, subs=[0, 1])
    hsb = B.alloc("hsb", [1024], BF16, subs=[0, 1])
    t1_l = [B.alloc("t1_%d" % i, [1024], F32, subs=[0, 1]) for i in range(2)]
    u1_l = [B.alloc("u1_%d" % i, [1024], F32) for i in range(2)]
    junk = B.alloc("junkc", [512], BF16)
    ssq = B.alloc("ssq", [2], F32)
    rsd = B.alloc("rsd", [2], F32)
    ybk = B.alloc("ybk", [1024], BF16)

    B.ps_rot = [0, 1, 2, 3]
    B.memset(hist.v(), 0.0)
    itc = 0
    for c in range(8):
        T0 = 256 * c
        tiles = [2 * c, 2 * c + 1]
        BT, CT, xs_tok, B_tok, sz = BT_l[c % 2], CT_l[c % 2], xs_tok_l[c % 2], B_tok_l[c % 2], sz_l[c % 2]
        xd, dt, da, csT, ncs, ecs, d2e, dec = (xd_l[c % 2], dt_l[c % 2], da_l[c % 2], csT_l[c % 2], ncs_l[c % 2],
                                               ecs_l[c % 2], d2e_l[c % 2], dec_l[c % 2])
        for cc in range(12):
            p = B.ps()
            for k in range(8):
                B.mm(p.v((SL, slice(0, 256))), wxbc.v((SL, k, ts_(cc)), sub=cc // 2),
                     hT.v((SL, k, slice(T0, T0 + 256)), sub=tiles), start=(k == 0), stop=(k == 7))
            xr_ = xr[itc % 3]
            a_ = acc[itc % 3]
            x_ = xc[itc % 3]
            itc += 1
            B.copy(xr_.v((SL, slice(0, 3))), hist.v((SL, cc, SL), sub=cc), eng="pool")
            B.copy(xr_.v((SL, slice(3, 259))), p.v((SL, slice(0, 256))), eng="act")
            B.copy(hist.v((SL, cc, SL), sub=cc), xr_.v((SL, slice(256, 259))), eng="pool")
            B.ts(a_.v(), xr_.v((SL, slice(0, 256))), cw.v((SL, cc, slice(0, 1))), ALU.mult, 0.0, ALU.add,
                 eng="pool")
            for j in range(1, 4):
                B.stt(a_.v(), xr_.v((SL, slice(j, j + 256))), cw.v((SL, cc, slice(j, j + 1))), a_.v(),
                      ALU.mult, ALU.add)
            if cc < 8:
                B.act(x_.v(), a_.v(), AF.Silu, bias=cb.v((SL, slice(cc, cc + 1))))
                pt = B.ps(bf=True)
                for st in range(2):
                    B.transpose(pt.v((SL, ts_(st))), x_.v((SL, ts_(st))), C.ident_b.v())
                pin = V(pt.ap[:, 0:256].rearrange("p (s t) -> p s t", s=2), pt.v().keys)
                B.copy(xs_tok.v((SL, SL, ts_(cc)), sub=cc), pin, eng="dve")
            elif cc < 10:
                g = cc - 8
                B.act(BT.v((SL, g, SL), sub=g), a_.v(), AF.Silu, bias=cb.v((SL, slice(cc, cc + 1))))
                pt = B.ps(bf=True)
                for st in range(2):
                    B.transpose(pt.v((SL, ts_(st))), BT.v((SL, g, ts_(st)), sub=g), C.ident_b.v())
                pin = V(pt.ap[:, 0:256].rearrange("p (s t) -> p s t", s=2), pt.v().keys)
                B.copy(B_tok.v((SL, SL, g, SL), sub=g), pin, eng="dve")
            else:
                g = cc - 10
                B.act(CT.v((SL, g, SL), sub=g), a_.v(), AF.Silu, bias=cb.v((SL, slice(cc, cc + 1))))
        for st in range(2):
            for hf in range(2):
                p = B.ps()
                for k in range(8):
                    B.mm(p.v(), hT.v((SL, k, ts_(tiles[st])), sub=tiles[st]), wz.v((SL, k, ts_(hf, 512)), sub=hf),
                         start=(k == 0), stop=(k == 7))
                B.act(sz.v((SL, st, ts_(hf, 512)), sub="%d%d" % (st, hf)), p.v(), AF.Silu)
        for st in range(2):
            p = B.ps()
            for k in range(8):
                B.mm(p.v((SL, slice(0, 16))), hT.v((SL, k, ts_(tiles[st])), sub=tiles[st]), wdt.v((SL, k, SL)),
                     start=(k == 0), stop=(k == 7))
            B.tt(xd.v((SL, st, SL)), p.v((SL, slice(0, 16))), dtb.v(), ALU.add)
        B.act(xd.v(), xd.v(), AF.Exp)
        B.act(dt.v(), xd.v(), AF.Ln, bias=one_c.v())
        A2 = V(bcast(A_bc.ap, [[0, 2], [1, 16]]), A_bc.v().keys)
        B.tt(da.v(), dt.v(), A2, ALU.mult)
        pcs = B.ps()
        B.mm(pcs.v((slice(0, 16), slice(0, 256))), da.v((SL, 0, SL)), Wt.v((SL, slice(128, 384))), start=True, stop=False)
        B.mm(pcs.v((slice(0, 16), slice(0, 256))), da.v((SL, 1, SL)), Wt.v((SL, slice(0, 256))), start=False, stop=True)
        B.copy(csT.v((slice(0, 16), SL)), pcs.v((slice(0, 16), slice(0, 256))), eng="act")
        pct = B.ps()
        B.mm(pct.v((SL, slice(0, 16))), Wt.v((SL, slice(128, 256))), da.v((SL, 0, SL)))
        B.mm(pct.v((SL, slice(16, 32))), Wt.v((SL, slice(256, 384))), da.v((SL, 0, SL)), start=True, stop=False)
        B.mm(pct.v((SL, slice(16, 32))), Wt.v((SL, slice(128, 256))), da.v((SL, 1, SL)), start=False, stop=True)
        B.mm(pct.v((SL, slice(32, 48))), W2.v((SL, slice(128, 256))), da.v((SL, 0, SL)), start=True, stop=False)
        B.mm(pct.v((SL, slice(32, 48))), W2.v((SL, slice(0, 128))), da.v((SL, 1, SL)), start=False, stop=True)
        B.mm(pct.v((SL, slice(48, 64))), W2.v((SL, slice(128, 256))), da.v((SL, 1, SL)))
        B.mm(pct.v((SL, slice(64, 80))), Wt.v((SL, slice(256, 384))), da.v((SL, 0, SL)), start=True, stop=False)
        B.mm(pct.v((SL, slice(64, 80))), Wt.v((SL, slice(256, 384))), da.v((SL, 1, SL)), start=False, stop=True)
        B.act(ecs.v(), V(pct.ap[:, 0:32].rearrange("p (s h) -> p s h", s=2), pct.v().keys), AF.Exp)
        B.ts(ncs.v(), V(pct.ap[:, 0:32].rearrange("p (s h) -> p s h", s=2), pct.v().keys), -1.0, ALU.mult)
        B.act(d2e.v(), V(pct.ap[:, 32:64].rearrange("p (s h) -> p s h", s=2), pct.v().keys), AF.Exp)
        B.act(dec.v(), pct.v((SL, slice(64, 80))), AF.Exp)
        for st in range(2):
            xs3 = V(xs_tok.ap[:, st, :].rearrange("p (h d) -> p h d", h=16), xs_tok.v().keys)
            dtb3 = V(bcast(dt.ap[:, st, :], [[1, 16], [0, 64]]), dt.v().keys)
            xo3 = V(xdt.ap[:, st, :].rearrange("p (h d) -> p h d", h=16), xdt.v().keys)
            B.tt(xo3, xs3, dtb3, ALU.mult, eng="pool")
            if c < 7:
                d3 = V(bcast(d2e.ap[:, st, :], [[1, 16], [0, 64]]), d2e.v().keys)
                xo4 = V(xdtd.ap[:, st, :].rearrange("p (h d) -> p h d", h=16), xdtd.v().keys)
                B.tt(xo4, xo3, d3, ALU.mult, eng="pool")
        for g in range(2):
            p = B.ps()
            B.mm(p.v((SL, slice(0, 256))), BT.v((SL, g, slice(0, 128)), sub=g), CT.v((SL, g, SL), sub=g))
            B.mm(p.v((SL, slice(256, 384))), BT.v((SL, g, slice(128, 256)), sub=g), CT.v((SL, g, slice(128, 256)), sub=g))
            B.copy(CBT.v((SL, g, SL), sub=g), p.v((SL, slice(0, 384))), eng="dve")
        for h in range(16):
            g = h // 8
            pd = B.ps()
            eh = V(bcast(Esel.ap[0:16, h:h + 1], [[0, 128]]), Esel.v().keys)
            B.mm(pd.v((SL, slice(0, 256))), eh, csT.v((slice(0, 16), SL)), start=True, stop=False)
            B.mm(pd.v((SL, slice(0, 128))), C.ident_b.v(), tri_b.v(), start=False, stop=True)
            B.mm(pd.v((SL, slice(256, 384))), eh, csT.v((slice(0, 16), slice(128, 256))), start=True, stop=False)
            B.mm(pd.v((SL, slice(256, 384))), C.ident_b.v(), tri_b.v(), start=False, stop=True)
            lt_ = LT[h % 3]
            mt_ = MT[h % 4]
            B.act(lt_.v((SL, slice(0, 256))), pd.v((SL, slice(0, 256))), AF.Exp, bias=ncs.v((SL, 0, slice(h, h + 1))))
            B.act(lt_.v((SL, slice(256, 384))), pd.v((SL, slice(256, 384))), AF.Exp, bias=ncs.v((SL, 1, slice(h, h + 1))))
            B.tt(mt_.v(), lt_.v(), CBT.v((SL, g, SL), sub=g), ALU.mult)
            hc = slice(h * 64, h * 64 + 64)
            y0 = B.ps_fixed(4 + g)
            y1 = B.ps_fixed(6 + g)
            oc = slice((h % 8) * 64, (h % 8) * 64 + 64)
            B.mm(y0.v((SL, oc)), mt_.v((SL, slice(0, 128))), xdt.v((SL, 0, hc)))
            B.mm(y1.v((SL, oc)), mt_.v((SL, slice(128, 256))), xdt.v((SL, 0, hc)), start=True, stop=False)
            B.mm(y1.v((SL, oc)), mt_.v((SL, slice(256, 384))), xdt.v((SL, 1, hc)), start=False, stop=True)
        for lt in range(2):
            u_ = u1_l[lt]
            t1 = t1_l[lt]
            for g in range(2):
                yb_ = B.ps_fixed(4 + 2 * lt + g)
                hs = ts_(g, 512)
                if c > 0:
                    p = B.ps()
                    B.mm(p.v(), CT.v((SL, g, ts_(lt)), sub=g), hsb.v((SL, hs), sub=g))
                    e3 = V(bcast(ecs.ap[:, lt, g * 8:(g + 1) * 8], [[1, 8], [0, 64]]), ecs.v().keys)
                    p3 = V(p.ap.rearrange("p (h d) -> p h d", h=8), p.v().keys)
                    t13 = V(t1.ap[:, hs].rearrange("p (h d) -> p h d", h=8), t1.v(sub=g).keys)
                    B.tt(t13, p3, e3, ALU.mult)
                    B.tt(u_.v((SL, hs)), yb_.v(), t1.v((SL, hs), sub=g), ALU.add)
                else:
                    B.copy(u_.v((SL, hs)), yb_.v(), eng="dve")
            xs3b = V(xs_tok.ap[:, lt, :].rearrange("p (h d) -> p h d", h=16), xs_tok.v().keys)
            dk3 = V(bcast(dsk.ap, [[1, 16], [0, 64]]), dsk.v().keys)
            t1o = V(t1.ap.rearrange("p (h d) -> p h d", h=16), t1.v().keys)
            B.tt(t1o, xs3b, dk3, ALU.mult, eng="pool")
            B.tt(u_.v(), u_.v(), t1.v(), ALU.add, eng="pool")
            B.tt(u_.v(), u_.v(), sz.v((SL, lt, SL), sub=["%d0" % lt, "%d1" % lt]), ALU.mult)
            for g in range(2):
                B.act(junk.v(), u_.v((SL, ts_(g, 512))), AF.Square, accum=ssq.v((SL, slice(g, g + 1))))
            B.act(rsd.v(), ssq.v(), AF.Sqrt, bias=C.eps.v(), scale=1.0 / 512)
            B.recip(rsd.v(), rsd.v())
            for g in range(2):
                B.stt(ybk.v((SL, ts_(g, 512))), u_.v((SL, ts_(g, 512))), rsd.v((SL, slice(g, g + 1))),
                      ng.v((SL, ts_(g, 512))), ALU.mult, ALU.mult)
            pt = B.ps(bf=True)
            for k in range(8):
                B.transpose(pt.v((SL, ts_(k))), ybk.v((SL, ts_(k))), C.ident_b.v())
            pin = V(pt.ap[:, 0:1024].rearrange("p (c t) -> p c t", c=8), pt.v().keys)
            B.copy(C.ybT.v((SL, SL, ts_(tiles[lt])), sub=tiles[lt]), pin, eng="act")
        if c < 7:
            for g in range(2):
                hs = ts_(g, 512)
                p = B.ps()
                for lt in range(2):
                    B.mm(p.v(), B_tok.v((SL, lt, g, SL), sub=g), xdtd.v((SL, lt, hs)), start=(lt == 0), stop=(lt == 1))
                if c > 0:
                    dc3 = V(bcast(dec.ap[:, g * 8:(g + 1) * 8], [[1, 8], [0, 64]]), dec.v().keys)
                    h3 = V(hst.ap[:, hs].rearrange("p (h d) -> p h d", h=8), hst.v(sub=g).keys)
                    B.tt(h3, h3, dc3, ALU.mult)
                    B.tt(hst.v((SL, hs), sub=g), hst.v((SL, hs), sub=g), p.v(), ALU.add)
                else:
                    B.copy(hst.v((SL, hs), sub=g), p.v(), eng="dve")
                B.copy(hsb.v((SL, hs), sub=g), hst.v((SL, hs), sub=g), eng="pool")
    B.ps_rot = list(range(8))


def phase_D(B, C):
    hT, yaT, ybT, mT = C.hT, C.yaT, C.ybT, C.mT
    dr = C.dram
    B.phase(114 * 1024)
    woa = B.alloc("woa", [4, 1024], BF16, subs=[0, 1, 2, 3])
    wob = B.alloc("wob", [8, 1024], BF16, subs=[0, 1, 2, 3])
    wga = B.alloc("wga", [8, 1024], BF16, subs=[0, 1, 2, 3])
    wgb = B.alloc("wgb", [8, 1024], BF16, subs=[0, 1, 2, 3])
    for j in range(4):
        cs_ = slice(j * 256, (j + 1) * 256)
        B.dma(woa.v((SL, SL, cs_), sub=j), dr["w_out_a"][:, cs_].rearrange("(k p) n -> p k n", p=128), eng="pool")
        B.dma(wga.v((SL, SL, cs_), sub=j),
              C.w_in[:, OFF_GA + j * 256:OFF_GA + (j + 1) * 256].rearrange("(k p) n -> p k n", p=128), eng="pool")
        B.dma(wob.v((SL, SL, cs_), sub=j), dr["w_out_b"][:, cs_].rearrange("(k p) n -> p k n", p=128), eng="pool")
        B.dma(wgb.v((SL, SL, cs_), sub=j),
              C.w_in[:, OFF_GB + j * 256:OFF_GB + (j + 1) * 256].rearrange("(k p) n -> p k n", p=128), eng="pool")
    sga = B.alloc("sga", [512], F32)
    sgb = B.alloc("sgb", [512], F32)
    m1 = B.alloc("m1", [512], F32)
    m2 = B.alloc("m2", [512], F32)
    wo = B.alloc_at("wo", 180 * 1024, [8, 1024], BF16, subs=[0, 1])
    assert B.top <= 180 * 1024, B.top
    for j in range(2):
        cs_ = slice(j * 512, (j + 1) * 512)
        B.dma(wo.v((SL, SL, cs_), sub=j), dr["w_out"][:, cs_].rearrange("(k p) n -> p k n", p=128), eng="pool",
              prefetch=True)
    for cc in range(8):
        j = cc // 2
        for tc in range(4):
            tsl = ts_(tc, 512)
            tl = list(range(4 * tc, 4 * tc + 4))
            pga = B.ps()
            for k in range(8):
                B.mm(pga.v(), wga.v((SL, k, ts_(cc)), sub=j), hT.v((SL, k, tsl), sub=tl), start=(k == 0), stop=(k == 7))
            B.act(sga.v(), pga.v(), AF.Sigmoid)
            pa = B.ps()
            for pr in range(4):
                B.mm(pa.v(), woa.v((SL, pr, ts_(cc)), sub=j), yaT.v((SL, pr, tsl), sub=[2 * pr, 2 * pr + 1]),
                     start=(pr == 0), stop=(pr == 3))
            B.tt(m1.v(), pa.v(), sga.v(), ALU.mult)
            pgb = B.ps()
            for k in range(8):
                B.mm(pgb.v(), wgb.v((SL, k, ts_(cc)), sub=j), hT.v((SL, k, tsl), sub=tl), start=(k == 0), stop=(k == 7))
            B.act(sgb.v(), pgb.v(), AF.Sigmoid)
            pb = B.ps()
            for k in range(8):
                B.mm(pb.v(), wob.v((SL, k, ts_(cc)), sub=j), ybT.v((SL, k, tsl), sub=tl), start=(k == 0), stop=(k == 7))
            B.tt(m2.v(), pb.v(), sgb.v(), ALU.mult)
            B.tt(mT.v((SL, cc, tsl), sub=tl), m1.v(), m2.v(), ALU.add, eng="pool")
    B.phase(114 * 1024)
    x1 = C.x1
    xt = [B.alloc("xt%d" % i, [D], F32) for i in range(2)]
    for i in range(NT):
        b = i % 2
        B.dma(xt[b].v(), C.x[ts_(i), :])
        for hf in range(2):
            p = B.ps()
            for k in range(8):
                B.mm(p.v(), mT.v((SL, k, ts_(i)), sub=i), wo.v((SL, k, ts_(hf, 512)), sub=hf), start=(k == 0), stop=(k == 7))
            B.tt(x1.v((SL, i, ts_(hf, 512)), sub=i), p.v(), xt[b].v((SL, ts_(hf, 512))), ALU.add)


def phase_E(B, C):
    x1 = C.x1
    dr = C.dram
    K = 1024
    TG = 6
    CAP = 128 * TG
    H0 = 66 * K + 8 * K * TG
    PM0 = H0 + 32 * K
    SM0 = PM0 + 16 * K
    SC0 = SM0 + 4 * K
    B.phase(SC0)
    h2s = B.alloc_at("h2s", 66 * K, [8, 512 * TG], BF16, subs=list(range(4 * TG)))
    h2_tok = B.alloc_at("h2_tok", H0, [NT, D], BF16, subs=list(range(NT)))
    top0 = B.top
    B.top = SM0
    comb = B.alloc("comb", [NT * 16], F32)
    dest = B.alloc("dest", [NT], F32)
    destm = B.alloc("destm", [TG, NT], F32)
    cgx = B.alloc("cgx", [NT, 8], BF16)
    flag_i = B.alloc("flag_i", [1], I32)
    sel8 = B.alloc("sel8", [4], F32)
    sel8b = B.alloc("sel8b", [4], BF16)
    sid = B.alloc("sid", [4 * TG], F32)
    assert B.top <= SM0 + 2 * K, B.top
    B.top = top0
    B.dma(sel8.v((slice(0, 8), SL)), dr["sel8"])
    B.copy(sel8b.v((slice(0, 8), SL)), sel8.v((slice(0, 8), SL)), eng="dve")
    B.dma(sid.v(), dr["sid"])
    g2 = B.alloc("g2", [8], F32)
    B.dma(g2.v(), dr["ffn_norm_g"])
    g2bc = B.alloc("g2bc", [D], F32)
    B.dma(g2bc.v(), dr["ffn_norm_g_bc"])
    wr = B.alloc("wr", [8, 20], F32)
    B.dma(wr.v((SL, SL, slice(0, 4))), dr["w_rg"].rearrange("(k p) n -> p k n", p=128))
    B.dma(wr.v((SL, SL, slice(4, 20))), dr["w_re"].rearrange("(k p) n -> p k n", p=128))
    br = B.alloc("br", [20], F32)
    B.dma(br.v(), dr["b_r"])
    Wt = B.alloc("WtE", [384], F32)
    B.dma(Wt.v(), dr["W_tri"])
    gidx = B.alloc("gidx", [4], F32)
    B.dma(gidx.v(), dr["gidx4"])
    xn = [B.alloc("xnf%d" % i, [D], F32) for i in range(2)]
    h2f = [B.alloc("h2f%d" % i, [8, 128], F32) for i in range(2)]
    junk = B.alloc("junke", [D], BF16)
    sm = [B.alloc("rsm%d" % i, [2], F32) for i in range(2)]
    LG = B.alloc("LG", [NT, 20], F32, subs=list(range(NT)))
    for i in range(NT):
        b = i % 2
        ssv = sm[b].v((SL, slice(0, 1)))
        rsv = sm[b].v((SL, slice(1, 2)))
        B.act(junk.v(), x1.v((SL, i, SL), sub=i), AF.Square, accum=ssv)
        B.act(rsv, ssv, AF.Sqrt, bias=C.eps.v(), scale=1.0 / D)
        B.recip(rsv, rsv)
        B.ts(xn[b].v(), x1.v((SL, i, SL), sub=i), rsv, ALU.mult)
        B.tt(h2_tok.v((SL, i, SL), sub=i), xn[b].v(), g2bc.v(), ALU.mult, eng="pool")
        for hf in range(2):
            p = B.ps()
            for c4 in range(4):
                c = hf * 4 + c4
                B.transpose(p.v((SL, ts_(c4))), xn[b].v((SL, ts_(c))), C.ident_f.v())
            pin = V(p.ap.rearrange("p (c t) -> p c t", c=4), p.v().keys)
            gb = V(bcast(g2.ap[:, hf * 4:hf * 4 + 4], [[1, 4], [0, 128]]), g2.v().keys)
            B.tt(h2f[b].v((SL, slice(hf * 4, hf * 4 + 4), SL)), pin, gb, ALU.mult)
        pl = B.ps()
        for k in range(8):
            B.mm(pl.v((SL, slice(0, 20))), h2f[b].v((SL, k, SL)), wr.v((SL, k, SL)), start=(k == 0), stop=(k == 7))
        B.tt(LG.v((SL, i, SL), sub=i), pl.v((SL, slice(0, 20))), br.v(), ALU.add)

    def sm_(name, n):
        return B.alloc(name, [NT * n], F32)

    def b3(t, steps, off=0):
        a = t.ap
        return V(bass.AP(a.tensor, a.offset + off, [list(a.ap[0])] + [list(x) for x in steps]), t.v().keys)
    lgk = LG.v().keys
    gmax, gsh, ge, gsum, gpw, goh = sm_("gmax", 1), sm_("gsh", 4), sm_("ge", 4), sm_("gsum", 1), sm_("gpw", 1), sm_("goh", 4)
    tmpg, em, m1, oh1, em2, m2, oh2 = sm_("tmpg", 16), sm_("em", 4), sm_("m1", 1), sm_("oh1", 4), sm_("em2", 4), sm_("m2", 1), sm_("oh2", 4)
    dd, ee, w1, w2, cig, tm2 = sm_("dd", 1), sm_("ee", 1), sm_("w1", 1), sm_("w2", 1), sm_("cig", 4), sm_("tm2", 4)
    gl3 = V(bcast(LG.ap[:, 0, 0:1], [[20, NT], [1, 4]]), lgk)
    B.reduce(gmax.v(), gl3, ALU.max)
    B.tt(b3(gsh, [[4, NT], [1, 4]]), gl3, b3(gmax, [[1, NT], [0, 4]]), ALU.subtract)
    B.act(ge.v(), gsh.v(), AF.Exp)
    B.reduce(gsum.v(), b3(ge, [[4, NT], [1, 4]]), ALU.add)
    B.recip(gpw.v(), gsum.v())
    B.ts(goh.v(), gsh.v(), 0.0, ALU.is_ge)
    el3 = V(bcast(LG.ap[:, 0, 4:5], [[20, NT], [1, 4], [4, 4]]), lgk)
    B.tt(b3(tmpg, [[16, NT], [4, 4], [1, 4]]), el3, b3(goh, [[4, NT], [0, 4], [1, 4]]), ALU.mult)
    B.reduce(b3(em, [[4, NT], [1, 4]]), b3(tmpg, [[16, NT], [4, 4], [1, 4]]), ALU.add)
    B.reduce(m1.v(), b3(em, [[4, NT], [1, 4]]), ALU.max)
    B.tt(b3(oh1, [[4, NT], [1, 4]]), b3(em, [[4, NT], [1, 4]]), b3(m1, [[1, NT], [0, 4]]), ALU.is_ge)
    B.stt(em2.v(), oh1.v(), -1e30, em.v(), ALU.mult, ALU.add)
    B.reduce(m2.v(), b3(em2, [[4, NT], [1, 4]]), ALU.max)
    B.tt(b3(oh2, [[4, NT], [1, 4]]), b3(em2, [[4, NT], [1, 4]]), b3(m2, [[1, NT], [0, 4]]), ALU.is_ge)
    B.tt(dd.v(), m2.v(), m1.v(), ALU.subtract)
    B.act(ee.v(), dd.v(), AF.Exp)
    B.ts(w1.v(), ee.v(), 1.0, ALU.add)
    B.recip(w1.v(), w1.v())
    B.tt(w2.v(), ee.v(), w1.v(), ALU.mult)
    B.tt(w1.v(), w1.v(), gpw.v(), ALU.mult)
    B.tt(w2.v(), w2.v(), gpw.v(), ALU.mult)
    B.tt(b3(cig, [[4, NT], [1, 4]]), b3(oh1, [[4, NT], [1, 4]]), b3(w1, [[1, NT], [0, 4]]), ALU.mult)
    B.tt(b3(tm2, [[4, NT], [1, 4]]), b3(oh2, [[4, NT], [1, 4]]), b3(w2, [[1, NT], [0, 4]]), ALU.mult)
    B.tt(cig.v(), cig.v(), tm2.v(), ALU.add)
    B.tt(b3(comb, [[16, NT], [4, 4], [1, 4]]), b3(goh, [[4, NT], [1, 4], [0, 4]]), b3(cig, [[4, NT], [0, 4], [1, 4]]),
         ALU.mult)
    B.copy(b3(cgx, [[8, NT], [1, 4]]), b3(cig, [[4, NT], [1, 4]]), eng="dve")
    B.copy(b3(tm2, [[4, NT], [1, 4]]), b3(cgx, [[8, NT], [1, 4]]), eng="dve")
    B.tt(tm2.v(), cig.v(), tm2.v(), ALU.subtract)
    B.copy(b3(cgx, [[8, NT], [1, 4]], off=4), b3(tm2, [[4, NT], [1, 4]]), eng="dve")
    pcn = B.ps()
    for i in range(NT):
        for i2 in range(i + 1):
            lhs = Wt.v((SL, slice(256, 384))) if i2 < i else Wt.v((SL, slice(127, 255)))
            B.mm(pcn.v((SL, slice(4 * i, 4 * i + 4))), lhs, goh.v((SL, slice(4 * i2, 4 * i2 + 4))),
                 start=(i2 == 0), stop=(i2 == i))
    cnt = sm_("cnt", 4)
    B.copy(cnt.v(), pcn.v((SL, slice(0, 64))), eng="act")
    rsel, gid, ovm, ov1 = sm_("rsel", 1), sm_("gid", 1), sm_("ovm", 1), B.alloc("ov1", [1], F32)
    B.tt(tmpg.v((SL, slice(0, 64))), goh.v(), cnt.v(), ALU.mult)
    B.reduce(rsel.v(), b3(tmpg, [[4, NT], [1, 4]]), ALU.add)
    B.tt(b3(tmpg, [[4, NT], [1, 4]]), b3(goh, [[4, NT], [1, 4]]), b3(gidx, [[0, NT], [1, 4]]), ALU.mult)
    B.reduce(gid.v(), b3(tmpg, [[4, NT], [1, 4]]), ALU.add)
    B.stt(dest.v(), gid.v(), float(CAP), rsel.v(), ALU.mult, ALU.add)
    for sc in range(TG):
        B.ts(destm.v((SL, sc, SL)), dest.v(), -512.0 * sc, ALU.add)
    B.ts(ovm.v(), rsel.v(), float(CAP), ALU.is_ge)
    B.reduce(ov1.v(), ovm.v(), ALU.max)
    pov = B.ps()
    B.transpose(pov.v((slice(0, 1), slice(0, 128))), ov1.v(), C.ident_f.v())
    ovr = B.alloc("ovr", [128], F32)
    B.copy(ovr.v((slice(0, 1), SL)), pov.v((slice(0, 1), slice(0, 128))), eng="act")
    ov2 = B.alloc("ov2", [1], F32)
    B.reduce(ov2.v((slice(0, 1), SL)), ovr.v((slice(0, 1), SL)), ALU.max)
    fo, fi_ = flag_i.v((slice(0, 1), SL)), ov2.v((slice(0, 1), SL))
    fw = B.s.op("dve", lambda h: h.tensor_copy(fo.ap, fi_.ap), reads=fi_.keys, writes=fo.keys, cost=0.1)

    C.dbgt.update(dest=dest, cgx=cgx, comb=comb, rsel=rsel, gid=gid)
    B.s.begin_branch(fw, flag_i.ap[0:1, 0:1])
    B.top = SC0
    Pm = [B.alloc_at("Pm%d" % i, PM0 + i * K, [512], BF16) for i in range(NT)]
    iota = B.alloc_at("iota", SM0 + 2 * K, [512], F32)
    B.dma(iota.v(), dr["iota512"])
    cTs8 = B.alloc("cTs8", [512 * TG], BF16, subs=list(range(TG)))
    ev_ = 0
    for sc in range(TG):
        for i in range(NT):
            B.ts(Pm[i].v(), iota.v(), destm.v((SL, sc, slice(i, i + 1))), ALU.is_equal)
        for c in range(8):
            p = B.ps()
            for i in range(NT):
                B.mm(p.v(), h2_tok.v((SL, i, ts_(c)), sub=i), Pm[i].v(), start=(i == 0), stop=(i == NT - 1))
            B.copy(h2s.v((SL, c, ts_(sc, 512)), sub=range(4 * sc, 4 * sc + 4)), p.v(), eng=("act" if ev_ % 2 == 0 else "dve"))
            ev_ += 1
        p8 = B.ps()
        for i in range(NT):
            B.mm(p8.v((slice(0, 8), SL)), cgx.v((SL, i, SL)), Pm[i].v(), start=(i == 0), stop=(i == NT - 1))
        B.copy(cTs8.v((slice(0, 8), ts_(sc, 512)), sub=sc), p8.v((slice(0, 8), SL)), eng="act")
    mS = B.mark()
    B.s.barrier()
    wbuf = []
    for u in range(2):
        o = H0 + u * 24 * K
        wbuf.append((B.alloc_at("wgS%d" % u, o, [8, 512], BF16, subs=[0, 1]), B.alloc_at("wuS%d" % u, o + 8 * K, [8, 512], BF16, subs=[0, 1]),
                     B.alloc_at("wdS%d" % u, o + 16 * K, [4, 1024], BF16)))

    def load_expert(e, wb):
        wg_, wu_, wd_ = wb[e % 2]
        for j in range(2):
            B.dma(wg_.v((SL, SL, ts_(j, 256)), sub=j), dr["w_gate"][e][:, j * 256:(j + 1) * 256].rearrange("(k p) n -> p k n", p=128),
                  eng="pool", prefetch=True)
            B.dma(wu_.v((SL, SL, ts_(j, 256)), sub=j), dr["w_up"][e][:, j * 256:(j + 1) * 256].rearrange("(k p) n -> p k n", p=128),
                  eng="pool", prefetch=True)
        B.dma(wd_.v(), dr["w_down"][e].rearrange("(k p) n -> p k n", p=128), eng="pool", prefetch=True)

    load_expert(0, wbuf)
    destT = B.alloc("destT", [128], F32)
    DB = B.alloc("DB", [S], F32, subs=list(range(NT)))
    pdt = B.ps()
    B.transpose(pdt.v((slice(0, 16), slice(0, 128))), dest.v(), C.ident_f.v())
    B.copy(destT.v((slice(0, 16), SL)), pdt.v((slice(0, 16), slice(0, 128))), eng="act")
    for q4 in range(4):
        pdb = B.ps()
        for j in range(4):
            i = 4 * q4 + j
            eh = V(bcast(C.ident_f.ap[0:16, i:i + 1], [[0, 128]]), C.ident_f.v().keys)
            B.mm(pdb.v((SL, ts_(j))), eh, destT.v((slice(0, 16), SL)))
        B.copy(DB.v((SL, ts_(q4, 512)), sub=range(4 * q4, 4 * q4 + 4)), pdb.v(), eng="act")
    ysg = B.alloc("ysg", [TG, D], BF16)
    he = [B.alloc("heS", [4, CAP], BF16)]
    PTt = [B.alloc("PTt%d" % i, [TG, 128], BF16) for i in range(2)]
    sg = [B.alloc("sgS%d" % i, [512], F32) for i in range(2)]
    it = 0
    ip = 0
    ipb = 0
    B.ps_rot = [0, 1, 2, 3, 4, 5]
    for g in range(4):
        gsub = list(range(TG * g, TG * g + TG))
        for el in range(4):
            e = 4 * g + el
            if e + 1 < 16:
                load_expert(e + 1, wbuf)
            wg_, wu_, wd_ = wbuf[e % 2]
            he_ = he[0]
            for (n0, nn) in [(a, min(512, CAP - a)) for a in range(0, CAP, 512)]:
                s0 = g * CAP + n0
                pbc = B.ps_fixed(6 + ipb % 2)
                ipb += 1
                eh = V(bcast(sel8b.ap[0:8, el:el + 1], [[0, 128]]), sel8b.v().keys)
                B.mm(pbc.v((SL, slice(0, nn))), eh, cTs8.v((slice(0, 8), slice(s0, s0 + nn))))
                for fc in range(4):
                    pg = B.ps()
                    for k in range(8):
                        B.mm(pg.v((SL, slice(0, nn))), wg_.v((SL, k, ts_(fc)), sub=fc // 2), h2s.v((SL, k, slice(s0, s0 + nn)), sub=gsub),
                             start=(k == 0), stop=(k == 7))
                    pu = B.ps()
                    for k in range(8):
                        B.mm(pu.v((SL, slice(0, nn))), wu_.v((SL, k, ts_(fc)), sub=fc // 2), h2s.v((SL, k, slice(s0, s0 + nn)), sub=gsub),
                             start=(k == 0), stop=(k == 7))
                    s_ = sg[it % 2]
                    it += 1
                    B.act(s_.v((SL, slice(0, nn))), pg.v((SL, slice(0, nn))), AF.Silu)
                    B.tt(s_.v((SL, slice(0, nn))), pu.v((SL, slice(0, nn))), s_.v((SL, slice(0, nn))), ALU.mult)
                    B.tt(he_.v((SL, fc, slice(n0, n0 + nn))), s_.v((SL, slice(0, nn))), pbc.v((SL, slice(0, nn))), ALU.mult)
            for st in range(TG):
                for hf in range(2):
                    po = B.ps()
                    for fc in range(4):
                        B.mm(po.v(), he_.v((SL, fc, ts_(st))), wd_.v((SL, fc, ts_(hf, 512))), start=(fc == 0), stop=(fc == 3))
                    yv = ysg.v((SL, st, ts_(hf, 512)))
                    if el == 0:
                        B.copy(yv, po.v(), eng="act")
                    else:
                        B.tt(yv, po.v(), yv, ALU.add)
        for i in range(NT):
            pt_ = PTt[ip % 2]
            ip += 1
            d3 = V(bcast(DB.ap[:, i * 128:i * 128 + 1], [[0, TG], [1, 128]]), DB.v(sub=i).keys)
            s3 = V(bcast(sid.ap[:, TG * g:TG * g + 1], [[1, TG], [0, 128]]), sid.v().keys)
            B.tt(pt_.v(), d3, s3, ALU.is_equal)
            for hf in range(2):
                po = B.ps()
                for st in range(TG):
                    B.mm(po.v(), pt_.v((SL, st, SL)), ysg.v((SL, st, ts_(hf, 512))), start=(st == 0), stop=(st == TG - 1))
                xv = x1.v((SL, i, ts_(hf, 512)), sub=i)
                B.tt(xv, po.v(), xv, ALU.add)

    B.s.begin_else()
    B.ps_rot = list(range(8))
    B.top = SC0
    cT = B.alloc("cT", [S], F32, subs=list(range(NT)))
    for i in range(NT):
        p = B.ps(bf=True)
        for c in range(8):
            B.transpose(p.v((SL, ts_(c))), h2_tok.v((SL, i, ts_(c)), sub=i), C.ident_b.v())
        pin = V(p.ap[:, 0:1024].rearrange("p (c t) -> p c t", c=8), p.v().keys)
        B.copy(h2s.v((SL, SL, ts_(i)), sub=i), pin, eng=("act" if i % 2 == 0 else "dve"))
    for q4 in range(4):
        pc = B.ps()
        for j in range(4):
            i = q4 * 4 + j
            B.transpose(pc.v((slice(0, 16), ts_(j))), comb.v((SL, slice(i * 16, i * 16 + 16))), C.ident_f.v())
        B.copy(cT.v((slice(0, 16), ts_(q4, 512)), sub=list(range(4 * q4, 4 * q4 + 4))), pc.v((slice(0, 16), SL)), eng="act")
    B.s.barrier()
    wbufd = []
    for u in range(2):
        o = H0 + u * 24 * K
        wbufd.append((B.alloc_at("wgD%d" % u, o, [8, 512], BF16, subs=[0, 1]), B.alloc_at("wuD%d" % u, o + 8 * K, [8, 512], BF16, subs=[0, 1]),
                      B.alloc_at("wdD%d" % u, o + 16 * K, [4, 1024], BF16)))
    load_expert(0, wbufd)
    B.ps_rot = [0, 1, 2, 3, 4, 5]
    heT = [B.alloc("heT%d" % i, [4, 512], BF16) for i in range(2)]
    sgd = [B.alloc("sg%d" % i, [512], F32) for i in range(2)]
    ttd = [B.alloc("tt%d" % i, [512], F32) for i in range(2)]
    it = 0
    ic = 0
    for e in range(16):
        if e + 1 < 16:
            load_expert(e + 1, wbufd)
        wg_, wu_, wd_ = wbufd[e % 2]
        for tc in range(4):
            tsl = ts_(tc, 512)
            tl = list(range(4 * tc, 4 * tc + 4))
            pbc = B.ps_fixed(6 + ic % 2)
            eh = V(bcast(C.ident_f.ap[0:16, e:e + 1], [[0, 128]]), C.ident_f.v().keys)
            B.mm(pbc.v(), eh, cT.v((slice(0, 16), tsl), sub=tl))
            he_ = heT[ic % 2]
            ic += 1
            for fc in range(4):
                pg = B.ps()
                for k in range(8):
                    B.mm(pg.v(), wg_.v((SL, k, ts_(fc)), sub=fc // 2), h2s.v((SL, k, tsl), sub=tl), start=(k == 0), stop=(k == 7))
                pu = B.ps()
                for k in range(8):
                    B.mm(pu.v(), wu_.v((SL, k, ts_(fc)), sub=fc // 2), h2s.v((SL, k, tsl), sub=tl), start=(k == 0), stop=(k == 7))
                s_ = sgd[it % 2]
                t_ = ttd[it % 2]
                it += 1
                B.act(s_.v(), pg.v(), AF.Silu)
                B.tt(t_.v(), pu.v(), s_.v(), ALU.mult)
                B.tt(he_.v((SL, fc, SL)), t_.v(), pbc.v(), ALU.mult)
            for i4 in range(4):
                i = 4 * tc + i4
                for hf in range(2):
                    po = B.ps()
                    for fc in range(4):
                        B.mm(po.v(), he_.v((SL, fc, ts_(i4))), wd_.v((SL, fc, ts_(hf, 512))), start=(fc == 0), stop=(fc == 3))
                    xv = x1.v((SL, i, ts_(hf, 512)), sub=i)
                    B.tt(xv, po.v(), xv, ALU.add)
    B.s.end_branch()
    B.ps_rot = list(range(8))
    C.dbgt.update(h2s0=V(h2s.ap[:, 0, :], h2s.v().keys), cTs8=V(cTs8.ap[0:8, :], cTs8.v().keys), ysg=V(ysg.ap[:, 0, :], ysg.v().keys), DB=DB)


def phase_F(B, C):
    x2, x2T = C.x1, C.h2T
    dr = C.dram
    B.phase(98 * 1024)
    wpg = B.alloc("wpg", [8, 1024], BF16, subs=[0, 1, 2, 3])
    wpp = B.alloc("wpp", [2, 1024], BF16)
    for j in range(4):
        cs_ = slice(j * 256, (j + 1) * 256)
        B.dma(wpg.v((SL, SL, cs_), sub=j), dr["w_ple_gate"][:, cs_].rearrange("(k p) n -> p k n", p=128), eng="pool")
    B.dma(wpp.v(), dr["w_ple_proj"].rearrange("(k p) n -> p k n", p=128), eng="pool")
    gf = B.alloc("gf", [1024], F32)
    B.dma(gf.v(), dr["final_norm_g"])
    pT = B.alloc("pT", [2, S], BF16, subs=list(range(NT)))
    xb = [B.alloc("xb%d" % i, [D], BF16) for i in range(2)]
    pt_ = [B.alloc("ptl%d" % i, [256], F32) for i in range(2)]
    pb_ = [B.alloc("pbl%d" % i, [256], BF16) for i in range(2)]
    for i in range(NT):
        b = i % 2
        B.copy(xb[b].v(), x2.v((SL, i, SL), sub=i), eng="pool")
        p = B.ps(bf=True)
        for c in range(8):
            B.transpose(p.v((SL, ts_(c))), xb[b].v((SL, ts_(c))), C.ident_b.v())
        pin = V(p.ap[:, 0:1024].rearrange("p (c t) -> p c t", c=8), p.v().keys)
        B.copy(x2T.v((SL, SL, ts_(i)), sub=i), pin, eng="act")
        B.dma(pt_[b].v(), dr["p"][ts_(i), :])
        B.copy(pb_[b].v(), pt_[b].v(), eng="dve")
        p2 = B.ps(bf=True)
        for c in range(2):
            B.transpose(p2.v((SL, ts_(c))), pb_[b].v((SL, ts_(c))), C.ident_b.v())
        pin2 = V(p2.ap[:, 0:256].rearrange("p (c t) -> p c t", c=2), p2.v().keys)
        B.copy(pT.v((SL, SL, ts_(i)), sub=i), pin2, eng="dve")
    sg = [B.alloc("sgf%d" % i, [512], F32) for i in range(2)]
    x3 = [B.alloc("x3_%d" % i, [D], F32) for i in range(2)]
    ot = [B.alloc("otf%d" % i, [D], F32) for i in range(2)]
    junk = B.alloc("junkf", [D], BF16)
    ssf = [B.alloc("ssf%d" % i, [2], F32) for i in range(2)]
    it = 0
    for i in range(NT):
        b = i % 2
        for hf in range(2):
            hs = ts_(hf, 512)
            pg = B.ps()
            for k in range(8):
                B.mm(pg.v(), x2T.v((SL, k, ts_(i)), sub=i), wpg.v((SL, k, hs), sub=[2 * hf, 2 * hf + 1]), start=(k == 0), stop=(k == 7))
            pp = B.ps()
            for k in range(2):
                B.mm(pp.v(), pT.v((SL, k, ts_(i)), sub=i), wpp.v((SL, k, hs)), start=(k == 0), stop=(k == 1))
            s_ = sg[it % 2]
            it += 1
            B.act(s_.v(), pg.v(), AF.Sigmoid)
            B.tt(s_.v(), pp.v(), s_.v(), ALU.mult)
            B.tt(x3[b].v((SL, hs)), x2.v((SL, i, hs), sub=i), s_.v(), ALU.add, eng="pool")
        B.act(junk.v(), x3[b].v(), AF.Square, accum=ssf[b].v((SL, slice(0, 1))))
        B.act(ssf[b].v((SL, slice(1, 2))), ssf[b].v((SL, slice(0, 1))), AF.Sqrt, bias=C.eps.v(), scale=1.0 / D)
        B.recip(ssf[b].v((SL, slice(1, 2))), ssf[b].v((SL, slice(1, 2))))
        B.stt(ot[b].v(), x3[b].v(), ssf[b].v((SL, slice(1, 2))), gf.v(), ALU.mult, ALU.mult)
        B.dma(C.out[ts_(i), :], ot[b].v(), is_out=True)


def build(dbg=None):
    import os
    nc = bass.Bass("TRN2", target_bir_lowering=False)
    C = Ctx()
    C.stop = os.environ.get('KSTOP', '')
    C.dbgt = {}
    C.dram = {}

    def din(name, shape, dt=F32):
        C.dram[name] = nc.dram_tensor(name, list(shape), dt, kind="ExternalInput").ap()
        return C.dram[name]

    C.x = din("x", [S, D])
    din("p", [S, 256])
    din("mix_norm_g", [128, 8])
    C.w_in = din("w_in", [D, IN_DIM])
    din("ident", [128, 128])
    din("tri_kq", [128, 128])
    din("qaug_c", [8, 4, S])
    din("kaug_c", [12, S])
    din("W_tri", [128, 384])
    din("W_tri2", [128, 384])
    din("Esel", [16, 16])
    din("conv_w", [128, 12, 4])
    din("conv_b", [128, 12])
    din("dt_bias", [128, 16])
    din("a_log", [128, 16])
    din("d_skip", [128, 16])
    din("ssd_norm_g", [128, 1024])
    din("w_out_a", [512, D])
    din("w_out_b", [D, D])
    din("w_out", [D, D])
    din("ffn_norm_g", [128, 8])
    din("w_rg", [D, 4])
    din("w_re", [D, 16])
    din("b_r", [128, 20])
    din("w_gate", [16, D, 512])
    din("w_up", [16, D, 512])
    din("w_down", [16, 512, D])
    din("w_ple_proj", [256, D])
    din("w_ple_gate", [D, D])
    din("final_norm_g", [128, D])
    din("ffn_norm_g_bc", [128, D])
    din("sel8", [8, 4])
    din("sid", [128, 24])
    din("gidx4", [128, 4])
    din("iota512", [128, 512])
    C.out = nc.dram_tensor("out", [S, D], F32, kind="ExternalOutput").ap()
    dbg_out = None
    if dbg is not None:
        dbg_out = nc.dram_tensor("dbg", list(dbg[1]), F32, kind="ExternalOutput").ap()

    B = Builder(nc, 212480)
    with ExitStack() as stack:
        B.setup(stack)
        sems = {e: stack.enter_context(nc.semaphore("sem_" + e)) for e in ENGS if e != "sp"}
        dma_sems = {q: [stack.enter_context(nc.semaphore("dsem_%s%d" % (q, i))) for i in range(Sched.NDMA)]
                    for q in ("sp", "pool")}
        eng_h = {"pe": nc.tensor, "act": nc.scalar, "dve": nc.vector, "pool": nc.gpsimd, "sp": nc.sync}
        regs = {e: stack.enter_context(eng_h[e].register("brflag_" + e)) for e in ENGS}
        block = stack.enter_context(nc.Block())

        ident_f = B.alloc("ident_f", [128], F32)
        C.ident_b = B.alloc("ident_b", [128], BF16)
        C.g1 = B.alloc("g1", [8], F32)
        B.dma(ident_f.v(), C.dram["ident"])
        B.dma(C.g1.v(), C.dram["mix_norm_g"])
        B.copy(C.ident_b.v(), ident_f.v(), eng="dve")
        C.ident_f = ident_f
        C.eps = B.alloc("eps", [1], F32)
        B.memset(C.eps.v(), EPS)
        assert B.top <= 2048
        K = 1024
        C.hT = B.alloc_at("hT", 2 * K, [8, S], BF16, subs=list(range(NT)))
        C.yaT = B.alloc_at("yaT", 34 * K, [4, S], BF16, subs=list(range(8)))
        C.ybT = B.alloc_at("ybT", 50 * K, [8, S], BF16, subs=list(range(NT)))
        C.mT = B.alloc_at("mT", 82 * K, [8, S], BF16, subs=list(range(NT)))
        C.x1 = B.alloc_at("x1", 2 * K, [NT, D], F32, subs=list(range(NT)))
        C.h2T = B.alloc_at("h2T", 66 * K, [8, S], BF16, subs=list(range(NT)))
        phase_A(B, C)
        if C.stop != "noB":
            phase_B(B, C)
        phase_C(B, C)
        phase_D(B, C)
        if dbg is not None and dbg[0] == "x1":
            B.phase(130 * K)
            for i in range(NT):
                B.dma(dbg_out[ts_(i), :], C.x1.v((SL, i, SL), sub=i), is_out=True)
        phase_E(B, C)
        if dbg is not None and dbg[0] in C.dbgt:
            B.phase(166 * K)
            dv = C.dbgt[dbg[0]]
            B.dma(dbg_out, dv if isinstance(dv, V) else dv.v(), is_out=True, eng="pool")
        if dbg is not None and dbg[0] == "x2":
            B.phase(130 * K)
            for i in range(NT):
                B.dma(dbg_out[ts_(i), :], C.x1.v((SL, i, SL), sub=i), is_out=True)
        phase_F(B, C)
        import time as _t
        _t0 = _t.time()
        B.s.emit(block, sems, dma_sems, reorder=(os.environ.get("KNOREORDER", "") == ""), regs=regs)
        if os.environ.get("KVERB"):
            print("emit s", _t.time() - _t0, "est us per segment", B.s.est, "stats", B.s.stats, "maxtop", B.maxtop)
    return nc, B


def host_consts():
    c = {}
    c["ident"] = np.eye(128, dtype=np.float32)
    s_ = np.arange(128)[:, None]
    t_ = np.arange(128)[None, :]
    c["tri_kq"] = np.where(t_ >= s_, 0.0, NEG).astype(np.float32)
    t = np.arange(S)
    bq = (t // 256).astype(np.float32)
    rq = (t % 256).astype(np.float32)
    qa = np.zeros((8, 4, S), np.float32)
    for h in range(8):
        sl = 2.0 ** (-(h + 1))
        qa[h, 0] = -8.0 * sl * 256.0 * bq
        qa[h, 1] = -8.0 * sl * rq
        qa[h, 2] = 8.0 * sl * 256.0
        qa[h, 3] = 8.0 * sl
    c["qaug_c"] = qa
    ka = np.zeros((12, S), np.float32)
    ka[0] = 1.0
    ka[1] = 1.0
    ka[2] = bq
    ka[3] = rq
    for j in range(8):
        ka[4 + j] = (bq == j).astype(np.float32)
    c["kaug_c"] = ka
    xx = np.arange(384)[None, :]
    ss_ = np.arange(128)[:, None]
    W = ((xx - 128) >= ss_).astype(np.float32)
    c["W_tri"] = W
    c["W_tri2"] = (1.0 - W).astype(np.float32)
    c["Esel"] = np.eye(16, dtype=np.float32)
    c["sel8"] = np.tile(np.eye(4, dtype=np.float32), (2, 1))
    c["sid"] = (np.arange(24)[None, :] * 128 + np.arange(128)[:, None]).astype(np.float32)
    c["gidx4"] = np.broadcast_to(np.arange(4, dtype=np.float32)[None, :], (128, 4)).copy()
    c["iota512"] = np.broadcast_to(np.arange(512, dtype=np.float32)[None, :], (128, 512)).copy()
    return c


def bc128(v):
    v = np.asarray(v, np.float32).reshape(1, -1)
    return np.ascontiguousarray(np.broadcast_to(v, (128, v.shape[1])))


def kernel(_dbg=None, **inputs):
    x = np.asarray(inputs["x"], dtype=np.float32)
    p = np.asarray(inputs["p"], dtype=np.float32)[0]
    nc, B = build(_dbg)
    f = lambda n: np.ascontiguousarray(np.asarray(inputs[n], np.float32)[0])
    shared = dict(host_consts())
    shared.update({
        "mix_norm_g": np.ascontiguousarray(f("mix_norm_g").reshape(8, 128).T),
        "w_in": f("w_in"),
        "conv_w": np.ascontiguousarray(f("conv_w").reshape(4, 12, 128).transpose(2, 1, 0)),
        "conv_b": np.ascontiguousarray(f("conv_b").reshape(12, 128).T),
        "dt_bias": bc128(f("dt_bias")), "a_log": bc128(f("a_log")),
        "d_skip": bc128(f("d_skip")), "ssd_norm_g": bc128(f("ssd_norm_g")),
        "w_out_a": f("w_out_a"), "w_out_b": f("w_out_b"), "w_out": f("w_out"),
        "ffn_norm_g": np.ascontiguousarray(f("ffn_norm_g").reshape(8, 128).T),
        "ffn_norm_g_bc": bc128(f("ffn_norm_g")),
        "w_rg": f("w_rg"), "w_re": f("w_re"),
        "b_r": bc128(np.concatenate([f("b_rg"), f("b_re")])),
        "w_gate": f("w_gate"), "w_up": f("w_up"), "w_down": f("w_down"),
        "w_ple_proj": f("w_ple_proj"), "w_ple_gate": f("w_ple_gate"),
        "final_norm_g": bc128(np.asarray(inputs["final_norm_g"], np.float32)),
    })
    in_maps = []
    for c in range(8):
        m = {"x": np.ascontiguousarray(x[c]), "p": np.ascontiguousarray(p[c])}
        m.update(shared)
        in_maps.append(m)
    res = run_bass_kernel_spmd(nc, in_maps, core_ids=list(range(8)))
    if _dbg is not None:
        return res.results[0]["dbg"]
    return np.stack([r["out"] for r in res.results], axis=0)
```
